# Optimizing a Trainium2 kernel written in Bass

```python
import math
import jax, jax.numpy as jnp
from jax import lax
import numpy as np

D_MODEL = 1024
BATCH = 16
SEQ = 2048
DEPTH = 1

MEM_LEN = 256
CHUNK = 64
ML_HEADS = 4
ML_DV = D_MODEL // ML_HEADS
ML_DQK = ML_DV // 2
ML_QK_W = ML_HEADS * ML_DQK
ML_V_W = ML_HEADS * ML_DV
GDN_DK = 128
GDN_DV = 128
GDN_QK_HEADS = D_MODEL // GDN_DK
GDN_V_HEADS = 2 * GDN_QK_HEADS
GDN_QK_W = GDN_QK_HEADS * GDN_DK
GDN_V_W = GDN_V_HEADS * GDN_DV
GDN_CONV_CH = 2 * GDN_QK_W + GDN_V_W
CONV_K = 4
XA_HEADS = 4
XA_DH = D_MODEL // XA_HEADS
N_EXPERTS = 32
TOP_K = 4
D_EXPERT = D_MODEL
SWIGLU_LIMIT = 7.0
SWIGLU_ALPHA = 1.702
MOE_BLOCK = 256
DN_ALPHA = (2 * DEPTH) ** 0.25
DN_BETA = (8 * DEPTH) ** -0.25
LN_EPS = 1e-5
RMS_EPS = 1e-6
IN_SPLITS = (ML_QK_W, ML_QK_W, ML_V_W, ML_V_W, 2 * ML_HEADS,
             GDN_CONV_CH, GDN_V_W, GDN_V_HEADS, GDN_V_HEADS, D_MODEL, D_MODEL)
IN_W = sum(IN_SPLITS)

kernel_name = 'hybrid_mlstm_gdn_xattn_moe_deepnorm'


def _split(t, sizes):
    idx = np.cumsum(sizes)[:-1].tolist()
    return jnp.split(t, idx, axis=-1)


def layer_norm(x, g, b):
    xf = x.astype(jnp.float32)
    mu = jnp.mean(xf, axis=-1, keepdims=True)
    var = jnp.mean(jnp.square(xf - mu), axis=-1, keepdims=True)
    return ((xf - mu) * lax.rsqrt(var + LN_EPS) * g + b).astype(x.dtype)


def rms_norm(x, g):
    xf = x.astype(jnp.float32)
    return xf * lax.rsqrt(jnp.mean(jnp.square(xf), axis=-1, keepdims=True) + RMS_EPS) * g


def l2_normalize(x):
    return x * lax.rsqrt(jnp.sum(jnp.square(x), axis=-1, keepdims=True) + RMS_EPS)


def _to_chunks(t):
    b, s, h = t.shape[:3]
    t = t.reshape((b, s // CHUNK, CHUNK, h) + t.shape[3:])
    return jnp.moveaxis(t, 3, 1)


def _from_chunks(t):
    b, h, n, l, d = t.shape
    return jnp.moveaxis(t, 1, 3).reshape(b, n * l, h, d)


def causal_depthwise_conv(x, w):
    c = x.shape[-1]
    return lax.conv_general_dilated(x, w[:, None, :].astype(x.dtype), window_strides=(1,),
                                    padding=[(CONV_K - 1, 0)],
                                    dimension_numbers=('NWC', 'WIO', 'NWC'),
                                    feature_group_count=c)


def mlstm_chunkwise(q, k, v, li, lf):
    q, k, v, li, lf = (_to_chunks(t) for t in (q, k, v, li, lf))
    bsz, nh = q.shape[:2]
    dk, dv = q.shape[-1], v.shape[-1]
    causal = jnp.tril(jnp.ones((CHUNK, CHUNK), dtype=bool))
    b = jnp.cumsum(lf, axis=-1)
    g = b[..., -1]
    dmat = jnp.where(causal, b[..., :, None] - b[..., None, :] + li[..., None, :], -jnp.inf)
    m_intra = jnp.max(dmat, axis=-1)
    p = jnp.exp(dmat - m_intra[..., None]) * jnp.einsum('bhnid,bhnjd->bhnij', q, k)
    intra_num = jnp.einsum('bhnij,bhnjv->bhniv', p, v)
    intra_den = jnp.sum(p, axis=-1)
    a = g[..., None] - b + li
    m_chunk = jnp.max(a, axis=-1)
    kw = k * jnp.exp(a - m_chunk[..., None])[..., None]
    xs = tuple(jnp.moveaxis(t, 2, 0) for t in
               (q, kw, v, b, g, m_chunk, m_intra, intra_num, intra_den))
    init = (jnp.zeros((bsz, nh, dk, dv), jnp.float32),
            jnp.zeros((bsz, nh, dk), jnp.float32),
            jnp.zeros((bsz, nh), jnp.float32))

    def step(carry, inp):
        c_st, n_st, m_st = carry
        q_c, kw_c, v_c, b_c, g_c, mc_c, mi_c, num_c, den_c = inp
        inter_log = b_c + m_st[..., None]
        m_out = jnp.maximum(inter_log, mi_c)
        s_inter = jnp.exp(inter_log - m_out)
        s_intra = jnp.exp(mi_c - m_out)
        num = (s_inter[..., None] * jnp.einsum('bhld,bhdv->bhlv', q_c, c_st)
               + s_intra[..., None] * num_c)
        den = s_inter * jnp.einsum('bhld,bhd->bhl', q_c, n_st) + s_intra * den_c
        h = num / jnp.maximum(jnp.abs(den), jnp.exp(-m_out))[..., None]
        m_new = jnp.maximum(g_c + m_st, mc_c)
        dec = jnp.exp(g_c + m_st - m_new)
        s_new = jnp.exp(mc_c - m_new)
        c_st = dec[..., None, None] * c_st + s_new[..., None, None] * jnp.einsum('bhld,bhlv->bhdv', kw_c, v_c)
        n_st = dec[..., None] * n_st + s_new[..., None] * jnp.sum(kw_c, axis=-2)
        return (c_st, n_st, m_new), h

    _, h = lax.scan(step, init, xs)
    return _from_chunks(jnp.moveaxis(h, 0, 2))


def gated_delta_chunkwise(q, k, v, gdec, beta):
    q, k, v, gdec, beta = (_to_chunks(t) for t in (q, k, v, gdec, beta))
    bsz, nh = q.shape[:2]
    dk, dv = q.shape[-1], v.shape[-1]
    incl = jnp.tril(jnp.ones((CHUNK, CHUNK), dtype=bool))
    strict = jnp.tril(jnp.ones((CHUNK, CHUNK), dtype=bool), k=-1)
    gam = jnp.cumsum(gdec, axis=-1)
    dec = jnp.exp(jnp.where(incl, gam[..., :, None] - gam[..., None, :], -jnp.inf))
    kk = jnp.einsum('bhnid,bhnjd->bhnij', k, k)
    t_mat = jnp.where(strict, beta[..., :, None] * kk * dec, 0.0) + jnp.eye(CHUNK, dtype=jnp.float32)
    rhs = jnp.concatenate([v * beta[..., None], k * (beta * jnp.exp(gam))[..., None]], axis=-1)
    sol = lax.linalg.triangular_solve(t_mat, rhs, left_side=True, lower=True, unit_diagonal=True)
    u, w = sol[..., :dv], sol[..., dv:]
    attn = jnp.einsum('bhnid,bhnjd->bhnij', q, k) * dec
    qd = q * jnp.exp(gam)[..., None]
    g_tot = gam[..., -1]
    kd = k * jnp.exp(g_tot[..., None] - gam)[..., None]
    xs = tuple(jnp.moveaxis(t, 2, 0) for t in (qd, w, u, attn, kd, g_tot))

    def step(s_st, inp):
        qd_c, w_c, u_c, at_c, kd_c, gt_c = inp
        v_new = u_c - jnp.einsum('bhld,bhdv->bhlv', w_c, s_st)
        o = jnp.einsum('bhld,bhdv->bhlv', qd_c, s_st) + jnp.einsum('bhij,bhjv->bhiv', at_c, v_new)
        s_st = jnp.exp(gt_c)[..., None, None] * s_st + jnp.einsum('bhld,bhlv->bhdv', kd_c, v_new)
        return s_st, o

    _, o = lax.scan(step, jnp.zeros((bsz, nh, dk, dv), jnp.float32), xs)
    return _from_chunks(jnp.moveaxis(o, 0, 2))


def hybrid_mixer(xn, w_in, ml_gate_bias, ml_norm_g, conv_w, a_log, dt_bias, gdn_norm_g,
                 w_br_ml, w_br_gdn, w_out):
    f32 = jnp.float32
    bsz, s, _ = xn.shape
    proj = xn @ w_in
    mq, mk, mv, mo, mif, gqkv, gz, ga, gb, gate_ml, gate_gdn = _split(proj, IN_SPLITS)
    q = mq.reshape(bsz, s, ML_HEADS, ML_DQK).astype(f32)
    k = mk.reshape(bsz, s, ML_HEADS, ML_DQK).astype(f32) * (ML_DQK ** -0.5)
    v = mv.reshape(bsz, s, ML_HEADS, ML_DV).astype(f32)
    pre = mif.astype(f32) + ml_gate_bias.astype(f32)
    li = pre[..., :ML_HEADS]
    lf = jax.nn.log_sigmoid(pre[..., ML_HEADS:])
    hm = mlstm_chunkwise(q, k, v, li, lf)
    hm = rms_norm(hm, ml_norm_g.reshape(ML_HEADS, ML_DV)) * \
        jax.nn.sigmoid(mo.astype(f32)).reshape(bsz, s, ML_HEADS, ML_DV)
    y_ml = hm.reshape(bsz, s, ML_V_W).astype(xn.dtype) @ w_br_ml
    c = jax.nn.silu(causal_depthwise_conv(gqkv, conv_w)).astype(f32)
    cq, ck, cv = _split(c, (GDN_QK_W, GDN_QK_W, GDN_V_W))
    rep = GDN_V_HEADS // GDN_QK_HEADS
    q = jnp.repeat(l2_normalize(cq.reshape(bsz, s, GDN_QK_HEADS, GDN_DK)), rep, axis=2) * (GDN_DK ** -0.5)
    k = jnp.repeat(l2_normalize(ck.reshape(bsz, s, GDN_QK_HEADS, GDN_DK)), rep, axis=2)
    v = cv.reshape(bsz, s, GDN_V_HEADS, GDN_DV)
    beta = jax.nn.sigmoid(gb.astype(f32))
    gdec = -jnp.exp(a_log.astype(f32)) * jax.nn.softplus(ga.astype(f32) + dt_bias.astype(f32))
    o = gated_delta_chunkwise(q, k, v, gdec, beta)
    o = rms_norm(o, gdn_norm_g) * jax.nn.silu(gz.astype(f32)).reshape(bsz, s, GDN_V_HEADS, GDN_DV)
    y_gdn = o.reshape(bsz, s, GDN_V_W).astype(xn.dtype) @ w_br_gdn
    merged = jax.nn.sigmoid(gate_ml) * y_ml + jax.nn.sigmoid(gate_gdn) * y_gdn
    return merged @ w_out


def memory_cross_attention(xn, mem, wq, wk, wv, wo):
    bsz, s, _ = xn.shape
    m = mem.shape[1]
    q = (xn @ wq).reshape(bsz, s, XA_HEADS, XA_DH)
    k = (mem @ wk).reshape(bsz, m, XA_HEADS, XA_DH)
    v = (mem @ wv).reshape(bsz, m, XA_HEADS, XA_DH)
    sc = jnp.einsum('bshd,bmhd->bhsm', q, k).astype(jnp.float32) * (XA_DH ** -0.5)
    p = jax.nn.softmax(sc, axis=-1).astype(v.dtype)
    o = jnp.einsum('bhsm,bmhd->bshd', p, v).reshape(bsz, s, D_MODEL)
    return o @ wo


def clamped_swiglu(gate, up):
    gate = jnp.minimum(gate, SWIGLU_LIMIT)
    up = jnp.clip(up, -SWIGLU_LIMIT, SWIGLU_LIMIT)
    return (up + 1.0) * (gate * jax.nn.sigmoid(SWIGLU_ALPHA * gate))


def routed_moe(h, w_router, b_router, w_gu, b_gu, w_dn, b_dn):
    bsz, s, d = h.shape
    n_tok = bsz * s
    n_asg = n_tok * TOP_K
    x2 = h.reshape(n_tok, d)
    logits = (x2 @ w_router).astype(jnp.float32) + b_router.astype(jnp.float32)
    top_v, top_e = lax.top_k(logits, TOP_K)
    gate_w = jax.nn.softmax(top_v, axis=-1)
    flat_e = top_e.reshape(n_asg)
    order = jnp.argsort(flat_e)
    sorted_e = flat_e[order]
    counts = jnp.bincount(flat_e, length=N_EXPERTS)
    padded = (counts + MOE_BLOCK - 1) // MOE_BLOCK * MOE_BLOCK
    grp_start = jnp.cumsum(counts) - counts
    pad_end = jnp.cumsum(padded)
    pad_start = pad_end - padded
    dest = pad_start[sorted_e] + (jnp.arange(n_asg) - grp_start[sorted_e])
    n_blocks = -(-n_asg // MOE_BLOCK) + N_EXPERTS
    n_rows = n_blocks * MOE_BLOCK
    row_tok = jnp.zeros((n_rows,), jnp.int32).at[dest].set((order // TOP_K).astype(jnp.int32))
    row_w = jnp.zeros((n_rows,), jnp.float32).at[dest].set(gate_w.reshape(n_asg)[order])
    block_e = jnp.minimum(jnp.searchsorted(pad_end, jnp.arange(n_blocks) * MOE_BLOCK, side='right'),
                          N_EXPERTS - 1)

    def expert_block(args):
        tok, wt, e = args
        xb = x2[tok]
        gu = xb @ w_gu[e] + b_gu[e]
        y = clamped_swiglu(gu[:, :D_EXPERT], gu[:, D_EXPERT:]) @ w_dn[e] + b_dn[e]
        return y.astype(jnp.float32) * wt[:, None]

    yb = lax.map(expert_block, (row_tok.reshape(n_blocks, MOE_BLOCK),
                                row_w.reshape(n_blocks, MOE_BLOCK), block_e))
    out = jnp.zeros((n_tok, d), jnp.float32).at[row_tok].add(yb.reshape(n_rows, d))
    return out.astype(h.dtype).reshape(bsz, s, d)


def setup_inputs(seed: int = 0) -> dict:
    key = jax.random.key(seed)
    ks = jax.random.split(key, 40)
    f32 = jnp.float32
    L = DEPTH

    def nrm(i, shape, scale):
        return jax.random.normal(ks[i], shape, f32) * scale

    dt = jnp.exp(jax.random.uniform(ks[9], (L, GDN_V_HEADS), f32, math.log(1e-3), math.log(1e-1)))
    return {
        'x': nrm(0, (BATCH, SEQ, D_MODEL), 1.0),
        'mem': nrm(1, (BATCH, MEM_LEN, D_MODEL), 1.0),
        'ln_in_g': 1.0 + nrm(2, (D_MODEL,), 0.02),
        'ln_in_b': nrm(3, (D_MODEL,), 0.02),
        'w_in': nrm(4, (L, D_MODEL, IN_W), D_MODEL ** -0.5),
        'ml_gate_bias': jnp.concatenate(
            [-2.0 + nrm(5, (L, ML_HEADS), 0.1),
             jnp.linspace(3.0, 6.0, ML_HEADS, dtype=f32)[None, :] + nrm(6, (L, ML_HEADS), 0.1)], axis=-1),
        'ml_norm_g': 1.0 + nrm(7, (L, ML_V_W), 0.02),
        'gdn_conv_w': nrm(8, (L, CONV_K, GDN_CONV_CH), CONV_K ** -0.5),
        'gdn_a_log': jnp.log(jax.random.uniform(ks[10], (L, GDN_V_HEADS), f32, 1.0, 16.0)),
        'gdn_dt_bias': dt + jnp.log(-jnp.expm1(-dt)),
        'gdn_norm_g': 1.0 + nrm(11, (L, GDN_DV), 0.02),
        'w_branch_ml': nrm(12, (L, ML_V_W, D_MODEL), ML_V_W ** -0.5),
        'w_branch_gdn': nrm(13, (L, GDN_V_W, D_MODEL), GDN_V_W ** -0.5),
        'w_mix_out': nrm(14, (L, D_MODEL, D_MODEL), DN_BETA * D_MODEL ** -0.5),
        'ln1_g': 1.0 + nrm(15, (L, D_MODEL), 0.02),
        'ln1_b': nrm(16, (L, D_MODEL), 0.02),
        'xa_wq': nrm(17, (L, D_MODEL, D_MODEL), D_MODEL ** -0.5),
        'xa_wk': nrm(18, (L, D_MODEL, D_MODEL), D_MODEL ** -0.5),
        'xa_wv': nrm(19, (L, D_MODEL, D_MODEL), D_MODEL ** -0.5),
        'xa_wo': nrm(20, (L, D_MODEL, D_MODEL), DN_BETA * D_MODEL ** -0.5),
        'ln2_g': 1.0 + nrm(21, (L, D_MODEL), 0.02),
        'ln2_b': nrm(22, (L, D_MODEL), 0.02),
        'w_router': nrm(23, (L, D_MODEL, N_EXPERTS), D_MODEL ** -0.5),
        'b_router': nrm(24, (L, N_EXPERTS), 0.01),
        'w_gu': nrm(25, (L, N_EXPERTS, D_MODEL, 2 * D_EXPERT), D_MODEL ** -0.5),
        'b_gu': nrm(26, (L, N_EXPERTS, 2 * D_EXPERT), 0.01),
        'w_dn': nrm(27, (L, N_EXPERTS, D_EXPERT, D_MODEL), DN_BETA * D_EXPERT ** -0.5),
        'b_dn': nrm(28, (L, N_EXPERTS, D_MODEL), 0.01),
        'ln3_g': 1.0 + nrm(29, (L, D_MODEL), 0.02),
        'ln3_b': nrm(30, (L, D_MODEL), 0.02),
    }


def reference(x, mem, ln_in_g, ln_in_b, w_in, ml_gate_bias, ml_norm_g, gdn_conv_w, gdn_a_log,
              gdn_dt_bias, gdn_norm_g, w_branch_ml, w_branch_gdn, w_mix_out, ln1_g, ln1_b,
              xa_wq, xa_wk, xa_wv, xa_wo, ln2_g, ln2_b, w_router, b_router, w_gu, b_gu,
              w_dn, b_dn, ln3_g, ln3_b):
    h = layer_norm(x, ln_in_g, ln_in_b)
    for l in range(DEPTH):
        mix = hybrid_mixer(h, w_in[l], ml_gate_bias[l], ml_norm_g[l], gdn_conv_w[l], gdn_a_log[l],
                           gdn_dt_bias[l], gdn_norm_g[l], w_branch_ml[l], w_branch_gdn[l], w_mix_out[l])
        h = layer_norm(DN_ALPHA * h + mix, ln1_g[l], ln1_b[l])
        xa = memory_cross_attention(h, mem, xa_wq[l], xa_wk[l], xa_wv[l], xa_wo[l])
        h = layer_norm(DN_ALPHA * h + xa, ln2_g[l], ln2_b[l])
        ff = routed_moe(h, w_router[l], b_router[l], w_gu[l], b_gu[l], w_dn[l], b_dn[l])
        h = layer_norm(DN_ALPHA * h + ff, ln3_g[l], ln3_b[l])
    return h
```

```python
import math
import numpy as np
from contextlib import ExitStack
import concourse.bass as bass
import concourse.mybir as mybir
from concourse.bass_utils import run_bass_kernel_spmd

F32 = mybir.dt.float32
BF16 = mybir.dt.bfloat16
I32 = mybir.dt.int32
AF = mybir.ActivationFunctionType
ALU = mybir.AluOpType
AX = mybir.AxisListType

D = 1024
IN_W = 11304
LN_EPS = 1e-5
RMS_EPS = 1e-6
DN_ALPHA = 2 ** 0.25
MEM = 256
NE = 32


class Buf:
    def __init__(self, t, name, parent=None):
        self.t = t
        self.name = name
        self._p = parent
        self._w = None
        self._r = []
        self.excl = False
        self.small = False

    @property
    def w(self):
        return self._p.w if self._p is not None else self._w

    @w.setter
    def w(self, v):
        if self._p is not None:
            self._p.w = v
        else:
            self._w = v

    @property
    def r(self):
        return self._p.r if self._p is not None else self._r

    @r.setter
    def r(self, v):
        if self._p is not None:
            self._p.r = v
        else:
            self._r = v

    def __getitem__(self, k):
        return self.t[k]


class FW:
    SEM_LIMIT = 30000

    def __init__(self, nc, es):
        self.nc = nc
        self.es = es
        self.es_sem = es
        self.eng = {'pe': nc.tensor, 'act': nc.scalar, 'dve': nc.vector, 'pool': nc.gpsimd, 'sp': nc.sync}
        self.esem = {}
        self.ecnt = {}
        self.known = {k: {} for k in self.eng}
        self.nsem = 0
        self.sem_owner = {}
        import os as _o
        self.force_small = False
        import os
        for k in self.eng:
            self._new_esem(k)
        self.dma_pool = [[self._sem(), 0] for _ in range(20)]
        self.dma_rr = 0
        self.n_inst = 0
        self.n_wait = 0

    def _sem(self):
        self.nsem += 1
        return self.es_sem.enter_context(self.nc.semaphore(f"sm{self.nsem}"))

    def _new_esem(self, k):
        self.esem[k] = self._sem()
        self.ecnt[k] = 0
        self.sem_owner[id(self.esem[k])] = k

    def sb(self, name, shape, dt, es=None):
        b = Buf((es or self.es).enter_context(self.nc.sbuf_tensor(name, shape, dt)), name)
        fs = 1
        for d in shape[1:]:
            fs *= d
        b.small = fs <= 256
        return b

    def ps(self, name, shape, dt, es=None):
        b = Buf((es or self.es).enter_context(self.nc.psum_tensor(name, shape, dt)), name)
        b.excl = True
        return b

    def _wait(self, ek, tok, small=True):
        if tok is None:
            return
        sem, val = tok
        key = id(sem)
        if self.sem_owner.get(key) == ek and (ek == 'pe' or (not small and not self.force_small)):
            return
        kn = self.known[ek]
        if kn.get(key, 0) >= val:
            return
        kn[key] = val
        self.eng[ek].wait_ge(sem, val)
        self.n_wait += 1

    def _deps(self, ek, r, w):
        for b in r:
            sm = b.small or (b._p is not None and b._p.small)
            self._wait(ek, b.w, sm)
            if b.excl:
                for t in b.r:
                    self._wait(ek, t, sm)
        for b in w:
            sm = b.small or (b._p is not None and b._p.small)
            self._wait(ek, b.w, sm)
            for t in b.r:
                self._wait(ek, t, sm)

    def _commit(self, tok, r, w):
        for b in r:
            if len(b.r) > 12:
                b.r = b.r[-12:] if False else b.r
            b.r.append(tok)
        for b in w:
            b.w = tok
            b.r = []

    def op(self, ek, fn, r=(), w=()):
        self._deps(ek, r, w)
        if self.ecnt[ek] >= self.SEM_LIMIT:
            self._new_esem(ek)
        ins = fn(self.eng[ek])
        self.ecnt[ek] += 1
        tok = (self.esem[ek], self.ecnt[ek])
        ins.then_inc(self.esem[ek], 1)
        self._commit(tok, r, w)
        self.n_inst += 1
        return ins

    def dma(self, ek, out, in_, r=(), w=(), fn=None, **kw):
        self._deps(ek, r, w)
        slot = self.dma_pool[self.dma_rr]
        self.dma_rr = (self.dma_rr + 1) % len(self.dma_pool)
        sem, cnt = slot
        if cnt > 0:
            self._wait(ek, (sem, cnt))
        if fn is not None:
            ins = fn(self.eng[ek])
        else:
            ins = self.eng[ek].dma_start(out=out, in_=in_, **kw)
        cnt += 16
        slot[1] = cnt
        ins.then_inc(sem, 16)
        tok = (sem, cnt)
        self._commit(tok, r, w)
        self.n_inst += 1
        return tok

    def barrier(self):
        toks = [(self.esem[k], self.ecnt[k]) for k in self.eng if self.ecnt[k] > 0]
        toks += [(sem, cnt) for sem, cnt in self.dma_pool if cnt > 0]
        for ek in self.eng:
            for t in toks:
                self._wait(ek, t)

    def finish(self, bufs):
        for b in bufs:
            self._wait('sp', b.w)


def build_nc(n_seq=2, S=2048, stage=99, dbg=()):
    nc = bass.Bass("TRN2", target_bir_lowering=False)
    T = n_seq * S
    NSUB = S // 128
    lnscale = math.log(128 ** -0.5)

    def din(name, shape, dt=F32):
        return Buf(nc.dram_tensor(name, list(shape), dt, kind="ExternalInput").ap(), name)

    def dscr(name, shape, dt):
        return Buf(nc.dram_tensor(name, list(shape), dt, kind="Internal").ap(), name)

    x_d = din("x", [T, D])
    mem_d = din("mem", [n_seq * MEM, D])
    vec_names = ["ln_in_g", "ln_in_b", "ln1_g", "ln1_b", "ln2_g", "ln2_b"]
    vec_late = ["ln3_g", "ln3_b"]
    vec_d = {n: din(n, [1, D]) for n in vec_names + vec_late}
    w_in_d = din("w_in", [D, IN_W])
    ml_gate_bias_d = din("ml_gate_bias", [1, 8])
    conv_w_d = din("gdn_conv_w", [4, 4096])
    a_log_d = din("gdn_a_log", [1, 16])
    dt_bias_d = din("gdn_dt_bias", [1, 16])
    gdn_norm_g_d = din("gdn_norm_g", [1, 128])
    w_br_ml_d = din("w_branch_ml", [D, D])
    w_br_gdn_d = din("w_branch_gdn", [2048, D])
    w_mix_d = din("w_mix_out", [D, D])
    xa_wq_d = din("xa_wq", [D, D])
    xa_wk_d = din("xa_wk", [D, D])
    xa_wv_d = din("xa_wv", [D, D])
    xa_wo_d = din("xa_wo", [D, D])
    w_router_d = din("w_router", [D, NE])
    b_router_d = din("b_router", [1, NE])
    w_gu_d = din("w_gu", [NE, D, 2 * D])
    b_gu_d = din("b_gu", [NE, 2 * D])
    w_dn_d = din("w_dn", [NE, D, D])
    b_dn_d = din("b_dn", [NE, D])
    out_d = Buf(nc.dram_tensor("out", [T, D], F32, kind="ExternalOutput").ap(), "out")
    dbg_d = {}
    for nm, shape in dbg:
        dbg_d[nm] = Buf(nc.dram_tensor(nm, list(shape), F32, kind="ExternalOutput").ap(), nm)

    w_in_b = dscr("w_in_b", [D, IN_W], BF16)
    w_br_ml_b = dscr("w_br_ml_b", [D, D], BF16)
    w_br_gdn_b = dscr("w_br_gdn_b", [2048, D], BF16)
    w_mix_b = dscr("w_mix_b", [D, D], BF16)
    xa_wq_b = dscr("xa_wq_b", [D, D], BF16)
    xa_wk_b = dscr("xa_wk_b", [D, D], BF16)
    xa_wv_b = dscr("xa_wv_b", [D, D], BF16)
    xa_wo_b = dscr("xa_wo_b", [D, D], BF16)

    h2_d = dscr("h2_d", [T, D], F32)
    h2T_d = dscr("h2T_d", [T // 128, 128, 8, 128], BF16)
    CAP = 768 if T >= 4096 else 128
    NSLOT = NE * CAP
    CBIG = 65535.0
    xg_d = dscr("xg_d", [NSLOT, D], F32)
    yg_d = dscr("yg_d", [NSLOT, D], F32)

    with ExitStack() as es:
        fw = FW(nc, es)
        es1 = es.enter_context(ExitStack())
        V = lambda fn, r=(), w=(): fw.op('dve', fn, r, w)
        A = lambda fn, r=(), w=(): fw.op('act', fn, r, w)
        G = lambda fn, r=(), w=(): fw.op('pool', fn, r, w)
        P = lambda fn, r=(), w=(): fw.op('pe', fn, r, w)

        ones_f = fw.sb("ones_f", [128, 128], F32)
        ident = fw.sb("ident", [128, 128], F32)
        UT = fw.sb("UT", [128, 128], F32)
        G(lambda e: e.memset(ones_f[:], 1.0), w=[ones_f])
        G(lambda e: e.affine_select(out=ident[:], in_=ones_f[:], pattern=[[-1, 128]], compare_op=ALU.is_equal,
                                    fill=0.0, base=0, channel_multiplier=1), r=[ones_f], w=[ident])
        G(lambda e: e.affine_select(out=UT[:], in_=ones_f[:], pattern=[[1, 128]], compare_op=ALU.is_ge,
                                    fill=0.0, base=0, channel_multiplier=-1), r=[ones_f], w=[UT])

        nhalf = fw.sb("nhalf", [128, 128], F32)
        G(lambda e: e.memset(nhalf[:], -0.5), w=[nhalf])
        st6 = fw.sb("st6", [128, 2, 6], F32)
        mv2 = fw.sb("mv2", [128, 2], F32)
        rstd1 = fw.sb("rstd1", [128, 1], F32)
        eps_t = fw.sb("eps_t", [128, 4], F32)
        NT_ALL = T // 128
        bc_reg = nc.gpsimd.to_reg(NSLOT - 1)
        slot_i = fw.sb("slot_i", [128, NT_ALL, 4], I32)
        gwk = fw.sb("gwk", [128, NT_ALL, 4], F32)
        fw.es = es1

        vec_t = {}
        for n in vec_names:
            vec_t[n] = fw.sb("c_" + n, [128, D], F32, es=es1)
            fw.dma('sp', vec_t[n][:], vec_d[n].t.partition_broadcast(128), r=[vec_d[n]], w=[vec_t[n]])
        mlb_t = fw.sb("mlb_t", [128, 8], F32)
        fw.dma('sp', mlb_t[:], ml_gate_bias_d.t.partition_broadcast(128), r=[ml_gate_bias_d], w=[mlb_t])
        w_small = fw.sb("w_small", [128, 8, 40], F32)
        wv = w_in_d.t.rearrange("(kc p) n -> p kc n", p=128)
        with nc.allow_non_contiguous_dma(reason="small gate columns"):
            fw.dma('sp', w_small[:, :, 0:8], wv[:, :, 3072:3080], r=[w_in_d], w=[w_small])
            fw.dma('sp', w_small[:, :, 8:40], wv[:, :, 9224:9256], r=[w_in_d], w=[w_small])

        banks = [fw.ps(f"bank{i}", [128, 512], F32) for i in range(8)]
        bank_rr = [0]

        def bank():
            b = banks[bank_rr[0]]
            bank_rr[0] = (bank_rr[0] + 1) % 8
            return b

        slabs = [fw.sb(f"slab{i}", [128, 8, 512], BF16, es=es1) for i in range(4)]
        cast_tmp = slabs
        cast_rr = [0]

        w_src = {"w_in_b": w_in_d, "w_br_ml_b": w_br_ml_d, "w_br_gdn_b": w_br_gdn_d, "w_mix_b": w_mix_d,
                 "xa_wq_b": xa_wq_d, "xa_wk_b": xa_wk_d, "xa_wv_b": xa_wv_d, "xa_wo_b": xa_wo_d}
        slab_keys = [("w_in_b", 0, c) for c in ([0, 512, 1024, 1536, 2048, 2560] + [3080 + 512 * j for j in range(8)]
                                               + [7176 + 512 * j for j in range(4)] + [9256, 9768, 10280, 10792])]
        if stage >= 3:
            slab_keys += [("w_br_ml_b", 0, 0), ("w_br_ml_b", 0, 512)]
            slab_keys += [("w_br_gdn_b", k0, c) for c in (0, 512) for k0 in (0, 8)]
            for nm in ["w_mix_b", "xa_wq_b", "xa_wk_b", "xa_wv_b", "xa_wo_b"]:
                slab_keys += [(nm, 0, 0), (nm, 0, 512)]
        slab_idx = {k: i for i, k in enumerate(slab_keys)}
        wslab_d = dscr("wslab_d", [len(slab_keys), 128, 4096], BF16)
        wslab_b = [Buf(wslab_d.t[i], f"wslab{i}") for i in range(len(slab_keys))]
        g8_tm = fw.sb("g8_tm", [8, 128], F32, es=es1)
        g1_tm = fw.sb("g1_tm", [1, 128], F32, es=es1)
        gfm_ml = fw.sb("gfm_ml", [128, 8], F32, es=es1)
        gfm_gdn = fw.sb("gfm_gdn", [128, 1], F32, es=es1)
        ml_norm_g_d = din("ml_norm_g", [1, D])
        fw.dma('sp', g8_tm[:], ml_norm_g_d.t.rearrange("o (c p) -> (o c) p", p=128), r=[ml_norm_g_d], w=[g8_tm])
        fw.dma('sp', g1_tm[:], gdn_norm_g_d.t, r=[gdn_norm_g_d], w=[g1_tm])
        bkq = bank()
        P(lambda e: e.transpose(bkq[:, 0:8], g8_tm[0:8, :], ident[0:8, 0:8]), r=[g8_tm, ident], w=[bkq])
        P(lambda e: e.transpose(bkq[:, 8:9], g1_tm[0:1, :], ident[0:1, 0:1]), r=[g1_tm, ident], w=[bkq])
        V(lambda e: e.tensor_copy(out=gfm_ml[:], in_=bkq[:, 0:8]), r=[bkq], w=[gfm_ml])
        V(lambda e: e.tensor_copy(out=gfm_gdn[:], in_=bkq[:, 8:9]), r=[bkq], w=[gfm_gdn])
        for (nm, k0, c0) in slab_keys:
            src = w_src[nm]
            tmp = cast_tmp[cast_rr[0]]
            cast_rr[0] = (cast_rr[0] + 1) % len(cast_tmp)
            sv = src.t[k0 * 128:(k0 + 8) * 128, c0:c0 + 512].rearrange("(kc p) n -> p kc n", p=128)
            fw.dma('pool', tmp[:], sv, r=[src], w=[tmp])
            if nm == "w_br_ml_b":
                for kc in range(8):
                    V(lambda e: e.tensor_scalar(out=tmp[:, kc, :], in0=tmp[:, kc, :], scalar1=gfm_ml[:, kc:kc + 1], scalar2=None, op0=ALU.mult),
                      r=[tmp, gfm_ml], w=[tmp])
            elif nm == "w_br_gdn_b":
                V(lambda e: e.tensor_scalar(out=tmp[:].rearrange("p a b -> p (a b)"), in0=tmp[:].rearrange("p a b -> p (a b)"),
                                            scalar1=gfm_gdn[:, 0:1], scalar2=None, op0=ALU.mult), r=[tmp, gfm_gdn], w=[tmp])
            fw.dma('sp', wslab_d.t[slab_idx[(nm, k0, c0)]], tmp[:].rearrange("p a b -> p (a b)"), r=[tmp], w=[wslab_b[slab_idx[(nm, k0, c0)]]])

        slab_rr = [0]

        def load_slab(wb, k0, kc, c0, w):
            sl = slabs[slab_rr[0]]
            slab_rr[0] = (slab_rr[0] + 1) % len(slabs)
            fw.dma('sp', sl[:].rearrange("p a b -> p (a b)"), wslab_d.t[slab_idx[(wb.name, k0, c0)]], r=[wslab_b[slab_idx[(wb.name, k0, c0)]]], w=[sl])
            return sl


        def layernorm(src, dst, gname, bname):
            for hf in range(2):
                V(lambda e: e.bn_stats(out=st6[:, hf, :], in_=src[:, hf * 512:(hf + 1) * 512]), r=[src], w=[st6])
            V(lambda e: e.bn_aggr(out=mv2[:], in_=st6[:].rearrange("p a b -> p (a b)")), r=[st6], w=[mv2])
            V(lambda e: e.tensor_scalar(out=rstd1[:], in0=mv2[:, 1:2], scalar1=LN_EPS, scalar2=None, op0=ALU.add), r=[mv2], w=[rstd1])
            G(lambda e: e.tensor_tensor(out=rstd1[:], in0=rstd1[:], in1=nhalf[:, 0:1], op=ALU.pow), r=[rstd1, nhalf], w=[rstd1])
            V(lambda e: e.tensor_scalar(out=dst[:], in0=src[:], scalar1=mv2[:, 0:1], scalar2=rstd1[:, 0:1],
                                        op0=ALU.subtract, op1=ALU.mult), r=[src, mv2, rstd1], w=[dst])
            V(lambda e: e.tensor_tensor(out=dst[:], in0=dst[:], in1=vec_t[gname][:], op=ALU.mult), r=[dst, vec_t[gname]], w=[dst])
            V(lambda e: e.tensor_tensor(out=dst[:], in0=dst[:], in1=vec_t[bname][:], op=ALU.add), r=[dst, vec_t[bname]], w=[dst])

        G(lambda e: e.memset(eps_t[:, 0:1], LN_EPS), w=[eps_t])
        G(lambda e: e.memset(eps_t[:, 1:2], RMS_EPS), w=[eps_t])
        G(lambda e: e.memset(eps_t[:, 2:3], lnscale), w=[eps_t])
        G(lambda e: e.memset(eps_t[:, 3:4], 1.0), w=[eps_t])


        def transpose_fm(src, ncol, dst_bf=None, dst_f=None):
            nch = ncol // 128
            for c0 in range(0, nch, 4):
                n = min(4, nch - c0)
                bk = bank()
                for c in range(n):
                    P(lambda e: e.transpose(bk[:, c * 128:(c + 1) * 128], src[:, (c0 + c) * 128:(c0 + c + 1) * 128], ident[:]),
                      r=[src, ident], w=[bk])
                pv = bk[:, 0:n * 128].rearrange("p (c t) -> p c t", t=128)
                if dst_bf is not None:
                    A(lambda e: e.activation(out=dst_bf[:, c0:c0 + n, :], in_=pv, func=AF.Copy), r=[bk], w=[dst_bf])
                if dst_f is not None:
                    V(lambda e: e.tensor_copy(out=dst_f[:, c0:c0 + n, :], in_=pv), r=[bk], w=[dst_f])

        def mm_tm(ps_ap, psb, xT, sl, kc, w, k_off=0):
            for k in range(kc):
                P(lambda e: e.matmul(ps_ap, lhsT=xT[:, k_off + k, :], rhs=sl[:, k, 0:w], start=(k == 0), stop=(k == kc - 1)),
                  r=[xT, sl], w=[psb])

        def mm_fm(ps_ap, psb, xT, sl, kc, m0, ntok=128):
            for k in range(kc):
                P(lambda e: e.matmul(ps_ap, lhsT=sl[:, k, m0:m0 + 128], rhs=xT[:, k, 0:ntok], start=(k == 0), stop=(k == kc - 1)),
                  r=[xT, sl], w=[psb])

        xt = fw.sb("xt", [128, D], F32)
        h0 = fw.sb("h0", [128, D], F32)
        xnT = fw.sb("xnT", [128, 8, 128], BF16)
        mqT = fw.sb("mqT", [128, 4, 128], BF16)
        mkT = fw.sb("mkT", [128, 4, 128], BF16)
        mk_tm = fw.sb("mk_tm", [128, 4, 128], F32)
        mv_aug = fw.sb("mv_aug", [128, 4, 258], BF16)
        mo_sig = fw.sb("mo_sig", [128, D], BF16)
        gsm = fw.sb("gsm", [128, 40], F32)
        C_f = [fw.sb(f"C_f{h}", [128, 258], F32) for h in range(4)]
        C_b = [fw.sb(f"C_b{h}", [128, 258], BF16) for h in range(4)]
        hm = fw.sb("hm", [128, D], F32)
        hm_n = fw.sb("hm_n", [128, D], F32)
        PTb = [fw.sb(f"PTb{i}", [128, 128], BF16) for i in range(2)]
        kwb = [fw.sb(f"kwb{i}", [128, 128], BF16) for i in range(2)]
        g8 = fw.sb("g8", [128, 8], F32)
        nlf = fw.sb("nlf", [128, 4], F32)
        t1 = fw.sb("t1", [128, 4], F32)
        t2 = fw.sb("t2", [128, 4], F32)
        a_p = fw.sb("a_p", [128, 4], F32)
        ag_p = fw.sb("ag_p", [128, 4], F32)
        eb = fw.sb("eb", [128, 4], F32)
        eG = fw.sb("eG", [128, 4], F32)
        dsc = fw.sb("dsc", [128, 4], F32)
        ssq = fw.sb("ssq", [128, 16], F32)
        junk = fw.sb("junk", [128, 256], F32)
        G(lambda e: e.memset(mv_aug[:], 1.0), w=[mv_aug])

        def dbg_out(name, src_ap, srcb, row0, ncol):
            if name in dbg_d:
                fw.dma('sp', dbg_d[name].t[row0:row0 + 128, 0:ncol], src_ap, r=[srcb], w=[dbg_d[name]])

        BLK = fw.sb("BLK", [128, 128], F32)
        Ublk = fw.sb("Ublk", [128, 128], F32)
        Yblk = fw.sb("Yblk", [128, 128], F32)
        maskS = fw.sb("maskS", [128, 128], F32)
        sel0 = fw.sb("sel0", [128, 128], F32)
        sel1 = fw.sb("sel1", [128, 128], F32)
        G(lambda e: e.memset(BLK[:], 0.0), w=[BLK])
        G(lambda e: e.memset(BLK[0:64, 0:64], 1.0), w=[BLK])
        G(lambda e: e.memset(BLK[64:128, 64:128], 1.0), w=[BLK])
        G(lambda e: e.memset(sel0[:], 0.0), w=[sel0])
        G(lambda e: e.memset(sel0[0:64, :], 1.0), w=[sel0])
        G(lambda e: e.memset(sel1[:], 0.0), w=[sel1])
        G(lambda e: e.memset(sel1[64:128, :], 1.0), w=[sel1])
        G(lambda e: e.tensor_tensor(out=Ublk[:], in0=UT[:], in1=BLK[:], op=ALU.mult), r=[UT, BLK], w=[Ublk])
        G(lambda e: e.tensor_tensor(out=Yblk[:], in0=BLK[:], in1=Ublk[:], op=ALU.subtract), r=[BLK, Ublk], w=[Yblk])
        G(lambda e: e.tensor_tensor(out=maskS[:], in0=Ublk[:], in1=ident[:], op=ALU.subtract), r=[Ublk, ident], w=[maskS])
        cw_tm = fw.sb("cw_tm", [32, 4, 128], F32)
        cw = fw.sb("cw", [128, 4, 32], F32)
        fw.dma('sp', cw_tm[:], conv_w_d.t.rearrange("j (c p) -> c j p", p=128), r=[conv_w_d], w=[cw_tm])
        bkc = bank()
        for j in range(4):
            P(lambda e: e.transpose(bkc[:, j * 32:(j + 1) * 32], cw_tm[0:32, j, :], ident[0:32, 0:32]), r=[cw_tm, ident], w=[bkc])
        V(lambda e: e.tensor_copy(out=cw[:].rearrange("p j c -> p (j c)"), in_=bkc[:, 0:128]), r=[bkc], w=[cw])
        expA = fw.sb("expA", [128, 16], F32)
        dtb = fw.sb("dtb", [128, 16], F32)
        fw.dma('sp', expA[:], a_log_d.t.partition_broadcast(128), r=[a_log_d], w=[expA])
        fw.dma('sp', dtb[:], dt_bias_d.t.partition_broadcast(128), r=[dt_bias_d], w=[dtb])
        A(lambda e: e.activation(out=expA[:], in_=expA[:], func=AF.Exp), r=[expA], w=[expA])
        halo = fw.sb("halo", [128, 32, 3], F32)
        zcb = [fw.sb(f"zc{i}", [128, 131], F32) for i in range(6)]
        ycb = [fw.sb(f"yc{i}", [128, 128], F32) for i in range(6)]
        scb = [fw.sb(f"sc{i}", [128, 128], F32) for i in range(6)]
        sqb = [fw.sb(f"sq{i}", [128, 128], F32) for i in range(6)]
        gqT = fw.sb("gqT", [128, 8, 128], BF16)
        gkT = fw.sb("gkT", [128, 8, 128], BF16)
        k_tm = fw.sb("k_tm", [128, 8, 128], F32)
        v_tm = fw.sb("v_tm", [128, 16, 128], BF16)
        gz_silu = fw.sb("gz_silu", [128, 2048], BF16)
        gg = fw.sb("gg", [128, 16 * 8], F32)
        S_f = [fw.sb(f"S_f{h}", [128, 128], F32) for h in range(16)]
        S_b = [fw.sb(f"S_b{h}", [128, 128], BF16) for h in range(16)]
        vnew = [fw.sb(f"vnew{h}", [128, 128], BF16) for h in range(16)]
        o_g = fw.sb("o_g", [128, 2048], F32)
        o_n = o_g
        xnT_f = Buf(o_g.t[:, 0:1024].rearrange("p (c t) -> p c t", t=128), "xnT_f", parent=o_g)
        GH = 4
        kkS = [fw.sb(f"kkS{i}", [128, 128], F32) for i in range(2)]
        qkS = [fw.sb(f"qkS{i}", [128, 128], F32) for i in range(2)]
        Ld = [fw.sb(f"Ld{i}", [128, 128], F32) for i in range(GH)]
        decb = [fw.sb(f"dec{i}", [128, 128], F32) for i in range(GH)]
        Nb = [[fw.sb(f"N{i}_{j}", [128, 128], F32) for j in range(2)] for i in range(GH)]
        Mb = [[fw.sb(f"M{i}_{j}", [128, 128], F32) for j in range(2)] for i in range(GH)]
        Rf = [fw.sb(f"Rf{i}", [128, 128], F32) for i in range(GH)]
        attnT = [fw.sb(f"attnT{i}", [128, 128], BF16) for i in range(8)]
        Rb = [fw.sb(f"Rb{i}", [128, 128], BF16) for i in range(8)]
        negWT = [fw.sb(f"negWT{i}", [128, 128], BF16) for i in range(8)]
        kd = [fw.sb(f"kd{i}", [128, 128], BF16) for i in range(8)]
        kgb = [fw.sb(f"kg{i}", [128, 128], BF16) for i in range(2)]
        p2s = [fw.sb(f"p2s{i}", [128, 128], F32) for i in range(2)]
        BETA, NBETA, NGD, EG, EGR, TMPG, ES = 0, 16, 32, 48, 64, 80, 96

        w_rt = fw.sb("w_rt", [128, 8, NE], F32)
        b_rt = fw.sb("b_rt", [128, NE], F32)
        with nc.allow_non_contiguous_dma(reason="router weights"):
            fw.dma('sp', w_rt[:], w_router_d.t.rearrange("(kc p) n -> p kc n", p=128), r=[w_router_d], w=[w_rt])
        fw.dma('sp', b_rt[:], b_router_d.t.partition_broadcast(128), r=[b_router_d], w=[b_rt])
        rt = fw.sb("rt", [128, 8, NE], F32)
        top8 = fw.sb("top8", [128, 24], F32)
        UTs = fw.sb("UTs", [128, 128], F32)
        G(lambda e: e.tensor_tensor(out=UTs[:], in0=UT[:], in1=ident[:], op=ALU.subtract), r=[UT, ident], w=[UTs])
        base_c = fw.sb("base_c", [128, NE], F32)
        G(lambda e: e.memset(base_c[:], 0.0), w=[base_c])
        eoff_i = fw.sb("eoff_i", [128, NE], I32)
        eoff = fw.sb("eoff", [128, NE], F32)
        G(lambda e: e.iota(eoff_i[:], pattern=[[CAP, NE]], base=0, channel_multiplier=0), w=[eoff_i])
        V(lambda e: e.tensor_copy(out=eoff[:], in_=eoff_i[:]), r=[eoff_i], w=[eoff])
        hmT = fw.sb("hmT", [128, 8, 128], BF16)
        oT = fw.sb("oT", [128, 16, 128], BF16)
        gsig = [fw.sb(f"gsig{i}", [128, 512], F32) for i in range(2)]
        memT = Buf(gz_silu.t[:].rearrange("p (k t) -> p k t", t=256), "memT", parent=gz_silu)
        KT = fw.sb("KT", [128, 8, 256], BF16)
        Vx = fw.sb("Vx", [128, 2, 1024], BF16)
        sc_all = fw.sb("sc_all", [128, 4, 256], F32)
        pT_all = fw.sb("pT_all", [128, 8, 128], BF16)
        smx = fw.sb("smx", [128, 16], F32)
        merged = hm
        h1 = xt
        h2 = hm_n

        import os as _os
        _prof = False
        cur_scope = [None]

        def mark(name, active=True):
            if cur_scope[0] is not None:
                nc.leave_named_scope(cur_scope[0][0], cur_scope[0][1], False)
                cur_scope[0] = None
            if _prof and active and name:
                sid, _ = nc.enter_named_scope(name, False)
                cur_scope[0] = (name, sid)

        for si in range(n_seq):
            for b_ in C_f + C_b + S_f + S_b:
                G(lambda e: e.memset(b_[:], 0.0), w=[b_])
            G(lambda e: e.memset(halo[:], 0.0), w=[halo])

            if stage >= 4:
                for mt in range(2):
                    fw.dma('sp', o_g[:, 0:1024], mem_d.t[si * MEM + mt * 128: si * MEM + (mt + 1) * 128, :], r=[mem_d], w=[o_g])
                    for c0 in range(0, 8, 4):
                        bk = bank()
                        for c in range(4):
                            P(lambda e: e.transpose(bk[:, c * 128:(c + 1) * 128], o_g[:, (c0 + c) * 128:(c0 + c + 1) * 128], ident[:]), r=[o_g, ident], w=[bk])
                        A(lambda e: e.activation(out=memT[:, c0:c0 + 4, mt * 128:(mt + 1) * 128], in_=bk[:, 0:512].rearrange("p (c t) -> p c t", t=128), func=AF.Copy),
                          r=[bk], w=[memT])
                for j in range(2):
                    sl = load_slab(xa_wk_b, 0, 8, j * 512, 512)
                    for m in range(4):
                        bk = bank()
                        mm_fm(bk[:, 0:256], bk, memT, sl, 8, m * 128, ntok=256)
                        A(lambda e: e.activation(out=KT[:, j * 4 + m, :], in_=bk[:, 0:256], func=AF.Copy), r=[bk], w=[KT])
                for j in range(2):
                    sl = load_slab(xa_wv_b, 0, 8, j * 512, 512)
                    for mt in range(2):
                        bk = bank()
                        for k in range(8):
                            P(lambda e: e.matmul(bk[:, 0:512], lhsT=memT[:, k, mt * 128:(mt + 1) * 128], rhs=sl[:, k, 0:512], start=(k == 0), stop=(k == 7)),
                              r=[memT, sl], w=[bk])
                        A(lambda e: e.activation(out=Vx[:, mt, j * 512:(j + 1) * 512], in_=bk[:, 0:512], func=AF.Copy), r=[bk], w=[Vx])
            for ti in range(NSUB):
                row0 = si * S + ti * 128
                mark('ln_in', si == 0 and ti == 2)
                fw.dma('sp', xt[:], x_d.t[row0:row0 + 128, :], r=[x_d], w=[xt])
                layernorm(xt, h0, "ln_in_g", "ln_in_b")
                if stage == 0:
                    fw.dma('sp', out_d.t[row0:row0 + 128, :], h0[:], r=[h0], w=[out_d])
                    continue
                transpose_fm(h0, D, dst_bf=xnT, dst_f=xnT_f)
                if stage == -1:
                    V(lambda e: e.tensor_copy(out=hm_n[:].rearrange("p (c t) -> p c t", t=128), in_=xnT[:]), r=[xnT], w=[hm_n])
                    fw.dma('sp', out_d.t[row0:row0 + 128, :], hm_n[:], r=[hm_n], w=[out_d])
                    continue
                mark('proj_small', si == 0 and ti == 2)
                bk = bank()
                for k in range(8):
                    P(lambda e: e.matmul(bk[:, 0:40], lhsT=xnT_f[:, k, :], rhs=w_small[:, k, :], start=(k == 0), stop=(k == 7)),
                      r=[xnT_f, w_small], w=[bk])
                V(lambda e: e.tensor_copy(out=gsm[:], in_=bk[:, 0:40]), r=[bk], w=[gsm])
                mark('proj_mlstm', si == 0 and ti == 2)
                sl = load_slab(w_in_b, 0, 8, 0, 512)
                for m in range(4):
                    bk = bank()
                    mm_fm(bk[:, 0:128], bk, xnT, sl, 8, m * 128)
                    A(lambda e: e.activation(out=mqT[:, m, :], in_=bk[:, 0:128], func=AF.Copy), r=[bk], w=[mqT])
                sl = load_slab(w_in_b, 0, 8, 512, 512)
                for m in range(4):
                    bk = bank()
                    mm_fm(bk[:, 0:128], bk, xnT, sl, 8, m * 128)
                    A(lambda e: e.activation(out=mkT[:, m, :], in_=bk[:, 0:128], func=AF.Copy), r=[bk], w=[mkT])
                bk = bank()
                mm_tm(bk[:, 0:512], bk, xnT, sl, 8, 512)
                V(lambda e: e.tensor_copy(out=mk_tm[:].rearrange("p h d -> p (h d)"), in_=bk[:, 0:512]), r=[bk], w=[mk_tm])
                for j in range(2):
                    sl = load_slab(w_in_b, 0, 8, 1024 + j * 512, 512)
                    bk = bank()
                    mm_tm(bk[:, 0:512], bk, xnT, sl, 8, 512)
                    A(lambda e: e.activation(out=mv_aug[:, 2 * j:2 * j + 2, 0:256], in_=bk[:, 0:512].rearrange("p (h d) -> p h d", d=256),
                                             func=AF.Copy), r=[bk], w=[mv_aug])
                for j in range(2):
                    sl = load_slab(w_in_b, 0, 8, 2048 + j * 512, 512)
                    bk = bank()
                    mm_tm(bk[:, 0:512], bk, xnT, sl, 8, 512)
                    A(lambda e: e.activation(out=mo_sig[:, j * 512:(j + 1) * 512], in_=bk[:, 0:512], func=AF.Sigmoid), r=[bk], w=[mo_sig])
                mark('mlstm_gates', si == 0 and ti == 2)
                V(lambda e: e.tensor_tensor(out=g8[:], in0=gsm[:, 0:8], in1=mlb_t[:], op=ALU.add), r=[gsm, mlb_t], w=[g8])
                A(lambda e: e.activation(out=nlf[:], in_=g8[:, 4:8], func=AF.Exp, scale=-1.0), r=[g8], w=[nlf])
                A(lambda e: e.activation(out=nlf[:], in_=nlf[:], func=AF.Ln, bias=eps_t[:, 3:4], scale=1.0), r=[nlf, eps_t], w=[nlf])
                bk = bank()
                P(lambda e: e.matmul(bk[:, 0:4], lhsT=UT[:], rhs=nlf[:], start=True, stop=True), r=[UT, nlf], w=[bk])
                P(lambda e: e.matmul(bk[:, 4:8], lhsT=ones_f[:], rhs=nlf[:], start=True, stop=True), r=[ones_f, nlf], w=[bk])
                V(lambda e: e.tensor_tensor(out=t1[:], in0=g8[:, 0:4], in1=bk[:, 0:4], op=ALU.add), r=[g8, bk], w=[t1])
                V(lambda e: e.tensor_tensor(out=t2[:], in0=t1[:], in1=bk[:, 4:8], op=ALU.subtract), r=[t1, bk], w=[t2])
                A(lambda e: e.activation(out=a_p[:], in_=t1[:], func=AF.Exp, bias=eps_t[:, 2:3], scale=1.0), r=[t1, eps_t], w=[a_p])
                A(lambda e: e.activation(out=ag_p[:], in_=t2[:], func=AF.Exp, bias=eps_t[:, 2:3], scale=1.0), r=[t2, eps_t], w=[ag_p])
                A(lambda e: e.activation(out=eb[:], in_=bk[:, 0:4], func=AF.Exp, scale=-1.0), r=[bk], w=[eb])
                A(lambda e: e.activation(out=eG[:], in_=bk[:, 4:8], func=AF.Exp, scale=-1.0), r=[bk], w=[eG])
                mark('mlstm_heads', si == 0 and ti == 2)
                for h in range(4):
                    pt = PTb[h % 2]
                    kw = kwb[h % 2]
                    bk = bank()
                    P(lambda e: e.matmul(bk[:, 0:128], lhsT=mkT[:, h, :], rhs=mqT[:, h, :], start=True, stop=True), r=[mkT, mqT], w=[bk])
                    V(lambda e: e.scalar_tensor_tensor(out=pt[:], in0=bk[:, 0:128], scalar=a_p[:, h:h + 1], in1=UT[:],
                                                       op0=ALU.mult, op1=ALU.mult), r=[bk, a_p, UT], w=[pt])
                    bk2 = bank()
                    P(lambda e: e.matmul(bk2[:, 0:257], lhsT=mqT[:, h, :], rhs=C_b[h][:, 0:257], start=True, stop=False), r=[mqT, C_b[h]], w=[bk2])
                    P(lambda e: e.matmul(bk2[:, 0:257], lhsT=pt[:], rhs=mv_aug[:, h, 0:257], start=False, stop=True), r=[pt, mv_aug], w=[bk2])
                    V(lambda e: e.tensor_tensor(out=dsc[:, 0:1], in0=bk2[:, 256:257], in1=eb[:, h:h + 1], op=ALU.mult), r=[bk2, eb], w=[dsc])
                    V(lambda e: e.tensor_scalar(out=dsc[:, 2:3], in0=dsc[:, 0:1], scalar1=-1.0, scalar2=None, op0=ALU.mult), r=[dsc], w=[dsc])
                    V(lambda e: e.scalar_tensor_tensor(out=dsc[:, 1:2], in0=dsc[:, 2:3], scalar=1.0, in1=dsc[:, 0:1], op0=ALU.max, op1=ALU.max),
                      r=[dsc], w=[dsc])
                    V(lambda e: e.reciprocal(out=dsc[:, 2:3], in_=dsc[:, 1:2]), r=[dsc], w=[dsc])
                    V(lambda e: e.tensor_tensor(out=dsc[:, 3:4], in0=dsc[:, 2:3], in1=eb[:, h:h + 1], op=ALU.mult), r=[dsc, eb], w=[dsc])
                    V(lambda e: e.tensor_scalar(out=hm[:, h * 256:(h + 1) * 256], in0=bk2[:, 0:256], scalar1=dsc[:, 3:4], scalar2=None,
                                                op0=ALU.mult), r=[bk2, dsc], w=[hm])
                    V(lambda e: e.tensor_scalar(out=kw[:], in0=mk_tm[:, h, :], scalar1=ag_p[:, h:h + 1], scalar2=None, op0=ALU.mult),
                      r=[mk_tm, ag_p], w=[kw])
                    bk3 = bank()
                    P(lambda e: e.matmul(bk3[:, 0:257], lhsT=kw[:], rhs=mv_aug[:, h, 0:257], start=True, stop=True), r=[kw, mv_aug], w=[bk3])
                    V(lambda e: e.scalar_tensor_tensor(out=C_f[h][:, 0:257], in0=C_f[h][:, 0:257], scalar=eG[:, h:h + 1], in1=bk3[:, 0:257],
                                                       op0=ALU.mult, op1=ALU.add), r=[C_f[h], eG, bk3], w=[C_f[h]])
                    G(lambda e: e.tensor_copy(out=C_b[h][:, 0:257], in_=C_f[h][:, 0:257]), r=[C_f[h]], w=[C_b[h]])
                    A(lambda e: e.activation(out=junk[:], in_=hm[:, h * 256:(h + 1) * 256], func=AF.Square, accum_out=ssq[:, h:h + 1]),
                      r=[hm], w=[junk, ssq])
                V(lambda e: e.tensor_scalar(out=ssq[:, 4:8], in0=ssq[:, 0:4], scalar1=1.0 / 256, scalar2=RMS_EPS, op0=ALU.mult, op1=ALU.add), r=[ssq], w=[ssq])
                G(lambda e: e.tensor_tensor(out=ssq[:, 8:12], in0=ssq[:, 4:8], in1=nhalf[:, 0:4], op=ALU.pow), r=[ssq, nhalf], w=[ssq])
                for h in range(4):
                    V(lambda e: e.scalar_tensor_tensor(out=hm_n[:, h * 256:(h + 1) * 256], in0=hm[:, h * 256:(h + 1) * 256],
                                                       scalar=ssq[:, 8 + h:9 + h], in1=mo_sig[:, h * 256:(h + 1) * 256],
                                                       op0=ALU.mult, op1=ALU.mult), r=[hm, ssq, mo_sig], w=[hm_n])
                dbg_out("d_h0", h0[:], h0, row0, D)
                dbg_out("d_hm", hm_n[:], hm_n, row0, D)
                if stage <= 1:
                    fw.dma('sp', out_d.t[row0:row0 + 128, :], hm_n[:], r=[hm_n], w=[out_d])
                    continue

                mark('gdn_conv', si == 0 and ti == 2)
                for sj in range(8):
                    sl = load_slab(w_in_b, 0, 8, 3080 + sj * 512, 512)
                    for m in range(4):
                        c = sj * 4 + m
                        zc = zcb[c % 6]; yc = ycb[c % 6]
                        bk = bank()
                        mm_fm(bk[:, 0:128], bk, xnT, sl, 8, m * 128)
                        G(lambda e: e.tensor_copy(out=zc[:, 0:3], in_=halo[:, c, :]), r=[halo], w=[zc])
                        A(lambda e: e.activation(out=zc[:, 3:131], in_=bk[:, 0:128], func=AF.Copy), r=[bk], w=[zc])
                        A(lambda e: e.activation(out=yc[:], in_=bk[:, 0:128], func=AF.Copy, scale=cw[:, 3, c:c + 1]), r=[bk, cw], w=[yc])
                        G(lambda e: e.tensor_copy(out=halo[:, c, :], in_=zc[:, 128:131]), r=[zc], w=[halo])
                    for m in range(4):
                        c = sj * 4 + m
                        zc = zcb[c % 6]; yc = ycb[c % 6]
                        for j in range(0, 3):
                            V(lambda e: e.scalar_tensor_tensor(out=yc[:], in0=zc[:, j:j + 128], scalar=cw[:, j, c:c + 1], in1=yc[:],
                                                               op0=ALU.mult, op1=ALU.add), r=[zc, cw, yc], w=[yc])
                    for m in range(4):
                        c = sj * 4 + m
                        yc = ycb[c % 6]; sc = scb[c % 6]
                        A(lambda e: e.activation(out=sc[:], in_=yc[:], func=AF.Silu), r=[yc], w=[sc])
                    for m in range(4):
                        c = sj * 4 + m
                        sc = scb[c % 6]; sq = sqb[c % 6]
                        if c < 16:
                            G(lambda e: e.tensor_tensor(out=sq[:], in0=sc[:], in1=sc[:], op=ALU.mult), r=[sc], w=[sq])
                            bk2 = bank()
                            P(lambda e: e.matmul(bk2[:, 0:128], lhsT=ones_f[:], rhs=sq[:], start=True, stop=True), r=[ones_f, sq], w=[bk2])
                            A(lambda e: e.activation(out=sq[:], in_=bk2[:, 0:128], func=AF.Sqrt, bias=eps_t[:, 1:2], scale=1.0), r=[bk2, eps_t], w=[sq])
                            V(lambda e: e.reciprocal(out=sq[:], in_=sq[:]), r=[sq], w=[sq])
                            if c < 8:
                                V(lambda e: e.scalar_tensor_tensor(out=gqT[:, c, :], in0=sc[:], scalar=128 ** -0.5, in1=sq[:],
                                                                   op0=ALU.mult, op1=ALU.mult), r=[sc, sq], w=[gqT])
                            else:
                                V(lambda e: e.tensor_tensor(out=sc[:], in0=sc[:], in1=sq[:], op=ALU.mult), r=[sc, sq], w=[sc])
                                G(lambda e: e.tensor_copy(out=gkT[:, c - 8, :], in_=sc[:]), r=[sc], w=[gkT])
                                bk3 = bank()
                                P(lambda e: e.transpose(bk3[:, 0:128], sc[:], ident[:]), r=[sc, ident], w=[bk3])
                                V(lambda e: e.tensor_copy(out=k_tm[:, c - 8, :], in_=bk3[:, 0:128]), r=[bk3], w=[k_tm])
                        else:
                            bk3 = bank()
                            P(lambda e: e.transpose(bk3[:, 0:128], sc[:], ident[:]), r=[sc, ident], w=[bk3])
                            A(lambda e: e.activation(out=v_tm[:, c - 16, :], in_=bk3[:, 0:128], func=AF.Copy), r=[bk3], w=[v_tm])
                mark('gdn_gz', si == 0 and ti == 2)
                for j in range(4):
                    sl = load_slab(w_in_b, 0, 8, 7176 + j * 512, 512)
                    bk = bank()
                    mm_tm(bk[:, 0:512], bk, xnT, sl, 8, 512)
                    A(lambda e: e.activation(out=gz_silu[:, j * 512:(j + 1) * 512], in_=bk[:, 0:512], func=AF.Silu), r=[bk], w=[gz_silu])
                mark('gdn_gates', si == 0 and ti == 2)
                A(lambda e: e.activation(out=gg[:, BETA:BETA + 16], in_=gsm[:, 24:40], func=AF.Sigmoid), r=[gsm], w=[gg])
                V(lambda e: e.tensor_scalar(out=gg[:, NBETA:NBETA + 16], in0=gg[:, BETA:BETA + 16], scalar1=-1.0, scalar2=None, op0=ALU.mult), r=[gg], w=[gg])
                V(lambda e: e.tensor_tensor(out=gg[:, TMPG:TMPG + 16], in0=gsm[:, 8:24], in1=dtb[:], op=ALU.add), r=[gsm, dtb], w=[gg])
                A(lambda e: e.activation(out=gg[:, TMPG:TMPG + 16], in_=gg[:, TMPG:TMPG + 16], func=AF.Exp), r=[gg], w=[gg])
                A(lambda e: e.activation(out=gg[:, TMPG:TMPG + 16], in_=gg[:, TMPG:TMPG + 16], func=AF.Ln, bias=eps_t[:, 3:4], scale=1.0), r=[gg, eps_t], w=[gg])
                V(lambda e: e.tensor_tensor(out=gg[:, NGD:NGD + 16], in0=gg[:, TMPG:TMPG + 16], in1=expA[:], op=ALU.mult), r=[gg, expA], w=[gg])
                bkg = bank()
                for qi, lm in enumerate([Ublk, BLK, sel0, sel1]):
                    P(lambda e: e.matmul(bkg[:, qi * 16:(qi + 1) * 16], lhsT=lm[:], rhs=gg[:, NGD:NGD + 16], start=True, stop=True), r=[lm, gg], w=[bkg])
                A(lambda e: e.activation(out=gg[:, EG:EG + 16], in_=bkg[:, 0:16], func=AF.Exp, scale=-1.0), r=[bkg], w=[gg])
                A(lambda e: e.activation(out=gg[:, TMPG:TMPG + 16], in_=bkg[:, 16:32], func=AF.Copy), r=[bkg], w=[gg])
                V(lambda e: e.tensor_tensor(out=gg[:, TMPG:TMPG + 16], in0=bkg[:, 0:16], in1=gg[:, TMPG:TMPG + 16], op=ALU.subtract), r=[bkg, gg], w=[gg])
                A(lambda e: e.activation(out=gg[:, EGR:EGR + 16], in_=gg[:, TMPG:TMPG + 16], func=AF.Exp), r=[gg], w=[gg])
                A(lambda e: e.activation(out=gg[:, ES:ES + 32], in_=bkg[:, 32:64], func=AF.Exp, scale=-1.0), r=[bkg], w=[gg])
                for half in range(2):
                  mark('gdn_inv%d' % half, si == 0 and ti == 2)
                  for g0 in range(half * 8, half * 8 + 8, GH):
                      for q2 in range(GH // 2):
                          hq = g0 // 2 + q2
                          bk = bank()
                          P(lambda e: e.matmul(bk[:, 0:128], lhsT=gkT[:, hq, :], rhs=gkT[:, hq, :], start=True, stop=True), r=[gkT], w=[bk])
                          V(lambda e: e.tensor_tensor(out=kkS[q2][:], in0=bk[:, 0:128], in1=maskS[:], op=ALU.mult), r=[bk, maskS], w=[kkS[q2]])
                          bk = bank()
                          P(lambda e: e.matmul(bk[:, 0:128], lhsT=gkT[:, hq, :], rhs=gqT[:, hq, :], start=True, stop=True), r=[gkT, gqT], w=[bk])
                          V(lambda e: e.tensor_tensor(out=qkS[q2][:], in0=bk[:, 0:128], in1=Ublk[:], op=ALU.mult), r=[bk, Ublk], w=[qkS[q2]])
                      for i in range(GH):
                          hv = g0 + i
                          G(lambda e: e.tensor_scalar(out=Ld[i][:], in0=Yblk[:], scalar1=gg[:, NGD + hv:NGD + hv + 1], scalar2=1.0,
                                                      op0=ALU.mult, op1=ALU.mult), r=[Yblk, gg], w=[Ld[i]])
                      for i in range(GH):
                          hv = g0 + i
                          bk = bank()
                          P(lambda e: e.matmul(bk[:, 0:128], lhsT=Ld[i][:], rhs=Ublk[:], start=True, stop=True), r=[Ld[i], Ublk], w=[bk])
                          A(lambda e: e.activation(out=decb[i][:], in_=bk[:, 0:128], func=AF.Exp, scale=-1.0), r=[bk], w=[decb[i]])
                          V(lambda e: e.scalar_tensor_tensor(out=Nb[i][0][:], in0=kkS[i // 2][:], scalar=gg[:, NBETA + hv:NBETA + hv + 1], in1=decb[i][:],
                                                             op0=ALU.mult, op1=ALU.mult), r=[kkS[i // 2], gg, decb[i]], w=[Nb[i][0]])
                          G(lambda e: e.tensor_tensor(out=attnT[hv % 8][:], in0=qkS[i // 2][:], in1=decb[i][:], op=ALU.mult), r=[qkS[i // 2], decb[i]], w=[attnT[hv % 8]])
                          G(lambda e: e.tensor_tensor(out=Rf[i][:], in0=Nb[i][0][:], in1=ident[:], op=ALU.add), r=[Nb[i][0], ident], w=[Rf[i]])
                      for i in range(GH):
                          bk = bank()
                          P(lambda e: e.transpose(bk[:, 0:128], Nb[i][0][:], ident[:]), r=[Nb[i][0], ident], w=[bk])
                          A(lambda e: e.activation(out=Mb[i][0][:], in_=bk[:, 0:128], func=AF.Copy), r=[bk], w=[Mb[i][0]])
                      cur = 0
                      for lv in range(5):
                          nxt = 1 - cur
                          for i in range(GH):
                              bk = bank()
                              P(lambda e: e.matmul(bk[:, 0:128], lhsT=Nb[i][cur][:], rhs=Mb[i][cur][:], start=True, stop=True), r=[Nb[i][cur], Mb[i][cur]], w=[bk])
                              A(lambda e: e.activation(out=Mb[i][nxt][:], in_=bk[:, 0:128], func=AF.Copy), r=[bk], w=[Mb[i][nxt]])
                          if lv < 4:
                              for i in range(GH):
                                  bk = bank()
                                  P(lambda e: e.matmul(bk[:, 0:128], lhsT=Mb[i][cur][:], rhs=Nb[i][cur][:], start=True, stop=True), r=[Nb[i][cur], Mb[i][cur]], w=[bk])
                                  V(lambda e: e.tensor_copy(out=Nb[i][nxt][:], in_=bk[:, 0:128]), r=[bk], w=[Nb[i][nxt]])
                          for i in range(GH):
                              bk = bank()
                              P(lambda e: e.matmul(bk[:, 0:128], lhsT=Mb[i][nxt][:], rhs=Rf[i][:], start=True, stop=True), r=[Mb[i][nxt], Rf[i]], w=[bk])
                              V(lambda e: e.tensor_tensor(out=Rf[i][:], in0=Rf[i][:], in1=bk[:, 0:128], op=ALU.add), r=[Rf[i], bk], w=[Rf[i]])
                          cur = nxt
                      for i in range(GH):
                          hv = g0 + i
                          hq = hv // 2
                          kg = kgb[i % 2]
                          G(lambda e: e.tensor_copy(out=Rb[hv % 8][:], in_=Rf[i][:]), r=[Rf[i]], w=[Rb[hv % 8]])
                          V(lambda e: e.tensor_scalar(out=kg[:], in0=k_tm[:, hq, :], scalar1=gg[:, EG + hv:EG + hv + 1], scalar2=None, op0=ALU.mult),
                            r=[k_tm, gg], w=[kg])
                          V(lambda e: e.tensor_scalar(out=kd[hv % 8][:], in0=k_tm[:, hq, :], scalar1=gg[:, EGR + hv:EGR + hv + 1], scalar2=None, op0=ALU.mult),
                            r=[k_tm, gg], w=[kd[hv % 8]])
                          bk = bank()
                          P(lambda e: e.matmul(bk[:, 0:128], lhsT=kg[:], rhs=Rb[hv % 8][:], start=True, stop=True), r=[kg, Rb[hv % 8]], w=[bk])
                          A(lambda e: e.activation(out=negWT[hv % 8][:], in_=bk[:, 0:128], func=AF.Copy, scale=-1.0), r=[bk], w=[negWT[hv % 8]])
                  mark('gdn_rec%d' % half, si == 0 and ti == 2)
                  for c2 in range(2):
                      hs = slice(c2 * 64, c2 * 64 + 64)
                      for hv in range(half * 8, half * 8 + 8):
                          bk = bank()
                          P(lambda e: e.matmul(bk[hs, 0:128], lhsT=Rb[hv % 8][hs, hs], rhs=v_tm[hs, hv, :], start=True, stop=False), r=[Rb[hv % 8], v_tm], w=[bk])
                          P(lambda e: e.matmul(bk[hs, 0:128], lhsT=negWT[hv % 8][:, hs], rhs=S_b[hv][:], start=False, stop=True), r=[negWT[hv % 8], S_b[hv]], w=[bk])
                          A(lambda e: e.activation(out=vnew[hv][hs, :], in_=bk[hs, 0:128], func=AF.Copy, scale=gg[hs, BETA + hv:BETA + hv + 1]),
                            r=[bk, gg], w=[vnew[hv]])
                      for hv in range(half * 8, half * 8 + 8):
                          hq = hv // 2
                          p2 = p2s[hv % 2]
                          bk = bank()
                          P(lambda e: e.matmul(bk[hs, 0:128], lhsT=gqT[:, hq, hs], rhs=S_b[hv][:], start=True, stop=True), r=[gqT, S_b[hv]], w=[bk])
                          P(lambda e: e.matmul(bk[hs, 128:256], lhsT=attnT[hv % 8][hs, hs], rhs=vnew[hv][hs, :], start=True, stop=True), r=[attnT[hv % 8], vnew[hv]], w=[bk])
                          A(lambda e: e.activation(out=p2[hs, :], in_=bk[hs, 128:256], func=AF.Copy), r=[bk], w=[p2])
                          V(lambda e: e.scalar_tensor_tensor(out=o_g[hs, hv * 128:(hv + 1) * 128], in0=bk[hs, 0:128], scalar=gg[hs, EG + hv:EG + hv + 1],
                                                             in1=p2[hs, :], op0=ALU.mult, op1=ALU.add), r=[bk, gg, p2], w=[o_g])
                          bk2 = bank()
                          P(lambda e: e.matmul(bk2[:, 0:128], lhsT=kd[hv % 8][hs, :], rhs=vnew[hv][hs, :], start=True, stop=True), r=[kd[hv % 8], vnew[hv]], w=[bk2])
                          V(lambda e: e.scalar_tensor_tensor(out=S_f[hv][:], in0=S_f[hv][:], scalar=gg[:, ES + c2 * 16 + hv:ES + c2 * 16 + hv + 1],
                                                             in1=bk2[:, 0:128], op0=ALU.mult, op1=ALU.add), r=[S_f[hv], gg, bk2], w=[S_f[hv]])
                          G(lambda e: e.tensor_copy(out=S_b[hv][:], in_=S_f[hv][:]), r=[S_f[hv]], w=[S_b[hv]])
                mark('gdn_norm', si == 0 and ti == 2)
                for hv in range(16):
                    A(lambda e: e.activation(out=junk[:, 0:128], in_=o_g[:, hv * 128:(hv + 1) * 128], func=AF.Square, accum_out=ssq[:, hv:hv + 1]),
                      r=[o_g], w=[junk, ssq])
                V(lambda e: e.tensor_scalar(out=ssq[:, 0:16], in0=ssq[:, 0:16], scalar1=1.0 / 128, scalar2=RMS_EPS, op0=ALU.mult, op1=ALU.add), r=[ssq], w=[ssq])
                G(lambda e: e.tensor_tensor(out=ssq[:, 0:16], in0=ssq[:, 0:16], in1=nhalf[:, 0:16], op=ALU.pow), r=[ssq, nhalf], w=[ssq])
                for hv in range(16):
                    V(lambda e: e.scalar_tensor_tensor(out=o_g[:, hv * 128:(hv + 1) * 128], in0=o_g[:, hv * 128:(hv + 1) * 128],
                                                       scalar=ssq[:, hv:hv + 1], in1=gz_silu[:, hv * 128:(hv + 1) * 128],
                                                       op0=ALU.mult, op1=ALU.mult), r=[o_g, ssq, gz_silu], w=[o_g])
                dbg_out("d_o", o_n[:], o_n, row0, 2048)
                dbg_out("d_kg", k_tm[:].rearrange("p h d -> p (h d)"), k_tm, row0, 1024)
                if stage <= 2:
                    fw.dma('sp', out_d.t[row0:row0 + 128, :], o_n[:, 0:1024], r=[o_n], w=[out_d])
                    continue

                mark('branch_mix', si == 0 and ti == 2)
                transpose_fm(hm_n, 1024, dst_bf=hmT)
                transpose_fm(o_n, 2048, dst_bf=oT)
                for j in range(2):
                    gs = gsig[0]
                    sl = load_slab(w_in_b, 0, 8, 9256 + j * 512, 512)
                    bk = bank()
                    mm_tm(bk[:, 0:512], bk, xnT, sl, 8, 512)
                    A(lambda e: e.activation(out=gs[:], in_=bk[:, 0:512], func=AF.Sigmoid), r=[bk], w=[gs])
                    sl = load_slab(w_br_ml_b, 0, 8, j * 512, 512)
                    bk = bank()
                    mm_tm(bk[:, 0:512], bk, hmT, sl, 8, 512)
                    V(lambda e: e.tensor_tensor(out=merged[:, j * 512:(j + 1) * 512], in0=bk[:, 0:512], in1=gs[:], op=ALU.mult), r=[bk, gs], w=[merged])
                    gs = gsig[1]
                    sl = load_slab(w_in_b, 0, 8, 10280 + j * 512, 512)
                    bk = bank()
                    mm_tm(bk[:, 0:512], bk, xnT, sl, 8, 512)
                    A(lambda e: e.activation(out=gs[:], in_=bk[:, 0:512], func=AF.Sigmoid), r=[bk], w=[gs])
                    bk = bank()
                    for kh in range(2):
                        sl = load_slab(w_br_gdn_b, kh * 8, 8, j * 512, 512)
                        for k in range(8):
                            P(lambda e: e.matmul(bk[:, 0:512], lhsT=oT[:, kh * 8 + k, :], rhs=sl[:, k, 0:512], start=(kh == 0 and k == 0), stop=(kh == 1 and k == 7)),
                              r=[oT, sl], w=[bk])
                    V(lambda e: e.tensor_tensor(out=gs[:], in0=bk[:, 0:512], in1=gs[:], op=ALU.mult), r=[bk, gs], w=[gs])
                    V(lambda e: e.tensor_tensor(out=merged[:, j * 512:(j + 1) * 512], in0=merged[:, j * 512:(j + 1) * 512], in1=gs[:], op=ALU.add), r=[merged, gs], w=[merged])
                transpose_fm(merged, 1024, dst_bf=hmT)
                for j in range(2):
                    sl = load_slab(w_mix_b, 0, 8, j * 512, 512)
                    bk = bank()
                    mm_tm(bk[:, 0:512], bk, hmT, sl, 8, 512)
                    V(lambda e: e.scalar_tensor_tensor(out=h1[:, j * 512:(j + 1) * 512], in0=h0[:, j * 512:(j + 1) * 512], scalar=DN_ALPHA, in1=bk[:, 0:512],
                                                       op0=ALU.mult, op1=ALU.add), r=[h0, bk], w=[h1])
                layernorm(h1, h1, "ln1_g", "ln1_b")
                dbg_out("d_h1", h1[:], h1, row0, D)
                if stage <= 3:
                    fw.dma('sp', out_d.t[row0:row0 + 128, :], h1[:], r=[h1], w=[out_d])
                    continue
                mark('xa', si == 0 and ti == 2)
                import os
                XC = 0
                if XC == 1:
                    fw.dma('sp', out_d.t[row0:row0 + 128, :], h1[:], r=[h1], w=[out_d])
                    continue
                transpose_fm(h1, 1024, dst_bf=xnT)
                for j in range(2):
                    sl = load_slab(xa_wq_b, 0, 8, j * 512, 512)
                    for m in range(4):
                        bk = bank()
                        mm_fm(bk[:, 0:128], bk, xnT, sl, 8, m * 128)
                        A(lambda e: e.activation(out=gqT[:, j * 4 + m, :], in_=bk[:, 0:128], func=AF.Copy, scale=1.0 / 16), r=[bk], w=[gqT])
                if XC == 2:
                    fw.dma('sp', out_d.t[row0:row0 + 128, :], h1[:], r=[h1], w=[out_d])
                    continue
                bkS = [bank(), bank()]
                for hh in range(4):
                    bk = bkS[hh // 2]
                    for c in range(2):
                        P(lambda e: e.matmul(bk[:, (hh % 2) * 256:(hh % 2 + 1) * 256], lhsT=gqT[:, 2 * hh + c, :], rhs=KT[:, 2 * hh + c, :], start=(c == 0), stop=(c == 1)),
                          r=[gqT, KT], w=[bk])
                for i2 in range(2):
                    V(lambda e: e.tensor_reduce(out=smx[:, 2 * i2:2 * i2 + 2], in_=bkS[i2][:, 0:512].rearrange("p (h m) -> p h m", m=256), axis=AX.X, op=ALU.max),
                      r=[bkS[i2]], w=[smx])
                V(lambda e: e.tensor_scalar(out=smx[:, 4:8], in0=smx[:, 0:4], scalar1=-1.0, scalar2=None, op0=ALU.mult), r=[smx], w=[smx])
                for hh in range(4):
                    A(lambda e: e.activation(out=sc_all[:, hh, :], in_=bkS[hh // 2][:, (hh % 2) * 256:(hh % 2 + 1) * 256], func=AF.Exp, bias=smx[:, 4 + hh:5 + hh], scale=1.0,
                                             accum_out=smx[:, 8 + hh:9 + hh]), r=[bkS[hh // 2], smx], w=[sc_all, smx])
                V(lambda e: e.reciprocal(out=smx[:, 12:16], in_=smx[:, 8:12]), r=[smx], w=[smx])
                for hh in range(4):
                    V(lambda e: e.tensor_scalar(out=sc_all[:, hh, :], in0=sc_all[:, hh, :], scalar1=smx[:, 12 + hh:13 + hh], scalar2=None, op0=ALU.mult), r=[sc_all, smx], w=[sc_all])
                for i2 in range(2):
                    bk = bank()
                    for q in range(4):
                        hh = 2 * i2 + q // 2
                        mt = q % 2
                        P(lambda e: e.transpose(bk[:, q * 128:(q + 1) * 128], sc_all[:, hh, mt * 128:(mt + 1) * 128], ident[:]), r=[sc_all, ident], w=[bk])
                    A(lambda e: e.activation(out=pT_all[:, 4 * i2:4 * i2 + 4, :], in_=bk[:, 0:512].rearrange("p (c t) -> p c t", t=128), func=AF.Copy), r=[bk], w=[pT_all])
                for i2 in range(2):
                    bk = bank()
                    for q in range(4):
                        hh = 2 * i2 + q // 2
                        c = q % 2
                        for mt in range(2):
                            P(lambda e: e.matmul(bk[:, q * 128:(q + 1) * 128], lhsT=Vx[:, mt, (2 * hh + c) * 128:(2 * hh + c + 1) * 128], rhs=pT_all[:, 2 * hh + mt, :],
                                                 start=(mt == 0), stop=(mt == 1)), r=[Vx, pT_all], w=[bk])
                    A(lambda e: e.activation(out=gkT[:, 4 * i2:4 * i2 + 4, :], in_=bk[:, 0:512].rearrange("p (c t) -> p c t", t=128), func=AF.Copy), r=[bk], w=[gkT])
                if XC in (3, 4, 5):
                    fw.dma('sp', out_d.t[row0:row0 + 128, :], h1[:], r=[h1], w=[out_d])
                    continue
                for j in range(2):
                    sl = load_slab(xa_wo_b, 0, 8, j * 512, 512)
                    bk = bank()
                    mm_tm(bk[:, 0:512], bk, gkT, sl, 8, 512)
                    V(lambda e: e.scalar_tensor_tensor(out=h2[:, j * 512:(j + 1) * 512], in0=h1[:, j * 512:(j + 1) * 512], scalar=DN_ALPHA, in1=bk[:, 0:512],
                                                       op0=ALU.mult, op1=ALU.add), r=[h1, bk], w=[h2])
                layernorm(h2, h2, "ln2_g", "ln2_b")
                dbg_out("d_h2", h2[:], h2, row0, D)
                if stage <= 4:
                    fw.dma('sp', out_d.t[row0:row0 + 128, :], h2[:], r=[h2], w=[out_d])
                    continue
                mark('router', si == 0 and ti == 2)
                fw.dma('sp', h2_d.t[row0:row0 + 128, :], h2[:], r=[h2], w=[h2_d])
                transpose_fm(h2, 1024, dst_f=xnT_f)
                bk = bank()
                for k in range(8):
                    P(lambda e: e.matmul(bk[:, 0:NE], lhsT=xnT_f[:, k, :], rhs=w_rt[:, k, :], start=(k == 0), stop=(k == 7)), r=[xnT_f, w_rt], w=[bk])
                V(lambda e: e.tensor_tensor(out=rt[:, 0, :], in0=bk[:, 0:NE], in1=b_rt[:], op=ALU.add), r=[bk, b_rt], w=[rt])
                V(lambda e: e.max(out=top8[:, 0:8], in_=rt[:, 0, :]), r=[rt], w=[top8])
                V(lambda e: e.tensor_scalar(out=rt[:, 1, :], in0=rt[:, 0, :], scalar1=top8[:, 3:4], scalar2=None, op0=ALU.is_ge), r=[rt, top8], w=[rt])
                V(lambda e: e.tensor_scalar(out=top8[:, 8:9], in0=top8[:, 0:1], scalar1=-1.0, scalar2=None, op0=ALU.mult), r=[top8], w=[top8])
                A(lambda e: e.activation(out=rt[:, 2, :], in_=rt[:, 0, :], func=AF.Exp, bias=top8[:, 8:9], scale=1.0), r=[rt, top8], w=[rt])
                V(lambda e: e.tensor_tensor(out=rt[:, 2, :], in0=rt[:, 2, :], in1=rt[:, 1, :], op=ALU.mult), r=[rt], w=[rt])
                V(lambda e: e.tensor_reduce(out=top8[:, 9:10], in_=rt[:, 2, :], axis=AX.X, op=ALU.add), r=[rt], w=[top8])
                V(lambda e: e.reciprocal(out=top8[:, 10:11], in_=top8[:, 9:10]), r=[top8], w=[top8])
                V(lambda e: e.tensor_scalar(out=rt[:, 3, :], in0=rt[:, 2, :], scalar1=top8[:, 10:11], scalar2=None, op0=ALU.mult), r=[rt, top8], w=[rt])
                tg_i = row0 // 128
                bk = bank()
                P(lambda e: e.matmul(bk[:, 0:NE], lhsT=UTs[:], rhs=rt[:, 1, :], start=True, stop=True), r=[UTs, rt], w=[bk])
                P(lambda e: e.matmul(bk[:, NE:2 * NE], lhsT=ones_f[:], rhs=rt[:, 1, :], start=True, stop=True), r=[ones_f, rt], w=[bk])
                V(lambda e: e.tensor_tensor(out=rt[:, 4, :], in0=bk[:, 0:NE], in1=base_c[:], op=ALU.add), r=[bk, base_c], w=[rt])
                V(lambda e: e.tensor_tensor(out=base_c[:], in0=bk[:, NE:2 * NE], in1=base_c[:], op=ALU.add), r=[bk, base_c], w=[base_c])
                V(lambda e: e.tensor_scalar(out=rt[:, 5, :], in0=rt[:, 4, :], scalar1=CAP - 0.5, scalar2=None, op0=ALU.is_lt), r=[rt], w=[rt])
                V(lambda e: e.tensor_tensor(out=rt[:, 5, :], in0=rt[:, 5, :], in1=rt[:, 1, :], op=ALU.mult), r=[rt], w=[rt])
                V(lambda e: e.tensor_tensor(out=rt[:, 4, :], in0=rt[:, 4, :], in1=eoff[:], op=ALU.add), r=[rt, eoff], w=[rt])
                V(lambda e: e.tensor_scalar(out=rt[:, 4, :], in0=rt[:, 4, :], scalar1=-1.0, scalar2=CBIG + 1.0, op0=ALU.mult, op1=ALU.add), r=[rt], w=[rt])
                V(lambda e: e.tensor_tensor(out=rt[:, 6, :], in0=rt[:, 4, :], in1=rt[:, 5, :], op=ALU.mult), r=[rt], w=[rt])
                V(lambda e: e.max(out=top8[:, 12:20], in_=rt[:, 6, :]), r=[rt], w=[top8])
                V(lambda e: e.tensor_scalar(out=top8[:, 20:24], in0=top8[:, 12:16], scalar1=-1.0, scalar2=CBIG + 1.0, op0=ALU.mult, op1=ALU.add), r=[top8], w=[top8])
                V(lambda e: e.tensor_copy(out=slot_i[:, tg_i, :], in_=top8[:, 20:24]), r=[top8], w=[slot_i])
                for k4 in range(4):
                    V(lambda e: e.scalar_tensor_tensor(out=rt[:, 7, :], in0=rt[:, 6, :], scalar=top8[:, 12 + k4:13 + k4], in1=rt[:, 3, :],
                                                       op0=ALU.is_equal, op1=ALU.mult), r=[rt, top8], w=[rt])
                    V(lambda e: e.tensor_reduce(out=gwk[:, tg_i, k4:k4 + 1], in_=rt[:, 7, :], axis=AX.X, op=ALU.add), r=[rt], w=[gwk])
                for k4 in range(4):
                    fw.dma('pool', None, None, r=[h2, slot_i], w=[xg_d],
                           fn=lambda e: e.indirect_dma_start(out=xg_d.t, out_offset=bass.IndirectOffsetOnAxis(ap=slot_i[:, tg_i, k4:k4 + 1], axis=0),
                                                             in_=h2[:], in_offset=None, bounds_check=bc_reg, oob_is_err=False))

        mark(None)
        if stage > 4:
            if "d_cnt" in dbg_d:
                fw.dma('sp', dbg_d["d_cnt"].t[0:128, 0:NE], base_c[:], r=[base_c], w=[dbg_d["d_cnt"]])
            fw.barrier()
            es1.close()
            fw.es = es
            for n in vec_late:
                vec_t[n] = fw.sb("c_" + n, [128, D], F32)
                fw.dma('sp', vec_t[n][:], vec_d[n].t.partition_broadcast(128), r=[vec_d[n]], w=[vec_t[n]])
            es2 = es.enter_context(ExitStack())
            fw.es = es2
            wgu = [fw.sb(f"wgu{i}", [128, 8, 2048], BF16) for i in range(2)]
            wdn = [fw.sb(f"wdn{i}", [128, 8, 1024], BF16) for i in range(2)]
            bgu_tm = [fw.sb(f"bgu_tm{i}", [16, 128], F32) for i in range(2)]
            bgu = [fw.sb(f"bgu{i}", [128, 16], F32) for i in range(2)]
            bdn_bc = [fw.sb(f"bdn_bc{i}", [128, D], F32) for i in range(2)]
            NXR = max(2, CAP // 128)
            xr = [fw.sb(f"xr{i}", [128, D], F32) for i in range(NXR)]
            xgT = fw.sb("xgT", [128, 8, CAP], BF16)
            actT = fw.sb("actT", [128, 8, CAP], BF16)
            tgb = [fw.sb(f"tg{i}", [128, 512], F32) for i in range(2)]
            tsb = [fw.sb(f"ts{i}", [128, 512], F32) for i in range(2)]
            tub = [fw.sb(f"tu{i}", [128, 512], F32) for i in range(2)]
            ysb = [fw.sb(f"ysb{i}", [128, D], F32) for i in range(2)]
            nchunks = [(n0, min(512, CAP - n0)) for n0 in range(0, CAP, 512)]

            def load_expert(ex):
                st = ex % 2
                wg_v = w_gu_d.t[ex].rearrange("(kc p) n -> p kc n", p=128)
                wd_v = w_dn_d.t[ex].rearrange("(kc p) n -> p kc n", p=128)
                for j in range(4):
                    fw.dma('pool', wgu[st][:, :, j * 512:(j + 1) * 512], wg_v[:, :, j * 512:(j + 1) * 512], r=[w_gu_d], w=[wgu[st]])
                fw.dma('sp', bgu_tm[st][:], b_gu_d.t[ex].rearrange("(c p) -> c p", p=128), r=[b_gu_d], w=[bgu_tm[st]])
                fw.dma('sp', bdn_bc[st][:], b_dn_d.t[ex:ex + 1, :].partition_broadcast(128), r=[b_dn_d], w=[bdn_bc[st]])

            wst = [fw.sb(f"wst{i}", [128, 4, 512], F32) for i in range(2)]

            def wdn_piece(ex, j):
                kh, ch = j // 2, j % 2
                return (w_dn_d.t[ex].rearrange("(kc p) n -> p kc n", p=128)[:, kh * 4:(kh + 1) * 4, ch * 512:(ch + 1) * 512],
                        wdn[ex % 2][:, kh * 4:(kh + 1) * 4, ch * 512:(ch + 1) * 512])

            def wdn_load(ex, j):
                src, _ = wdn_piece(ex, j)
                fw.dma('sp', wst[j % 2][:], src, r=[w_dn_d], w=[wst[j % 2]])

            def wdn_cast(ex, j):
                _, dst = wdn_piece(ex, j)
                A(lambda e: e.activation(out=dst, in_=wst[j % 2][:], func=AF.Copy), r=[wst[j % 2]], w=[wdn[ex % 2]])

            def load_x(ex):
                for i in range(CAP // 128):
                    xx = xr[i % NXR]
                    fw.dma('sp', xx[:], xg_d.t[ex * CAP + i * 128: ex * CAP + (i + 1) * 128, :], r=[xg_d], w=[xx])

            load_expert(0)
            load_x(0)
            for j in range(4):
                wdn_load(0, j)
                wdn_cast(0, j)
            for ex in range(NE):
                st = ex % 2
                if ex + 1 < NE:
                    load_expert(ex + 1)
                bk = bank()
                P(lambda e: e.transpose(bk[:, 0:16], bgu_tm[st][0:16, :], ident[0:16, 0:16]), r=[bgu_tm[st], ident], w=[bk])
                V(lambda e: e.tensor_copy(out=bgu[st][:], in_=bk[:, 0:16]), r=[bk], w=[bgu[st]])
                for i in range(CAP // 128):
                    xx = xr[i % NXR]
                    for c0 in range(0, 8, 4):
                        bk = bank()
                        for c in range(4):
                            P(lambda e: e.transpose(bk[:, c * 128:(c + 1) * 128], xx[:, (c0 + c) * 128:(c0 + c + 1) * 128], ident[:]), r=[xx, ident], w=[bk])
                        pv = bk[:, 0:512].rearrange("p (c t) -> p c t", t=128)
                        if c0 == 0:
                            A(lambda e: e.activation(out=xgT[:, c0:c0 + 4, i * 128:(i + 1) * 128], in_=pv, func=AF.Copy), r=[bk], w=[xgT])
                        else:
                            V(lambda e: e.tensor_copy(out=xgT[:, c0:c0 + 4, i * 128:(i + 1) * 128], in_=pv), r=[bk], w=[xgT])
                if ex + 1 < NE:
                    load_x(ex + 1)
                    wdn_load(ex + 1, 0)
                    wdn_load(ex + 1, 1)
                for f in range(8):
                    if ex + 1 < NE and f % 2 == 1:
                        jj = f // 2
                        wdn_cast(ex + 1, jj)
                        if jj + 2 < 4:
                            wdn_load(ex + 1, jj + 2)
                    for ci, (n0, nn) in enumerate(nchunks):
                        tg = tgb[ci % 2]; ts = tsb[ci % 2]; tu = tub[ci % 2]
                        bkg = bank()
                        for k in range(8):
                            P(lambda e: e.matmul(bkg[:, 0:nn], lhsT=wgu[st][:, k, f * 128:(f + 1) * 128], rhs=xgT[:, k, n0:n0 + nn], start=(k == 0), stop=(k == 7)),
                              r=[wgu[st], xgT], w=[bkg])
                        bku = bank()
                        for k in range(8):
                            P(lambda e: e.matmul(bku[:, 0:nn], lhsT=wgu[st][:, k, 1024 + f * 128:1024 + (f + 1) * 128], rhs=xgT[:, k, n0:n0 + nn], start=(k == 0), stop=(k == 7)),
                              r=[wgu[st], xgT], w=[bku])
                        V(lambda e: e.tensor_scalar(out=tg[:, 0:nn], in0=bkg[:, 0:nn], scalar1=bgu[st][:, f:f + 1], scalar2=7.0, op0=ALU.add, op1=ALU.min), r=[bkg, bgu[st]], w=[tg])
                        A(lambda e: e.activation(out=ts[:, 0:nn], in_=tg[:, 0:nn], func=AF.Sigmoid, scale=1.702), r=[tg], w=[ts])
                        G(lambda e: e.tensor_tensor(out=tg[:, 0:nn], in0=tg[:, 0:nn], in1=ts[:, 0:nn], op=ALU.mult), r=[tg, ts], w=[tg])
                        V(lambda e: e.tensor_scalar(out=tu[:, 0:nn], in0=bku[:, 0:nn], scalar1=bgu[st][:, 8 + f:9 + f], scalar2=7.0, op0=ALU.add, op1=ALU.min), r=[bku, bgu[st]], w=[tu])
                        V(lambda e: e.tensor_scalar(out=tu[:, 0:nn], in0=tu[:, 0:nn], scalar1=-7.0, scalar2=1.0, op0=ALU.max, op1=ALU.add), r=[tu], w=[tu])
                        G(lambda e: e.tensor_tensor(out=actT[:, f, n0:n0 + nn], in0=tu[:, 0:nn], in1=tg[:, 0:nn], op=ALU.mult), r=[tu, tg], w=[actT])
                for i in range(CAP // 128):
                    ys = ysb[i % 2]
                    for half in range(2):
                        bk = bank()
                        for k in range(8):
                            P(lambda e: e.matmul(bk[:, 0:512], lhsT=actT[:, k, i * 128:(i + 1) * 128], rhs=wdn[st][:, k, half * 512:(half + 1) * 512], start=(k == 0), stop=(k == 7)),
                              r=[actT, wdn[st]], w=[bk])
                        V(lambda e: e.tensor_tensor(out=ys[:, half * 512:(half + 1) * 512], in0=bk[:, 0:512], in1=bdn_bc[st][:, half * 512:(half + 1) * 512], op=ALU.add),
                          r=[bk, bdn_bc[st]], w=[ys])
                    fw.dma('sp', yg_d.t[ex * CAP + i * 128: ex * CAP + (i + 1) * 128, :], ys[:], r=[ys], w=[yg_d])
            fw.barrier()
            es2.close()
            fw.es = es
            fh = [fw.sb(f"fh{i}", [128, D], F32) for i in range(2)]
            gb = [[fw.sb(f"gb{i}_{k}", [128, D], F32) for k in range(4)] for i in range(2)]
            for t in range(T // 128):
                hh2 = fh[t % 2]
                fw.dma('sp', hh2[:], h2_d.t[t * 128:(t + 1) * 128, :], r=[h2_d], w=[hh2])
                for k4 in range(4):
                    gk = gb[t % 2][k4]
                    V(lambda e: e.memset(gk[:], 0.0), w=[gk])
                    fw.dma('pool', None, None, r=[yg_d, slot_i], w=[gk],
                           fn=lambda e: e.indirect_dma_start(out=gk[:], out_offset=None, in_=yg_d.t,
                                                             in_offset=bass.IndirectOffsetOnAxis(ap=slot_i[:, t, k4:k4 + 1], axis=0),
                                                             bounds_check=bc_reg, oob_is_err=False))
                V(lambda e: e.tensor_scalar(out=hh2[:], in0=hh2[:], scalar1=DN_ALPHA, scalar2=None, op0=ALU.mult), r=[hh2], w=[hh2])
                for k4 in range(4):
                    gk = gb[t % 2][k4]
                    V(lambda e: e.scalar_tensor_tensor(out=hh2[:], in0=gk[:], scalar=gwk[:, t, k4:k4 + 1], in1=hh2[:], op0=ALU.mult, op1=ALU.add),
                      r=[gk, gwk, hh2], w=[hh2])
                layernorm(hh2, hh2, "ln3_g", "ln3_b")
                fw.dma('sp', out_d.t[t * 128:(t + 1) * 128, :], hh2[:], r=[hh2], w=[out_d])

        fw.finish([out_d] + list(dbg_d.values()))
        es1.close()
        print("instructions", fw.n_inst, "waits", fw.n_wait, "sems", fw.nsem)
    return nc


_IN_NAMES = ["x", "mem", "ln_in_g", "ln_in_b", "w_in", "ml_gate_bias", "ml_norm_g", "gdn_conv_w", "gdn_a_log",
             "gdn_dt_bias", "gdn_norm_g", "w_branch_ml", "w_branch_gdn", "w_mix_out", "ln1_g", "ln1_b",
             "xa_wq", "xa_wk", "xa_wv", "xa_wo", "ln2_g", "ln2_b", "w_router", "b_router", "w_gu", "b_gu",
             "w_dn", "b_dn", "ln3_g", "ln3_b"]


def make_in_map(inputs, b0, n_seq, S):
    f = lambda a: np.ascontiguousarray(np.asarray(a, dtype=np.float32))
    m = {}
    m["x"] = f(inputs["x"][b0:b0 + n_seq, :S]).reshape(n_seq * S, D)
    m["mem"] = f(inputs["mem"][b0:b0 + n_seq]).reshape(n_seq * MEM, D)
    for n in ["ln_in_g", "ln_in_b"]:
        m[n] = f(inputs[n]).reshape(1, D)
    for n in ["ml_norm_g", "ln1_g", "ln1_b", "ln2_g", "ln2_b", "ln3_g", "ln3_b"]:
        m[n] = f(inputs[n]).reshape(1, D)
    m["w_in"] = f(inputs["w_in"]).reshape(D, IN_W)
    m["ml_gate_bias"] = f(inputs["ml_gate_bias"]).reshape(1, 8)
    m["gdn_conv_w"] = f(inputs["gdn_conv_w"]).reshape(4, 4096)
    m["gdn_a_log"] = f(inputs["gdn_a_log"]).reshape(1, 16)
    m["gdn_dt_bias"] = f(inputs["gdn_dt_bias"]).reshape(1, 16)
    m["gdn_norm_g"] = f(inputs["gdn_norm_g"]).reshape(1, 128)
    m["w_branch_ml"] = f(inputs["w_branch_ml"]).reshape(D, D)
    m["w_branch_gdn"] = f(inputs["w_branch_gdn"]).reshape(2048, D)
    for n, k in [("w_mix_out", "w_mix_out"), ("xa_wq", "xa_wq"), ("xa_wk", "xa_wk"), ("xa_wv", "xa_wv"), ("xa_wo", "xa_wo")]:
        m[n] = f(inputs[k]).reshape(D, D)
    m["w_router"] = f(inputs["w_router"]).reshape(D, NE)
    m["b_router"] = f(inputs["b_router"]).reshape(1, NE)
    m["w_gu"] = f(inputs["w_gu"]).reshape(NE, D, 2 * D)
    m["b_gu"] = f(inputs["b_gu"]).reshape(NE, 2 * D)
    m["w_dn"] = f(inputs["w_dn"]).reshape(NE, D, D)
    m["b_dn"] = f(inputs["b_dn"]).reshape(NE, D)
    return m


def kernel(**inputs):
    n_cores = 8
    n_seq, S = 2, 2048
    nc = build_nc(n_seq, S)
    in_maps = [make_in_map(inputs, c * n_seq, n_seq, S) for c in range(n_cores)]
    res = run_bass_kernel_spmd(nc, in_maps, core_ids=list(range(n_cores)))
    outs = [np.asarray(r["out"], dtype=np.float32).reshape(n_seq, S, D) for r in res.results]
    return np.concatenate(outs, axis=0)
```

```python
import math
import numpy as np
from contextlib import ExitStack
import concourse.bass as bass
import concourse.mybir as mybir
from concourse.bass_utils import run_bass_kernel_spmd

F32 = mybir.dt.float32
BF16 = mybir.dt.bfloat16
I32 = mybir.dt.int32
AF = mybir.ActivationFunctionType
ALU = mybir.AluOpType
AX = mybir.AxisListType

D = 1024
IN_W = 11304
LN_EPS = 1e-5
RMS_EPS = 1e-6
DN_ALPHA = 2 ** 0.25
MEM = 256
NE = 32


class Buf:
    def __init__(self, t, name, parent=None):
        self.t = t
        self.name = name
        self._p = parent
        self._w = None
        self._r = []
        self.excl = False
        self.small = False

    @property
    def w(self):
        return self._p.w if self._p is not None else self._w

    @w.setter
    def w(self, v):
        if self._p is not None:
            self._p.w = v
        else:
            self._w = v

    @property
    def r(self):
        return self._p.r if self._p is not None else self._r

    @r.setter
    def r(self, v):
        if self._p is not None:
            self._p.r = v
        else:
            self._r = v

    def __getitem__(self, k):
        return self.t[k]


class FW:
    SEM_LIMIT = 30000

    def __init__(self, nc, es):
        self.nc = nc
        self.es = es
        self.es_sem = es
        self.eng = {'pe': nc.tensor, 'act': nc.scalar, 'dve': nc.vector, 'pool': nc.gpsimd, 'sp': nc.sync}
        self.esem = {}
        self.ecnt = {}
        self.known = {k: {} for k in self.eng}
        self.nsem = 0
        self.sem_owner = {}
        import os as _o
        self.force_small = False
        import os
        for k in self.eng:
            self._new_esem(k)
        self.dma_pool = [[self._sem(), 0] for _ in range(20)]
        self.dma_rr = 0
        self.n_inst = 0
        self.n_wait = 0

    def _sem(self):
        self.nsem += 1
        return self.es_sem.enter_context(self.nc.semaphore(f"sm{self.nsem}"))

    def _new_esem(self, k):
        self.esem[k] = self._sem()
        self.ecnt[k] = 0
        self.sem_owner[id(self.esem[k])] = k

    def sb(self, name, shape, dt, es=None):
        b = Buf((es or self.es).enter_context(self.nc.sbuf_tensor(name, shape, dt)), name)
        fs = 1
        for d in shape[1:]:
            fs *= d
        b.small = fs <= 256
        return b

    def ps(self, name, shape, dt, es=None):
        b = Buf((es or self.es).enter_context(self.nc.psum_tensor(name, shape, dt)), name)
        b.excl = True
        return b

    def _wait(self, ek, tok, small=True):
        if tok is None:
            return
        sem, val = tok
        key = id(sem)
        if self.sem_owner.get(key) == ek and (ek == 'pe' or (not small and not self.force_small)):
            return
        kn = self.known[ek]
        if kn.get(key, 0) >= val:
            return
        kn[key] = val
        self.eng[ek].wait_ge(sem, val)
        self.n_wait += 1

    def _deps(self, ek, r, w):
        for b in r:
            sm = b.small or (b._p is not None and b._p.small)
            self._wait(ek, b.w, sm)
            if b.excl:
                for t in b.r:
                    self._wait(ek, t, sm)
        for b in w:
            sm = b.small or (b._p is not None and b._p.small)
            self._wait(ek, b.w, sm)
            for t in b.r:
                self._wait(ek, t, sm)

    def _commit(self, tok, r, w):
        for b in r:
            if len(b.r) > 12:
                b.r = b.r[-12:] if False else b.r
            b.r.append(tok)
        for b in w:
            b.w = tok
            b.r = []

    def op(self, ek, fn, r=(), w=()):
        self._deps(ek, r, w)
        if self.ecnt[ek] >= self.SEM_LIMIT:
            self._new_esem(ek)
        ins = fn(self.eng[ek])
        self.ecnt[ek] += 1
        tok = (self.esem[ek], self.ecnt[ek])
        ins.then_inc(self.esem[ek], 1)
        self._commit(tok, r, w)
        self.n_inst += 1
        return ins

    def dma(self, ek, out, in_, r=(), w=(), fn=None, **kw):
        self._deps(ek, r, w)
        slot = self.dma_pool[self.dma_rr]
        self.dma_rr = (self.dma_rr + 1) % len(self.dma_pool)
        sem, cnt = slot
        if cnt > 0:
            self._wait(ek, (sem, cnt))
        if fn is not None:
            ins = fn(self.eng[ek])
        else:
            ins = self.eng[ek].dma_start(out=out, in_=in_, **kw)
        cnt += 16
        slot[1] = cnt
        ins.then_inc(sem, 16)
        tok = (sem, cnt)
        self._commit(tok, r, w)
        self.n_inst += 1
        return tok

    def barrier(self):
        toks = [(self.esem[k], self.ecnt[k]) for k in self.eng if self.ecnt[k] > 0]
        toks += [(sem, cnt) for sem, cnt in self.dma_pool if cnt > 0]
        for ek in self.eng:
            for t in toks:
                self._wait(ek, t)

    def finish(self, bufs):
        for b in bufs:
            self._wait('sp', b.w)


def build_nc(n_seq=2, S=2048, stage=99, dbg=()):
    nc = bass.Bass("TRN2", target_bir_lowering=False)
    T = n_seq * S
    NSUB = S // 128
    lnscale = math.log(128 ** -0.5)

    def din(name, shape, dt=F32):
        return Buf(nc.dram_tensor(name, list(shape), dt, kind="ExternalInput").ap(), name)

    def dscr(name, shape, dt):
        return Buf(nc.dram_tensor(name, list(shape), dt, kind="Internal").ap(), name)

    x_d = din("x", [T, D])
    mem_d = din("mem", [n_seq * MEM, D])
    vec_names = ["ln_in_g", "ln_in_b", "ln1_g", "ln1_b", "ln2_g", "ln2_b"]
    vec_late = ["ln3_g", "ln3_b"]
    vec_d = {n: din(n, [1, D]) for n in vec_names + vec_late}
    w_in_d = din("w_in", [D, IN_W])
    ml_gate_bias_d = din("ml_gate_bias", [1, 8])
    conv_w_d = din("gdn_conv_w", [4, 4096])
    a_log_d = din("gdn_a_log", [1, 16])
    dt_bias_d = din("gdn_dt_bias", [1, 16])
    gdn_norm_g_d = din("gdn_norm_g", [1, 128])
    w_br_ml_d = din("w_branch_ml", [D, D])
    w_br_gdn_d = din("w_branch_gdn", [2048, D])
    w_mix_d = din("w_mix_out", [D, D])
    xa_wq_d = din("xa_wq", [D, D])
    xa_wk_d = din("xa_wk", [D, D])
    xa_wv_d = din("xa_wv", [D, D])
    xa_wo_d = din("xa_wo", [D, D])
    w_router_d = din("w_router", [D, NE])
    b_router_d = din("b_router", [1, NE])
    w_gu_d = din("w_gu", [NE, D, 2 * D])
    b_gu_d = din("b_gu", [NE, 2 * D])
    w_dn_d = din("w_dn", [NE, D, D])
    b_dn_d = din("b_dn", [NE, D])
    out_d = Buf(nc.dram_tensor("out", [T, D], F32, kind="ExternalOutput").ap(), "out")
    dbg_d = {}
    for nm, shape in dbg:
        dbg_d[nm] = Buf(nc.dram_tensor(nm, list(shape), F32, kind="ExternalOutput").ap(), nm)

    w_in_b = dscr("w_in_b", [D, IN_W], BF16)
    w_br_ml_b = dscr("w_br_ml_b", [D, D], BF16)
    w_br_gdn_b = dscr("w_br_gdn_b", [2048, D], BF16)
    w_mix_b = dscr("w_mix_b", [D, D], BF16)
    xa_wq_b = dscr("xa_wq_b", [D, D], BF16)
    xa_wk_b = dscr("xa_wk_b", [D, D], BF16)
    xa_wv_b = dscr("xa_wv_b", [D, D], BF16)
    xa_wo_b = dscr("xa_wo_b", [D, D], BF16)

    h2_d = dscr("h2_d", [T, D], F32)
    h2T_d = dscr("h2T_d", [T // 128, 128, 8, 128], BF16)
    CAP = 768 if T >= 4096 else 128
    NSLOT = NE * CAP
    CBIG = 65535.0
    xg_d = dscr("xg_d", [NSLOT, D], F32)
    yg_d = dscr("yg_d", [NSLOT, D], F32)

    with ExitStack() as es:
        fw = FW(nc, es)
        es1 = es.enter_context(ExitStack())
        V = lambda fn, r=(), w=(): fw.op('dve', fn, r, w)
        A = lambda fn, r=(), w=(): fw.op('act', fn, r, w)
        G = lambda fn, r=(), w=(): fw.op('pool', fn, r, w)
        P = lambda fn, r=(), w=(): fw.op('pe', fn, r, w)

        ones_f = fw.sb("ones_f", [128, 128], F32)
        ident = fw.sb("ident", [128, 128], F32)
        UT = fw.sb("UT", [128, 128], F32)
        G(lambda e: e.memset(ones_f[:], 1.0), w=[ones_f])
        G(lambda e: e.affine_select(out=ident[:], in_=ones_f[:], pattern=[[-1, 128]], compare_op=ALU.is_equal,
                                    fill=0.0, base=0, channel_multiplier=1), r=[ones_f], w=[ident])
        G(lambda e: e.affine_select(out=UT[:], in_=ones_f[:], pattern=[[1, 128]], compare_op=ALU.is_ge,
                                    fill=0.0, base=0, channel_multiplier=-1), r=[ones_f], w=[UT])

        nhalf = fw.sb("nhalf", [128, 128], F32)
        G(lambda e: e.memset(nhalf[:], -0.5), w=[nhalf])
        st6 = fw.sb("st6", [128, 2, 6], F32)
        mv2 = fw.sb("mv2", [128, 2], F32)
        rstd1 = fw.sb("rstd1", [128, 1], F32)
        eps_t = fw.sb("eps_t", [128, 4], F32)
        NT_ALL = T // 128
        bc_reg = nc.gpsimd.to_reg(NSLOT - 1)
        slot_i = fw.sb("slot_i", [128, NT_ALL, 4], I32)
        gwk = fw.sb("gwk", [128, NT_ALL, 4], F32)
        fw.es = es1

        vec_t = {}
        for n in vec_names:
            vec_t[n] = fw.sb("c_" + n, [128, D], F32, es=es1)
            fw.dma('sp', vec_t[n][:], vec_d[n].t.partition_broadcast(128), r=[vec_d[n]], w=[vec_t[n]])
        mlb_t = fw.sb("mlb_t", [128, 8], F32)
        fw.dma('sp', mlb_t[:], ml_gate_bias_d.t.partition_broadcast(128), r=[ml_gate_bias_d], w=[mlb_t])
        w_small = fw.sb("w_small", [128, 8, 40], F32)
        wv = w_in_d.t.rearrange("(kc p) n -> p kc n", p=128)
        with nc.allow_non_contiguous_dma(reason="small gate columns"):
            fw.dma('sp', w_small[:, :, 0:8], wv[:, :, 3072:3080], r=[w_in_d], w=[w_small])
            fw.dma('sp', w_small[:, :, 8:40], wv[:, :, 9224:9256], r=[w_in_d], w=[w_small])

        banks = [fw.ps(f"bank{i}", [128, 512], F32) for i in range(8)]
        bank_rr = [0]

        def bank():
            b = banks[bank_rr[0]]
            bank_rr[0] = (bank_rr[0] + 1) % 8
            return b

        slabs = [fw.sb(f"slab{i}", [128, 8, 512], BF16, es=es1) for i in range(4)]
        cast_tmp = slabs
        cast_rr = [0]

        w_src = {"w_in_b": w_in_d, "w_br_ml_b": w_br_ml_d, "w_br_gdn_b": w_br_gdn_d, "w_mix_b": w_mix_d,
                 "xa_wq_b": xa_wq_d, "xa_wk_b": xa_wk_d, "xa_wv_b": xa_wv_d, "xa_wo_b": xa_wo_d}
        slab_keys = [("w_in_b", 0, c) for c in ([0, 512, 1024, 1536, 2048, 2560] + [3080 + 512 * j for j in range(8)]
                                               + [7176 + 512 * j for j in range(4)] + [9256, 9768, 10280, 10792])]
        if stage >= 3:
            slab_keys += [("w_br_ml_b", 0, 0), ("w_br_ml_b", 0, 512)]
            slab_keys += [("w_br_gdn_b", k0, c) for c in (0, 512) for k0 in (0, 8)]
            for nm in ["w_mix_b", "xa_wq_b", "xa_wk_b", "xa_wv_b", "xa_wo_b"]:
                slab_keys += [(nm, 0, 0), (nm, 0, 512)]
        slab_idx = {k: i for i, k in enumerate(slab_keys)}
        wslab_d = dscr("wslab_d", [len(slab_keys), 128, 4096], BF16)
        wslab_b = [Buf(wslab_d.t[i], f"wslab{i}") for i in range(len(slab_keys))]
        g8_tm = fw.sb("g8_tm", [8, 128], F32, es=es1)
        g1_tm = fw.sb("g1_tm", [1, 128], F32, es=es1)
        gfm_ml = fw.sb("gfm_ml", [128, 8], F32, es=es1)
        gfm_gdn = fw.sb("gfm_gdn", [128, 1], F32, es=es1)
        ml_norm_g_d = din("ml_norm_g", [1, D])
        fw.dma('sp', g8_tm[:], ml_norm_g_d.t.rearrange("o (c p) -> (o c) p", p=128), r=[ml_norm_g_d], w=[g8_tm])
        fw.dma('sp', g1_tm[:], gdn_norm_g_d.t, r=[gdn_norm_g_d], w=[g1_tm])
        bkq = bank()
        P(lambda e: e.transpose(bkq[:, 0:8], g8_tm[0:8, :], ident[0:8, 0:8]), r=[g8_tm, ident], w=[bkq])
        P(lambda e: e.transpose(bkq[:, 8:9], g1_tm[0:1, :], ident[0:1, 0:1]), r=[g1_tm, ident], w=[bkq])
        V(lambda e: e.tensor_copy(out=gfm_ml[:], in_=bkq[:, 0:8]), r=[bkq], w=[gfm_ml])
        V(lambda e: e.tensor_copy(out=gfm_gdn[:], in_=bkq[:, 8:9]), r=[bkq], w=[gfm_gdn])
        for (nm, k0, c0) in slab_keys:
            src = w_src[nm]
            tmp = cast_tmp[cast_rr[0]]
            cast_rr[0] = (cast_rr[0] + 1) % len(cast_tmp)
            sv = src.t[k0 * 128:(k0 + 8) * 128, c0:c0 + 512].rearrange("(kc p) n -> p kc n", p=128)
            fw.dma('pool', tmp[:], sv, r=[src], w=[tmp])
            if nm == "w_br_ml_b":
                for kc in range(8):
                    V(lambda e: e.tensor_scalar(out=tmp[:, kc, :], in0=tmp[:, kc, :], scalar1=gfm_ml[:, kc:kc + 1], scalar2=None, op0=ALU.mult),
                      r=[tmp, gfm_ml], w=[tmp])
            elif nm == "w_br_gdn_b":
                V(lambda e: e.tensor_scalar(out=tmp[:].rearrange("p a b -> p (a b)"), in0=tmp[:].rearrange("p a b -> p (a b)"),
                                            scalar1=gfm_gdn[:, 0:1], scalar2=None, op0=ALU.mult), r=[tmp, gfm_gdn], w=[tmp])
            fw.dma('sp', wslab_d.t[slab_idx[(nm, k0, c0)]], tmp[:].rearrange("p a b -> p (a b)"), r=[tmp], w=[wslab_b[slab_idx[(nm, k0, c0)]]])

        slab_rr = [0]

        def load_slab(wb, k0, kc, c0, w):
            sl = slabs[slab_rr[0]]
            slab_rr[0] = (slab_rr[0] + 1) % len(slabs)
            fw.dma('sp', sl[:].rearrange("p a b -> p (a b)"), wslab_d.t[slab_idx[(wb.name, k0, c0)]], r=[wslab_b[slab_idx[(wb.name, k0, c0)]]], w=[sl])
            return sl


        def layernorm(src, dst, gname, bname):
            for hf in range(2):
                V(lambda e: e.bn_stats(out=st6[:, hf, :], in_=src[:, hf * 512:(hf + 1) * 512]), r=[src], w=[st6])
            V(lambda e: e.bn_aggr(out=mv2[:], in_=st6[:].rearrange("p a b -> p (a b)")), r=[st6], w=[mv2])
            V(lambda e: e.tensor_scalar(out=rstd1[:], in0=mv2[:, 1:2], scalar1=LN_EPS, scalar2=None, op0=ALU.add), r=[mv2], w=[rstd1])
            G(lambda e: e.tensor_tensor(out=rstd1[:], in0=rstd1[:], in1=nhalf[:, 0:1], op=ALU.pow), r=[rstd1, nhalf], w=[rstd1])
            V(lambda e: e.tensor_scalar(out=dst[:], in0=src[:], scalar1=mv2[:, 0:1], scalar2=rstd1[:, 0:1],
                                        op0=ALU.subtract, op1=ALU.mult), r=[src, mv2, rstd1], w=[dst])
            V(lambda e: e.tensor_tensor(out=dst[:], in0=dst[:], in1=vec_t[gname][:], op=ALU.mult), r=[dst, vec_t[gname]], w=[dst])
            V(lambda e: e.tensor_tensor(out=dst[:], in0=dst[:], in1=vec_t[bname][:], op=ALU.add), r=[dst, vec_t[bname]], w=[dst])

        G(lambda e: e.memset(eps_t[:, 0:1], LN_EPS), w=[eps_t])
        G(lambda e: e.memset(eps_t[:, 1:2], RMS_EPS), w=[eps_t])
        G(lambda e: e.memset(eps_t[:, 2:3], lnscale), w=[eps_t])
        G(lambda e: e.memset(eps_t[:, 3:4], 1.0), w=[eps_t])


        def transpose_fm(src, ncol, dst_bf=None, dst_f=None):
            nch = ncol // 128
            for c0 in range(0, nch, 4):
                n = min(4, nch - c0)
                bk = bank()
                for c in range(n):
                    P(lambda e: e.transpose(bk[:, c * 128:(c + 1) * 128], src[:, (c0 + c) * 128:(c0 + c + 1) * 128], ident[:]),
                      r=[src, ident], w=[bk])
                pv = bk[:, 0:n * 128].rearrange("p (c t) -> p c t", t=128)
                if dst_bf is not None:
                    A(lambda e: e.activation(out=dst_bf[:, c0:c0 + n, :], in_=pv, func=AF.Copy), r=[bk], w=[dst_bf])
                if dst_f is not None:
                    V(lambda e: e.tensor_copy(out=dst_f[:, c0:c0 + n, :], in_=pv), r=[bk], w=[dst_f])

        def mm_tm(ps_ap, psb, xT, sl, kc, w, k_off=0):
            for k in range(kc):
                P(lambda e: e.matmul(ps_ap, lhsT=xT[:, k_off + k, :], rhs=sl[:, k, 0:w], start=(k == 0), stop=(k == kc - 1)),
                  r=[xT, sl], w=[psb])

        def mm_fm(ps_ap, psb, xT, sl, kc, m0, ntok=128):
            for k in range(kc):
                P(lambda e: e.matmul(ps_ap, lhsT=sl[:, k, m0:m0 + 128], rhs=xT[:, k, 0:ntok], start=(k == 0), stop=(k == kc - 1)),
                  r=[xT, sl], w=[psb])

        xt = fw.sb("xt", [128, D], F32)
        h0 = fw.sb("h0", [128, D], F32)
        xnT = fw.sb("xnT", [128, 8, 128], BF16)
        mqT = fw.sb("mqT", [128, 4, 128], BF16)
        mkT = fw.sb("mkT", [128, 4, 128], BF16)
        mk_tm = fw.sb("mk_tm", [128, 4, 128], F32)
        mv_aug = fw.sb("mv_aug", [128, 4, 258], BF16)
        mo_sig = fw.sb("mo_sig", [128, D], BF16)
        gsm = fw.sb("gsm", [128, 40], F32)
        C_f = [fw.sb(f"C_f{h}", [128, 258], F32) for h in range(4)]
        C_b = [fw.sb(f"C_b{h}", [128, 258], BF16) for h in range(4)]
        hm = fw.sb("hm", [128, D], F32)
        hm_n = fw.sb("hm_n", [128, D], F32)
        PTb = [fw.sb(f"PTb{i}", [128, 128], BF16) for i in range(2)]
        kwb = [fw.sb(f"kwb{i}", [128, 128], BF16) for i in range(2)]
        g8 = fw.sb("g8", [128, 8], F32)
        nlf = fw.sb("nlf", [128, 4], F32)
        t1 = fw.sb("t1", [128, 4], F32)
        t2 = fw.sb("t2", [128, 4], F32)
        a_p = fw.sb("a_p", [128, 4], F32)
        ag_p = fw.sb("ag_p", [128, 4], F32)
        eb = fw.sb("eb", [128, 4], F32)
        eG = fw.sb("eG", [128, 4], F32)
        dsc = fw.sb("dsc", [128, 4], F32)
        ssq = fw.sb("ssq", [128, 16], F32)
        junk = fw.sb("junk", [128, 256], F32)
        G(lambda e: e.memset(mv_aug[:], 1.0), w=[mv_aug])

        def dbg_out(name, src_ap, srcb, row0, ncol):
            if name in dbg_d:
                fw.dma('sp', dbg_d[name].t[row0:row0 + 128, 0:ncol], src_ap, r=[srcb], w=[dbg_d[name]])

        BLK = fw.sb("BLK", [128, 128], F32)
        Ublk = fw.sb("Ublk", [128, 128], F32)
        Yblk = fw.sb("Yblk", [128, 128], F32)
        maskS = fw.sb("maskS", [128, 128], F32)
        sel0 = fw.sb("sel0", [128, 128], F32)
        sel1 = fw.sb("sel1", [128, 128], F32)
        G(lambda e: e.memset(BLK[:], 0.0), w=[BLK])
        G(lambda e: e.memset(BLK[0:64, 0:64], 1.0), w=[BLK])
        G(lambda e: e.memset(BLK[64:128, 64:128], 1.0), w=[BLK])
        G(lambda e: e.memset(sel0[:], 0.0), w=[sel0])
        G(lambda e: e.memset(sel0[0:64, :], 1.0), w=[sel0])
        G(lambda e: e.memset(sel1[:], 0.0), w=[sel1])
        G(lambda e: e.memset(sel1[64:128, :], 1.0), w=[sel1])
        G(lambda e: e.tensor_tensor(out=Ublk[:], in0=UT[:], in1=BLK[:], op=ALU.mult), r=[UT, BLK], w=[Ublk])
        G(lambda e: e.tensor_tensor(out=Yblk[:], in0=BLK[:], in1=Ublk[:], op=ALU.subtract), r=[BLK, Ublk], w=[Yblk])
        G(lambda e: e.tensor_tensor(out=maskS[:], in0=Ublk[:], in1=ident[:], op=ALU.subtract), r=[Ublk, ident], w=[maskS])
        cw_tm = fw.sb("cw_tm", [32, 4, 128], F32)
        cw = fw.sb("cw", [128, 4, 32], F32)
        fw.dma('sp', cw_tm[:], conv_w_d.t.rearrange("j (c p) -> c j p", p=128), r=[conv_w_d], w=[cw_tm])
        bkc = bank()
        for j in range(4):
            P(lambda e: e.transpose(bkc[:, j * 32:(j + 1) * 32], cw_tm[0:32, j, :], ident[0:32, 0:32]), r=[cw_tm, ident], w=[bkc])
        V(lambda e: e.tensor_copy(out=cw[:].rearrange("p j c -> p (j c)"), in_=bkc[:, 0:128]), r=[bkc], w=[cw])
        expA = fw.sb("expA", [128, 16], F32)
        dtb = fw.sb("dtb", [128, 16], F32)
        fw.dma('sp', expA[:], a_log_d.t.partition_broadcast(128), r=[a_log_d], w=[expA])
        fw.dma('sp', dtb[:], dt_bias_d.t.partition_broadcast(128), r=[dt_bias_d], w=[dtb])
        A(lambda e: e.activation(out=expA[:], in_=expA[:], func=AF.Exp), r=[expA], w=[expA])
        halo = fw.sb("halo", [128, 32, 3], F32)
        zcb = [fw.sb(f"zc{i}", [128, 131], F32) for i in range(6)]
        ycb = [fw.sb(f"yc{i}", [128, 128], F32) for i in range(6)]
        scb = [fw.sb(f"sc{i}", [128, 128], F32) for i in range(6)]
        sqb = [fw.sb(f"sq{i}", [128, 128], F32) for i in range(6)]
        gqT = fw.sb("gqT", [128, 8, 128], BF16)
        gkT = fw.sb("gkT", [128, 8, 128], BF16)
        k_tm = fw.sb("k_tm", [128, 8, 128], F32)
        v_tm = fw.sb("v_tm", [128, 16, 128], BF16)
        gz_silu = fw.sb("gz_silu", [128, 2048], BF16)
        gg = fw.sb("gg", [128, 16 * 8], F32)
        S_f = [fw.sb(f"S_f{h}", [128, 128], F32) for h in range(16)]
        S_b = [fw.sb(f"S_b{h}", [128, 128], BF16) for h in range(16)]
        vnew = [fw.sb(f"vnew{h}", [128, 128], BF16) for h in range(16)]
        o_g = fw.sb("o_g", [128, 2048], F32)
        o_n = o_g
        xnT_f = Buf(o_g.t[:, 0:1024].rearrange("p (c t) -> p c t", t=128), "xnT_f", parent=o_g)
        GH = 4
        kkS = [fw.sb(f"kkS{i}", [128, 128], F32) for i in range(2)]
        qkS = [fw.sb(f"qkS{i}", [128, 128], F32) for i in range(2)]
        Ld = [fw.sb(f"Ld{i}", [128, 128], F32) for i in range(GH)]
        decb = [fw.sb(f"dec{i}", [128, 128], F32) for i in range(GH)]
        Nb = [[fw.sb(f"N{i}_{j}", [128, 128], F32) for j in range(2)] for i in range(GH)]
        Mb = [[fw.sb(f"M{i}_{j}", [128, 128], F32) for j in range(2)] for i in range(GH)]
        Rf = [fw.sb(f"Rf{i}", [128, 128], F32) for i in range(GH)]
        attnT = [fw.sb(f"attnT{i}", [128, 128], BF16) for i in range(8)]
        Rb = [fw.sb(f"Rb{i}", [128, 128], BF16) for i in range(8)]
        negWT = [fw.sb(f"negWT{i}", [128, 128], BF16) for i in range(8)]
        kd = [fw.sb(f"kd{i}", [128, 128], BF16) for i in range(8)]
        kgb = [fw.sb(f"kg{i}", [128, 128], BF16) for i in range(2)]
        p2s = [fw.sb(f"p2s{i}", [128, 128], F32) for i in range(2)]
        BETA, NBETA, NGD, EG, EGR, TMPG, ES = 0, 16, 32, 48, 64, 80, 96

        w_rt = fw.sb("w_rt", [128, 8, NE], F32)
        b_rt = fw.sb("b_rt", [128, NE], F32)
        with nc.allow_non_contiguous_dma(reason="router weights"):
            fw.dma('sp', w_rt[:], w_router_d.t.rearrange("(kc p) n -> p kc n", p=128), r=[w_router_d], w=[w_rt])
        fw.dma('sp', b_rt[:], b_router_d.t.partition_broadcast(128), r=[b_router_d], w=[b_rt])
        rt = fw.sb("rt", [128, 8, NE], F32)
        top8 = fw.sb("top8", [128, 24], F32)
        UTs = fw.sb("UTs", [128, 128], F32)
        G(lambda e: e.tensor_tensor(out=UTs[:], in0=UT[:], in1=ident[:], op=ALU.subtract), r=[UT, ident], w=[UTs])
        base_c = fw.sb("base_c", [128, NE], F32)
        G(lambda e: e.memset(base_c[:], 0.0), w=[base_c])
        eoff_i = fw.sb("eoff_i", [128, NE], I32)
        eoff = fw.sb("eoff", [128, NE], F32)
        G(lambda e: e.iota(eoff_i[:], pattern=[[CAP, NE]], base=0, channel_multiplier=0), w=[eoff_i])
        V(lambda e: e.tensor_copy(out=eoff[:], in_=eoff_i[:]), r=[eoff_i], w=[eoff])
        hmT = fw.sb("hmT", [128, 8, 128], BF16)
        oT = fw.sb("oT", [128, 16, 128], BF16)
        gsig = [fw.sb(f"gsig{i}", [128, 512], F32) for i in range(2)]
        memT = Buf(gz_silu.t[:].rearrange("p (k t) -> p k t", t=256), "memT", parent=gz_silu)
        KT = fw.sb("KT", [128, 8, 256], BF16)
        Vx = fw.sb("Vx", [128, 2, 1024], BF16)
        sc_all = fw.sb("sc_all", [128, 4, 256], F32)
        pT_all = fw.sb("pT_all", [128, 8, 128], BF16)
        smx = fw.sb("smx", [128, 16], F32)
        merged = hm
        h1 = xt
        h2 = hm_n

        import os as _os
        _prof = False
        cur_scope = [None]

        def mark(name, active=True):
            if cur_scope[0] is not None:
                nc.leave_named_scope(cur_scope[0][0], cur_scope[0][1], False)
                cur_scope[0] = None
            if _prof and active and name:
                sid, _ = nc.enter_named_scope(name, False)
                cur_scope[0] = (name, sid)

        for si in range(n_seq):
            for b_ in C_f + C_b + S_f + S_b:
                G(lambda e: e.memset(b_[:], 0.0), w=[b_])
            G(lambda e: e.memset(halo[:], 0.0), w=[halo])

            if stage >= 4:
                for mt in range(2):
                    fw.dma('sp', o_g[:, 0:1024], mem_d.t[si * MEM + mt * 128: si * MEM + (mt + 1) * 128, :], r=[mem_d], w=[o_g])
                    for c0 in range(0, 8, 4):
                        bk = bank()
                        for c in range(4):
                            P(lambda e: e.transpose(bk[:, c * 128:(c + 1) * 128], o_g[:, (c0 + c) * 128:(c0 + c + 1) * 128], ident[:]), r=[o_g, ident], w=[bk])
                        A(lambda e: e.activation(out=memT[:, c0:c0 + 4, mt * 128:(mt + 1) * 128], in_=bk[:, 0:512].rearrange("p (c t) -> p c t", t=128), func=AF.Copy),
                          r=[bk], w=[memT])
                for j in range(2):
                    sl = load_slab(xa_wk_b, 0, 8, j * 512, 512)
                    for m in range(4):
                        bk = bank()
                        mm_fm(bk[:, 0:256], bk, memT, sl, 8, m * 128, ntok=256)
                        A(lambda e: e.activation(out=KT[:, j * 4 + m, :], in_=bk[:, 0:256], func=AF.Copy), r=[bk], w=[KT])
                for j in range(2):
                    sl = load_slab(xa_wv_b, 0, 8, j * 512, 512)
                    for mt in range(2):
                        bk = bank()
                        for k in range(8):
                            P(lambda e: e.matmul(bk[:, 0:512], lhsT=memT[:, k, mt * 128:(mt + 1) * 128], rhs=sl[:, k, 0:512], start=(k == 0), stop=(k == 7)),
                              r=[memT, sl], w=[bk])
                        A(lambda e: e.activation(out=Vx[:, mt, j * 512:(j + 1) * 512], in_=bk[:, 0:512], func=AF.Copy), r=[bk], w=[Vx])
            for ti in range(NSUB):
                row0 = si * S + ti * 128
                mark('ln_in', si == 0 and ti == 2)
                fw.dma('sp', xt[:], x_d.t[row0:row0 + 128, :], r=[x_d], w=[xt])
                layernorm(xt, h0, "ln_in_g", "ln_in_b")
                if stage == 0:
                    fw.dma('sp', out_d.t[row0:row0 + 128, :], h0[:], r=[h0], w=[out_d])
                    continue
                transpose_fm(h0, D, dst_bf=xnT, dst_f=xnT_f)
                if stage == -1:
                    V(lambda e: e.tensor_copy(out=hm_n[:].rearrange("p (c t) -> p c t", t=128), in_=xnT[:]), r=[xnT], w=[hm_n])
                    fw.dma('sp', out_d.t[row0:row0 + 128, :], hm_n[:], r=[hm_n], w=[out_d])
                    continue
                mark('proj_small', si == 0 and ti == 2)
                bk = bank()
                for k in range(8):
                    P(lambda e: e.matmul(bk[:, 0:40], lhsT=xnT_f[:, k, :], rhs=w_small[:, k, :], start=(k == 0), stop=(k == 7)),
                      r=[xnT_f, w_small], w=[bk])
                V(lambda e: e.tensor_copy(out=gsm[:], in_=bk[:, 0:40]), r=[bk], w=[gsm])
                mark('proj_mlstm', si == 0 and ti == 2)
                sl = load_slab(w_in_b, 0, 8, 0, 512)
                for m in range(4):
                    bk = bank()
                    mm_fm(bk[:, 0:128], bk, xnT, sl, 8, m * 128)
                    A(lambda e: e.activation(out=mqT[:, m, :], in_=bk[:, 0:128], func=AF.Copy), r=[bk], w=[mqT])
                sl = load_slab(w_in_b, 0, 8, 512, 512)
                for m in range(4):
                    bk = bank()
                    mm_fm(bk[:, 0:128], bk, xnT, sl, 8, m * 128)
                    A(lambda e: e.activation(out=mkT[:, m, :], in_=bk[:, 0:128], func=AF.Copy), r=[bk], w=[mkT])
                bk = bank()
                mm_tm(bk[:, 0:512], bk, xnT, sl, 8, 512)
                V(lambda e: e.tensor_copy(out=mk_tm[:].rearrange("p h d -> p (h d)"), in_=bk[:, 0:512]), r=[bk], w=[mk_tm])
                for j in range(2):
                    sl = load_slab(w_in_b, 0, 8, 1024 + j * 512, 512)
                    bk = bank()
                    mm_tm(bk[:, 0:512], bk, xnT, sl, 8, 512)
                    A(lambda e: e.activation(out=mv_aug[:, 2 * j:2 * j + 2, 0:256], in_=bk[:, 0:512].rearrange("p (h d) -> p h d", d=256),
                                             func=AF.Copy), r=[bk], w=[mv_aug])
                for j in range(2):
                    sl = load_slab(w_in_b, 0, 8, 2048 + j * 512, 512)
                    bk = bank()
                    mm_tm(bk[:, 0:512], bk, xnT, sl, 8, 512)
                    A(lambda e: e.activation(out=mo_sig[:, j * 512:(j + 1) * 512], in_=bk[:, 0:512], func=AF.Sigmoid), r=[bk], w=[mo_sig])
                mark('mlstm_gates', si == 0 and ti == 2)
                V(lambda e: e.tensor_tensor(out=g8[:], in0=gsm[:, 0:8], in1=mlb_t[:], op=ALU.add), r=[gsm, mlb_t], w=[g8])
                A(lambda e: e.activation(out=nlf[:], in_=g8[:, 4:8], func=AF.Exp, scale=-1.0), r=[g8], w=[nlf])
                A(lambda e: e.activation(out=nlf[:], in_=nlf[:], func=AF.Ln, bias=eps_t[:, 3:4], scale=1.0), r=[nlf, eps_t], w=[nlf])
                bk = bank()
                P(lambda e: e.matmul(bk[:, 0:4], lhsT=UT[:], rhs=nlf[:], start=True, stop=True), r=[UT, nlf], w=[bk])
                P(lambda e: e.matmul(bk[:, 4:8], lhsT=ones_f[:], rhs=nlf[:], start=True, stop=True), r=[ones_f, nlf], w=[bk])
                V(lambda e: e.tensor_tensor(out=t1[:], in0=g8[:, 0:4], in1=bk[:, 0:4], op=ALU.add), r=[g8, bk], w=[t1])
                V(lambda e: e.tensor_tensor(out=t2[:], in0=t1[:], in1=bk[:, 4:8], op=ALU.subtract), r=[t1, bk], w=[t2])
                A(lambda e: e.activation(out=a_p[:], in_=t1[:], func=AF.Exp, bias=eps_t[:, 2:3], scale=1.0), r=[t1, eps_t], w=[a_p])
                A(lambda e: e.activation(out=ag_p[:], in_=t2[:], func=AF.Exp, bias=eps_t[:, 2:3], scale=1.0), r=[t2, eps_t], w=[ag_p])
                A(lambda e: e.activation(out=eb[:], in_=bk[:, 0:4], func=AF.Exp, scale=-1.0), r=[bk], w=[eb])
                A(lambda e: e.activation(out=eG[:], in_=bk[:, 4:8], func=AF.Exp, scale=-1.0), r=[bk], w=[eG])
                mark('mlstm_heads', si == 0 and ti == 2)
                for h in range(4):
                    pt = PTb[h % 2]
                    kw = kwb[h % 2]
                    bk = bank()
                    P(lambda e: e.matmul(bk[:, 0:128], lhsT=mkT[:, h, :], rhs=mqT[:, h, :], start=True, stop=True), r=[mkT, mqT], w=[bk])
                    V(lambda e: e.scalar_tensor_tensor(out=pt[:], in0=bk[:, 0:128], scalar=a_p[:, h:h + 1], in1=UT[:],
                                                       op0=ALU.mult, op1=ALU.mult), r=[bk, a_p, UT], w=[pt])
                    bk2 = bank()
                    P(lambda e: e.matmul(bk2[:, 0:257], lhsT=mqT[:, h, :], rhs=C_b[h][:, 0:257], start=True, stop=False), r=[mqT, C_b[h]], w=[bk2])
                    P(lambda e: e.matmul(bk2[:, 0:257], lhsT=pt[:], rhs=mv_aug[:, h, 0:257], start=False, stop=True), r=[pt, mv_aug], w=[bk2])
                    V(lambda e: e.tensor_tensor(out=dsc[:, 0:1], in0=bk2[:, 256:257], in1=eb[:, h:h + 1], op=ALU.mult), r=[bk2, eb], w=[dsc])
                    V(lambda e: e.tensor_scalar(out=dsc[:, 2:3], in0=dsc[:, 0:1], scalar1=-1.0, scalar2=None, op0=ALU.mult), r=[dsc], w=[dsc])
                    V(lambda e: e.scalar_tensor_tensor(out=dsc[:, 1:2], in0=dsc[:, 2:3], scalar=1.0, in1=dsc[:, 0:1], op0=ALU.max, op1=ALU.max),
                      r=[dsc], w=[dsc])
                    V(lambda e: e.reciprocal(out=dsc[:, 2:3], in_=dsc[:, 1:2]), r=[dsc], w=[dsc])
                    V(lambda e: e.tensor_tensor(out=dsc[:, 3:4], in0=dsc[:, 2:3], in1=eb[:, h:h + 1], op=ALU.mult), r=[dsc, eb], w=[dsc])
                    V(lambda e: e.tensor_scalar(out=hm[:, h * 256:(h + 1) * 256], in0=bk2[:, 0:256], scalar1=dsc[:, 3:4], scalar2=None,
                                                op0=ALU.mult), r=[bk2, dsc], w=[hm])
                    V(lambda e: e.tensor_scalar(out=kw[:], in0=mk_tm[:, h, :], scalar1=ag_p[:, h:h + 1], scalar2=None, op0=ALU.mult),
                      r=[mk_tm, ag_p], w=[kw])
                    bk3 = bank()
                    P(lambda e: e.matmul(bk3[:, 0:257], lhsT=kw[:], rhs=mv_aug[:, h, 0:257], start=True, stop=True), r=[kw, mv_aug], w=[bk3])
                    V(lambda e: e.scalar_tensor_tensor(out=C_f[h][:, 0:257], in0=C_f[h][:, 0:257], scalar=eG[:, h:h + 1], in1=bk3[:, 0:257],
                                                       op0=ALU.mult, op1=ALU.add), r=[C_f[h], eG, bk3], w=[C_f[h]])
                    G(lambda e: e.tensor_copy(out=C_b[h][:, 0:257], in_=C_f[h][:, 0:257]), r=[C_f[h]], w=[C_b[h]])
                    A(lambda e: e.activation(out=junk[:], in_=hm[:, h * 256:(h + 1) * 256], func=AF.Square, accum_out=ssq[:, h:h + 1]),
                      r=[hm], w=[junk, ssq])
                V(lambda e: e.tensor_scalar(out=ssq[:, 4:8], in0=ssq[:, 0:4], scalar1=1.0 / 256, scalar2=RMS_EPS, op0=ALU.mult, op1=ALU.add), r=[ssq], w=[ssq])
                G(lambda e: e.tensor_tensor(out=ssq[:, 8:12], in0=ssq[:, 4:8], in1=nhalf[:, 0:4], op=ALU.pow), r=[ssq, nhalf], w=[ssq])
                for h in range(4):
                    V(lambda e: e.scalar_tensor_tensor(out=hm_n[:, h * 256:(h + 1) * 256], in0=hm[:, h * 256:(h + 1) * 256],
                                                       scalar=ssq[:, 8 + h:9 + h], in1=mo_sig[:, h * 256:(h + 1) * 256],
                                                       op0=ALU.mult, op1=ALU.mult), r=[hm, ssq, mo_sig], w=[hm_n])
                dbg_out("d_h0", h0[:], h0, row0, D)
                dbg_out("d_hm", hm_n[:], hm_n, row0, D)
                if stage <= 1:
                    fw.dma('sp', out_d.t[row0:row0 + 128, :], hm_n[:], r=[hm_n], w=[out_d])
                    continue

                mark('gdn_conv', si == 0 and ti == 2)
                for sj in range(8):
                    sl = load_slab(w_in_b, 0, 8, 3080 + sj * 512, 512)
                    for m in range(4):
                        c = sj * 4 + m
                        zc = zcb[c % 6]; yc = ycb[c % 6]
                        bk = bank()
                        mm_fm(bk[:, 0:128], bk, xnT, sl, 8, m * 128)
                        G(lambda e: e.tensor_copy(out=zc[:, 0:3], in_=halo[:, c, :]), r=[halo], w=[zc])
                        A(lambda e: e.activation(out=zc[:, 3:131], in_=bk[:, 0:128], func=AF.Copy), r=[bk], w=[zc])
                        A(lambda e: e.activation(out=yc[:], in_=bk[:, 0:128], func=AF.Copy, scale=cw[:, 3, c:c + 1]), r=[bk, cw], w=[yc])
                        G(lambda e: e.tensor_copy(out=halo[:, c, :], in_=zc[:, 128:131]), r=[zc], w=[halo])
                    for m in range(4):
                        c = sj * 4 + m
                        zc = zcb[c % 6]; yc = ycb[c % 6]
                        for j in range(0, 3):
                            V(lambda e: e.scalar_tensor_tensor(out=yc[:], in0=zc[:, j:j + 128], scalar=cw[:, j, c:c + 1], in1=yc[:],
                                                               op0=ALU.mult, op1=ALU.add), r=[zc, cw, yc], w=[yc])
                    for m in range(4):
                        c = sj * 4 + m
                        yc = ycb[c % 6]; sc = scb[c % 6]
                        A(lambda e: e.activation(out=sc[:], in_=yc[:], func=AF.Silu), r=[yc], w=[sc])
                    for m in range(4):
                        c = sj * 4 + m
                        sc = scb[c % 6]; sq = sqb[c % 6]
                        if c < 16:
                            G(lambda e: e.tensor_tensor(out=sq[:], in0=sc[:], in1=sc[:], op=ALU.mult), r=[sc], w=[sq])
                            bk2 = bank()
                            P(lambda e: e.matmul(bk2[:, 0:128], lhsT=ones_f[:], rhs=sq[:], start=True, stop=True), r=[ones_f, sq], w=[bk2])
                            A(lambda e: e.activation(out=sq[:], in_=bk2[:, 0:128], func=AF.Sqrt, bias=eps_t[:, 1:2], scale=1.0), r=[bk2, eps_t], w=[sq])
                            V(lambda e: e.reciprocal(out=sq[:], in_=sq[:]), r=[sq], w=[sq])
                            if c < 8:
                                V(lambda e: e.scalar_tensor_tensor(out=gqT[:, c, :], in0=sc[:], scalar=128 ** -0.5, in1=sq[:],
                                                                   op0=ALU.mult, op1=ALU.mult), r=[sc, sq], w=[gqT])
                            else:
                                V(lambda e: e.tensor_tensor(out=sc[:], in0=sc[:], in1=sq[:], op=ALU.mult), r=[sc, sq], w=[sc])
                                G(lambda e: e.tensor_copy(out=gkT[:, c - 8, :], in_=sc[:]), r=[sc], w=[gkT])
                                bk3 = bank()
                                P(lambda e: e.transpose(bk3[:, 0:128], sc[:], ident[:]), r=[sc, ident], w=[bk3])
                                V(lambda e: e.tensor_copy(out=k_tm[:, c - 8, :], in_=bk3[:, 0:128]), r=[bk3], w=[k_tm])
                        else:
                            bk3 = bank()
                            P(lambda e: e.transpose(bk3[:, 0:128], sc[:], ident[:]), r=[sc, ident], w=[bk3])
                            A(lambda e: e.activation(out=v_tm[:, c - 16, :], in_=bk3[:, 0:128], func=AF.Copy), r=[bk3], w=[v_tm])
                mark('gdn_gz', si == 0 and ti == 2)
                for j in range(4):
                    sl = load_slab(w_in_b, 0, 8, 7176 + j * 512, 512)
                    bk = bank()
                    mm_tm(bk[:, 0:512], bk, xnT, sl, 8, 512)
                    A(lambda e: e.activation(out=gz_silu[:, j * 512:(j + 1) * 512], in_=bk[:, 0:512], func=AF.Silu), r=[bk], w=[gz_silu])
                mark('gdn_gates', si == 0 and ti == 2)
                A(lambda e: e.activation(out=gg[:, BETA:BETA + 16], in_=gsm[:, 24:40], func=AF.Sigmoid), r=[gsm], w=[gg])
                V(lambda e: e.tensor_scalar(out=gg[:, NBETA:NBETA + 16], in0=gg[:, BETA:BETA + 16], scalar1=-1.0, scalar2=None, op0=ALU.mult), r=[gg], w=[gg])
                V(lambda e: e.tensor_tensor(out=gg[:, TMPG:TMPG + 16], in0=gsm[:, 8:24], in1=dtb[:], op=ALU.add), r=[gsm, dtb], w=[gg])
                A(lambda e: e.activation(out=gg[:, TMPG:TMPG + 16], in_=gg[:, TMPG:TMPG + 16], func=AF.Exp), r=[gg], w=[gg])
                A(lambda e: e.activation(out=gg[:, TMPG:TMPG + 16], in_=gg[:, TMPG:TMPG + 16], func=AF.Ln, bias=eps_t[:, 3:4], scale=1.0), r=[gg, eps_t], w=[gg])
                V(lambda e: e.tensor_tensor(out=gg[:, NGD:NGD + 16], in0=gg[:, TMPG:TMPG + 16], in1=expA[:], op=ALU.mult), r=[gg, expA], w=[gg])
                bkg = bank()
                for qi, lm in enumerate([Ublk, BLK, sel0, sel1]):
                    P(lambda e: e.matmul(bkg[:, qi * 16:(qi + 1) * 16], lhsT=lm[:], rhs=gg[:, NGD:NGD + 16], start=True, stop=True), r=[lm, gg], w=[bkg])
                A(lambda e: e.activation(out=gg[:, EG:EG + 16], in_=bkg[:, 0:16], func=AF.Exp, scale=-1.0), r=[bkg], w=[gg])
                A(lambda e: e.activation(out=gg[:, TMPG:TMPG + 16], in_=bkg[:, 16:32], func=AF.Copy), r=[bkg], w=[gg])
                V(lambda e: e.tensor_tensor(out=gg[:, TMPG:TMPG + 16], in0=bkg[:, 0:16], in1=gg[:, TMPG:TMPG + 16], op=ALU.subtract), r=[bkg, gg], w=[gg])
                A(lambda e: e.activation(out=gg[:, EGR:EGR + 16], in_=gg[:, TMPG:TMPG + 16], func=AF.Exp), r=[gg], w=[gg])
                A(lambda e: e.activation(out=gg[:, ES:ES + 32], in_=bkg[:, 32:64], func=AF.Exp, scale=-1.0), r=[bkg], w=[gg])
                for half in range(2):
                  mark('gdn_inv%d' % half, si == 0 and ti == 2)
                  for g0 in range(half * 8, half * 8 + 8, GH):
                      for q2 in range(GH // 2):
                          hq = g0 // 2 + q2
                          bk = bank()
                          P(lambda e: e.matmul(bk[:, 0:128], lhsT=gkT[:, hq, :], rhs=gkT[:, hq, :], start=True, stop=True), r=[gkT], w=[bk])
                          V(lambda e: e.tensor_tensor(out=kkS[q2][:], in0=bk[:, 0:128], in1=maskS[:], op=ALU.mult), r=[bk, maskS], w=[kkS[q2]])
                          bk = bank()
                          P(lambda e: e.matmul(bk[:, 0:128], lhsT=gkT[:, hq, :], rhs=gqT[:, hq, :], start=True, stop=True), r=[gkT, gqT], w=[bk])
                          V(lambda e: e.tensor_tensor(out=qkS[q2][:], in0=bk[:, 0:128], in1=Ublk[:], op=ALU.mult), r=[bk, Ublk], w=[qkS[q2]])
                      for i in range(GH):
                          hv = g0 + i
                          G(lambda e: e.tensor_scalar(out=Ld[i][:], in0=Yblk[:], scalar1=gg[:, NGD + hv:NGD + hv + 1], scalar2=1.0,
                                                      op0=ALU.mult, op1=ALU.mult), r=[Yblk, gg], w=[Ld[i]])
                      for i in range(GH):
                          hv = g0 + i
                          bk = bank()
                          P(lambda e: e.matmul(bk[:, 0:128], lhsT=Ld[i][:], rhs=Ublk[:], start=True, stop=True), r=[Ld[i], Ublk], w=[bk])
                          A(lambda e: e.activation(out=decb[i][:], in_=bk[:, 0:128], func=AF.Exp, scale=-1.0), r=[bk], w=[decb[i]])
                          V(lambda e: e.scalar_tensor_tensor(out=Nb[i][0][:], in0=kkS[i // 2][:], scalar=gg[:, NBETA + hv:NBETA + hv + 1], in1=decb[i][:],
                                                             op0=ALU.mult, op1=ALU.mult), r=[kkS[i // 2], gg, decb[i]], w=[Nb[i][0]])
                          G(lambda e: e.tensor_tensor(out=attnT[hv % 8][:], in0=qkS[i // 2][:], in1=decb[i][:], op=ALU.mult), r=[qkS[i // 2], decb[i]], w=[attnT[hv % 8]])
                          G(lambda e: e.tensor_tensor(out=Rf[i][:], in0=Nb[i][0][:], in1=ident[:], op=ALU.add), r=[Nb[i][0], ident], w=[Rf[i]])
                      for i in range(GH):
                          bk = bank()
                          P(lambda e: e.transpose(bk[:, 0:128], Nb[i][0][:], ident[:]), r=[Nb[i][0], ident], w=[bk])
                          A(lambda e: e.activation(out=Mb[i][0][:], in_=bk[:, 0:128], func=AF.Copy), r=[bk], w=[Mb[i][0]])
                      cur = 0
                      for lv in range(5):
                          nxt = 1 - cur
                          for i in range(GH):
                              bk = bank()
                              P(lambda e: e.matmul(bk[:, 0:128], lhsT=Nb[i][cur][:], rhs=Mb[i][cur][:], start=True, stop=True), r=[Nb[i][cur], Mb[i][cur]], w=[bk])
                              A(lambda e: e.activation(out=Mb[i][nxt][:], in_=bk[:, 0:128], func=AF.Copy), r=[bk], w=[Mb[i][nxt]])
                          if lv < 4:
                              for i in range(GH):
                                  bk = bank()
                                  P(lambda e: e.matmul(bk[:, 0:128], lhsT=Mb[i][cur][:], rhs=Nb[i][cur][:], start=True, stop=True), r=[Nb[i][cur], Mb[i][cur]], w=[bk])
                                  V(lambda e: e.tensor_copy(out=Nb[i][nxt][:], in_=bk[:, 0:128]), r=[bk], w=[Nb[i][nxt]])
                          for i in range(GH):
                              bk = bank()
                              P(lambda e: e.matmul(bk[:, 0:128], lhsT=Mb[i][nxt][:], rhs=Rf[i][:], start=True, stop=True), r=[Mb[i][nxt], Rf[i]], w=[bk])
                              V(lambda e: e.tensor_tensor(out=Rf[i][:], in0=Rf[i][:], in1=bk[:, 0:128], op=ALU.add), r=[Rf[i], bk], w=[Rf[i]])
                          cur = nxt
                      for i in range(GH):
                          hv = g0 + i
                          hq = hv // 2
                          kg = kgb[i % 2]
                          G(lambda e: e.tensor_copy(out=Rb[hv % 8][:], in_=Rf[i][:]), r=[Rf[i]], w=[Rb[hv % 8]])
                          V(lambda e: e.tensor_scalar(out=kg[:], in0=k_tm[:, hq, :], scalar1=gg[:, EG + hv:EG + hv + 1], scalar2=None, op0=ALU.mult),
                            r=[k_tm, gg], w=[kg])
                          V(lambda e: e.tensor_scalar(out=kd[hv % 8][:], in0=k_tm[:, hq, :], scalar1=gg[:, EGR + hv:EGR + hv + 1], scalar2=None, op0=ALU.mult),
                            r=[k_tm, gg], w=[kd[hv % 8]])
                          bk = bank()
                          P(lambda e: e.matmul(bk[:, 0:128], lhsT=kg[:], rhs=Rb[hv % 8][:], start=True, stop=True), r=[kg, Rb[hv % 8]], w=[bk])
                          A(lambda e: e.activation(out=negWT[hv % 8][:], in_=bk[:, 0:128], func=AF.Copy, scale=-1.0), r=[bk], w=[negWT[hv % 8]])
                  mark('gdn_rec%d' % half, si == 0 and ti == 2)
                  for c2 in range(2):
                      hs = slice(c2 * 64, c2 * 64 + 64)
                      for hv in range(half * 8, half * 8 + 8):
                          bk = bank()
                          P(lambda e: e.matmul(bk[hs, 0:128], lhsT=Rb[hv % 8][hs, hs], rhs=v_tm[hs, hv, :], start=True, stop=False), r=[Rb[hv % 8], v_tm], w=[bk])
                          P(lambda e: e.matmul(bk[hs, 0:128], lhsT=negWT[hv % 8][:, hs], rhs=S_b[hv][:], start=False, stop=True), r=[negWT[hv % 8], S_b[hv]], w=[bk])
                          A(lambda e: e.activation(out=vnew[hv][hs, :], in_=bk[hs, 0:128], func=AF.Copy, scale=gg[hs, BETA + hv:BETA + hv + 1]),
                            r=[bk, gg], w=[vnew[hv]])
                      for hv in range(half * 8, half * 8 + 8):
                          hq = hv // 2
                          p2 = p2s[hv % 2]
                          bk = bank()
                          P(lambda e: e.matmul(bk[hs, 0:128], lhsT=gqT[:, hq, hs], rhs=S_b[hv][:], start=True, stop=True), r=[gqT, S_b[hv]], w=[bk])
                          P(lambda e: e.matmul(bk[hs, 128:256], lhsT=attnT[hv % 8][hs, hs], rhs=vnew[hv][hs, :], start=True, stop=True), r=[attnT[hv % 8], vnew[hv]], w=[bk])
                          A(lambda e: e.activation(out=p2[hs, :], in_=bk[hs, 128:256], func=AF.Copy), r=[bk], w=[p2])
                          V(lambda e: e.scalar_tensor_tensor(out=o_g[hs, hv * 128:(hv + 1) * 128], in0=bk[hs, 0:128], scalar=gg[hs, EG + hv:EG + hv + 1],
                                                             in1=p2[hs, :], op0=ALU.mult, op1=ALU.add), r=[bk, gg, p2], w=[o_g])
                          bk2 = bank()
                          P(lambda e: e.matmul(bk2[:, 0:128], lhsT=kd[hv % 8][hs, :], rhs=vnew[hv][hs, :], start=True, stop=True), r=[kd[hv % 8], vnew[hv]], w=[bk2])
                          V(lambda e: e.scalar_tensor_tensor(out=S_f[hv][:], in0=S_f[hv][:], scalar=gg[:, ES + c2 * 16 + hv:ES + c2 * 16 + hv + 1],
                                                             in1=bk2[:, 0:128], op0=ALU.mult, op1=ALU.add), r=[S_f[hv], gg, bk2], w=[S_f[hv]])
                          G(lambda e: e.tensor_copy(out=S_b[hv][:], in_=S_f[hv][:]), r=[S_f[hv]], w=[S_b[hv]])
                mark('gdn_norm', si == 0 and ti == 2)
                for hv in range(16):
                    A(lambda e: e.activation(out=junk[:, 0:128], in_=o_g[:, hv * 128:(hv + 1) * 128], func=AF.Square, accum_out=ssq[:, hv:hv + 1]),
                      r=[o_g], w=[junk, ssq])
                V(lambda e: e.tensor_scalar(out=ssq[:, 0:16], in0=ssq[:, 0:16], scalar1=1.0 / 128, scalar2=RMS_EPS, op0=ALU.mult, op1=ALU.add), r=[ssq], w=[ssq])
                G(lambda e: e.tensor_tensor(out=ssq[:, 0:16], in0=ssq[:, 0:16], in1=nhalf[:, 0:16], op=ALU.pow), r=[ssq, nhalf], w=[ssq])
                for hv in range(16):
                    V(lambda e: e.scalar_tensor_tensor(out=o_g[:, hv * 128:(hv + 1) * 128], in0=o_g[:, hv * 128:(hv + 1) * 128],
                                                       scalar=ssq[:, hv:hv + 1], in1=gz_silu[:, hv * 128:(hv + 1) * 128],
                                                       op0=ALU.mult, op1=ALU.mult), r=[o_g, ssq, gz_silu], w=[o_g])
                dbg_out("d_o", o_n[:], o_n, row0, 2048)
                dbg_out("d_kg", k_tm[:].rearrange("p h d -> p (h d)"), k_tm, row0, 1024)
                if stage <= 2:
                    fw.dma('sp', out_d.t[row0:row0 + 128, :], o_n[:, 0:1024], r=[o_n], w=[out_d])
                    continue

                mark('branch_mix', si == 0 and ti == 2)
                transpose_fm(hm_n, 1024, dst_bf=hmT)
                transpose_fm(o_n, 2048, dst_bf=oT)
                for j in range(2):
                    gs = gsig[0]
                    sl = load_slab(w_in_b, 0, 8, 9256 + j * 512, 512)
                    bk = bank()
                    mm_tm(bk[:, 0:512], bk, xnT, sl, 8, 512)
                    A(lambda e: e.activation(out=gs[:], in_=bk[:, 0:512], func=AF.Sigmoid), r=[bk], w=[gs])
                    sl = load_slab(w_br_ml_b, 0, 8, j * 512, 512)
                    bk = bank()
                    mm_tm(bk[:, 0:512], bk, hmT, sl, 8, 512)
                    V(lambda e: e.tensor_tensor(out=merged[:, j * 512:(j + 1) * 512], in0=bk[:, 0:512], in1=gs[:], op=ALU.mult), r=[bk, gs], w=[merged])
                    gs = gsig[1]
                    sl = load_slab(w_in_b, 0, 8, 10280 + j * 512, 512)
                    bk = bank()
                    mm_tm(bk[:, 0:512], bk, xnT, sl, 8, 512)
                    A(lambda e: e.activation(out=gs[:], in_=bk[:, 0:512], func=AF.Sigmoid), r=[bk], w=[gs])
                    bk = bank()
                    for kh in range(2):
                        sl = load_slab(w_br_gdn_b, kh * 8, 8, j * 512, 512)
                        for k in range(8):
                            P(lambda e: e.matmul(bk[:, 0:512], lhsT=oT[:, kh * 8 + k, :], rhs=sl[:, k, 0:512], start=(kh == 0 and k == 0), stop=(kh == 1 and k == 7)),
                              r=[oT, sl], w=[bk])
                    V(lambda e: e.tensor_tensor(out=gs[:], in0=bk[:, 0:512], in1=gs[:], op=ALU.mult), r=[bk, gs], w=[gs])
                    V(lambda e: e.tensor_tensor(out=merged[:, j * 512:(j + 1) * 512], in0=merged[:, j * 512:(j + 1) * 512], in1=gs[:], op=ALU.add), r=[merged, gs], w=[merged])
                transpose_fm(merged, 1024, dst_bf=hmT)
                for j in range(2):
                    sl = load_slab(w_mix_b, 0, 8, j * 512, 512)
                    bk = bank()
                    mm_tm(bk[:, 0:512], bk, hmT, sl, 8, 512)
                    V(lambda e: e.scalar_tensor_tensor(out=h1[:, j * 512:(j + 1) * 512], in0=h0[:, j * 512:(j + 1) * 512], scalar=DN_ALPHA, in1=bk[:, 0:512],
                                                       op0=ALU.mult, op1=ALU.add), r=[h0, bk], w=[h1])
                layernorm(h1, h1, "ln1_g", "ln1_b")
                dbg_out("d_h1", h1[:], h1, row0, D)
                if stage <= 3:
                    fw.dma('sp', out_d.t[row0:row0 + 128, :], h1[:], r=[h1], w=[out_d])
                    continue
                mark('xa', si == 0 and ti == 2)
                import os
                XC = 0
                if XC == 1:
                    fw.dma('sp', out_d.t[row0:row0 + 128, :], h1[:], r=[h1], w=[out_d])
                    continue
                transpose_fm(h1, 1024, dst_bf=xnT)
                for j in range(2):
                    sl = load_slab(xa_wq_b, 0, 8, j * 512, 512)
                    for m in range(4):
                        bk = bank()
                        mm_fm(bk[:, 0:128], bk, xnT, sl, 8, m * 128)
                        A(lambda e: e.activation(out=gqT[:, j * 4 + m, :], in_=bk[:, 0:128], func=AF.Copy, scale=1.0 / 16), r=[bk], w=[gqT])
                if XC == 2:
                    fw.dma('sp', out_d.t[row0:row0 + 128, :], h1[:], r=[h1], w=[out_d])
                    continue
                bkS = [bank(), bank()]
                for hh in range(4):
                    bk = bkS[hh // 2]
                    for c in range(2):
                        P(lambda e: e.matmul(bk[:, (hh % 2) * 256:(hh % 2 + 1) * 256], lhsT=gqT[:, 2 * hh + c, :], rhs=KT[:, 2 * hh + c, :], start=(c == 0), stop=(c == 1)),
                          r=[gqT, KT], w=[bk])
                for i2 in range(2):
                    V(lambda e: e.tensor_reduce(out=smx[:, 2 * i2:2 * i2 + 2], in_=bkS[i2][:, 0:512].rearrange("p (h m) -> p h m", m=256), axis=AX.X, op=ALU.max),
                      r=[bkS[i2]], w=[smx])
                V(lambda e: e.tensor_scalar(out=smx[:, 4:8], in0=smx[:, 0:4], scalar1=-1.0, scalar2=None, op0=ALU.mult), r=[smx], w=[smx])
                for hh in range(4):
                    A(lambda e: e.activation(out=sc_all[:, hh, :], in_=bkS[hh // 2][:, (hh % 2) * 256:(hh % 2 + 1) * 256], func=AF.Exp, bias=smx[:, 4 + hh:5 + hh], scale=1.0,
                                             accum_out=smx[:, 8 + hh:9 + hh]), r=[bkS[hh // 2], smx], w=[sc_all, smx])
                V(lambda e: e.reciprocal(out=smx[:, 12:16], in_=smx[:, 8:12]), r=[smx], w=[smx])
                for hh in range(4):
                    V(lambda e: e.tensor_scalar(out=sc_all[:, hh, :], in0=sc_all[:, hh, :], scalar1=smx[:, 12 + hh:13 + hh], scalar2=None, op0=ALU.mult), r=[sc_all, smx], w=[sc_all])
                for i2 in range(2):
                    bk = bank()
                    for q in range(4):
                        hh = 2 * i2 + q // 2
                        mt = q % 2
                        P(lambda e: e.transpose(bk[:, q * 128:(q + 1) * 128], sc_all[:, hh, mt * 128:(mt + 1) * 128], ident[:]), r=[sc_all, ident], w=[bk])
                    A(lambda e: e.activation(out=pT_all[:, 4 * i2:4 * i2 + 4, :], in_=bk[:, 0:512].rearrange("p (c t) -> p c t", t=128), func=AF.Copy), r=[bk], w=[pT_all])
                for i2 in range(2):
                    bk = bank()
                    for q in range(4):
                        hh = 2 * i2 + q // 2
                        c = q % 2
                        for mt in range(2):
                            P(lambda e: e.matmul(bk[:, q * 128:(q + 1) * 128], lhsT=Vx[:, mt, (2 * hh + c) * 128:(2 * hh + c + 1) * 128], rhs=pT_all[:, 2 * hh + mt, :],
                                                 start=(mt == 0), stop=(mt == 1)), r=[Vx, pT_all], w=[bk])
                    A(lambda e: e.activation(out=gkT[:, 4 * i2:4 * i2 + 4, :], in_=bk[:, 0:512].rearrange("p (c t) -> p c t", t=128), func=AF.Copy), r=[bk], w=[gkT])
                if XC in (3, 4, 5):
                    fw.dma('sp', out_d.t[row0:row0 + 128, :], h1[:], r=[h1], w=[out_d])
                    continue
                for j in range(2):
                    sl = load_slab(xa_wo_b, 0, 8, j * 512, 512)
                    bk = bank()
                    mm_tm(bk[:, 0:512], bk, gkT, sl, 8, 512)
                    V(lambda e: e.scalar_tensor_tensor(out=h2[:, j * 512:(j + 1) * 512], in0=h1[:, j * 512:(j + 1) * 512], scalar=DN_ALPHA, in1=bk[:, 0:512],
                                                       op0=ALU.mult, op1=ALU.add), r=[h1, bk], w=[h2])
                layernorm(h2, h2, "ln2_g", "ln2_b")
                dbg_out("d_h2", h2[:], h2, row0, D)
                if stage <= 4:
                    fw.dma('sp', out_d.t[row0:row0 + 128, :], h2[:], r=[h2], w=[out_d])
                    continue
                mark('router', si == 0 and ti == 2)
                fw.dma('sp', h2_d.t[row0:row0 + 128, :], h2[:], r=[h2], w=[h2_d])
                transpose_fm(h2, 1024, dst_f=xnT_f)
                bk = bank()
                for k in range(8):
                    P(lambda e: e.matmul(bk[:, 0:NE], lhsT=xnT_f[:, k, :], rhs=w_rt[:, k, :], start=(k == 0), stop=(k == 7)), r=[xnT_f, w_rt], w=[bk])
                V(lambda e: e.tensor_tensor(out=rt[:, 0, :], in0=bk[:, 0:NE], in1=b_rt[:], op=ALU.add), r=[bk, b_rt], w=[rt])
                V(lambda e: e.max(out=top8[:, 0:8], in_=rt[:, 0, :]), r=[rt], w=[top8])
                V(lambda e: e.tensor_scalar(out=rt[:, 1, :], in0=rt[:, 0, :], scalar1=top8[:, 3:4], scalar2=None, op0=ALU.is_ge), r=[rt, top8], w=[rt])
                V(lambda e: e.tensor_scalar(out=top8[:, 8:9], in0=top8[:, 0:1], scalar1=-1.0, scalar2=None, op0=ALU.mult), r=[top8], w=[top8])
                A(lambda e: e.activation(out=rt[:, 2, :], in_=rt[:, 0, :], func=AF.Exp, bias=top8[:, 8:9], scale=1.0), r=[rt, top8], w=[rt])
                V(lambda e: e.tensor_tensor(out=rt[:, 2, :], in0=rt[:, 2, :], in1=rt[:, 1, :], op=ALU.mult), r=[rt], w=[rt])
                V(lambda e: e.tensor_reduce(out=top8[:, 9:10], in_=rt[:, 2, :], axis=AX.X, op=ALU.add), r=[rt], w=[top8])
                V(lambda e: e.reciprocal(out=top8[:, 10:11], in_=top8[:, 9:10]), r=[top8], w=[top8])
                V(lambda e: e.tensor_scalar(out=rt[:, 3, :], in0=rt[:, 2, :], scalar1=top8[:, 10:11], scalar2=None, op0=ALU.mult), r=[rt, top8], w=[rt])
                tg_i = row0 // 128
                bk = bank()
                P(lambda e: e.matmul(bk[:, 0:NE], lhsT=UTs[:], rhs=rt[:, 1, :], start=True, stop=True), r=[UTs, rt], w=[bk])
                P(lambda e: e.matmul(bk[:, NE:2 * NE], lhsT=ones_f[:], rhs=rt[:, 1, :], start=True, stop=True), r=[ones_f, rt], w=[bk])
                V(lambda e: e.tensor_tensor(out=rt[:, 4, :], in0=bk[:, 0:NE], in1=base_c[:], op=ALU.add), r=[bk, base_c], w=[rt])
                V(lambda e: e.tensor_tensor(out=base_c[:], in0=bk[:, NE:2 * NE], in1=base_c[:], op=ALU.add), r=[bk, base_c], w=[base_c])
                V(lambda e: e.tensor_scalar(out=rt[:, 5, :], in0=rt[:, 4, :], scalar1=CAP - 0.5, scalar2=None, op0=ALU.is_lt), r=[rt], w=[rt])
                V(lambda e: e.tensor_tensor(out=rt[:, 5, :], in0=rt[:, 5, :], in1=rt[:, 1, :], op=ALU.mult), r=[rt], w=[rt])
                V(lambda e: e.tensor_tensor(out=rt[:, 4, :], in0=rt[:, 4, :], in1=eoff[:], op=ALU.add), r=[rt, eoff], w=[rt])
                V(lambda e: e.tensor_scalar(out=rt[:, 4, :], in0=rt[:, 4, :], scalar1=-1.0, scalar2=CBIG + 1.0, op0=ALU.mult, op1=ALU.add), r=[rt], w=[rt])
                V(lambda e: e.tensor_tensor(out=rt[:, 6, :], in0=rt[:, 4, :], in1=rt[:, 5, :], op=ALU.mult), r=[rt], w=[rt])
                V(lambda e: e.max(out=top8[:, 12:20], in_=rt[:, 6, :]), r=[rt], w=[top8])
                V(lambda e: e.tensor_scalar(out=top8[:, 20:24], in0=top8[:, 12:16], scalar1=-1.0, scalar2=CBIG + 1.0, op0=ALU.mult, op1=ALU.add), r=[top8], w=[top8])
                V(lambda e: e.tensor_copy(out=slot_i[:, tg_i, :], in_=top8[:, 20:24]), r=[top8], w=[slot_i])
                for k4 in range(4):
                    V(lambda e: e.scalar_tensor_tensor(out=rt[:, 7, :], in0=rt[:, 6, :], scalar=top8[:, 12 + k4:13 + k4], in1=rt[:, 3, :],
                                                       op0=ALU.is_equal, op1=ALU.mult), r=[rt, top8], w=[rt])
                    V(lambda e: e.tensor_reduce(out=gwk[:, tg_i, k4:k4 + 1], in_=rt[:, 7, :], axis=AX.X, op=ALU.add), r=[rt], w=[gwk])
                for k4 in range(4):
                    fw.dma('pool', None, None, r=[h2, slot_i], w=[xg_d],
                           fn=lambda e: e.indirect_dma_start(out=xg_d.t, out_offset=bass.IndirectOffsetOnAxis(ap=slot_i[:, tg_i, k4:k4 + 1], axis=0),
                                                             in_=h2[:], in_offset=None, bounds_check=bc_reg, oob_is_err=False))

        mark(None)
        if stage > 4:
            if "d_cnt" in dbg_d:
                fw.dma('sp', dbg_d["d_cnt"].t[0:128, 0:NE], base_c[:], r=[base_c], w=[dbg_d["d_cnt"]])
            fw.barrier()
            es1.close()
            fw.es = es
            for n in vec_late:
                vec_t[n] = fw.sb("c_" + n, [128, D], F32)
                fw.dma('sp', vec_t[n][:], vec_d[n].t.partition_broadcast(128), r=[vec_d[n]], w=[vec_t[n]])
            es2 = es.enter_context(ExitStack())
            fw.es = es2
            wg = [fw.sb(f"wg{i}", [128, 8, 1024], BF16) for i in range(2)]
            wu = [fw.sb(f"wu{i}", [128, 8, 1024], BF16) for i in range(2)]
            wdn = [fw.sb(f"wdn{i}", [128, 8, 1024], BF16) for i in range(2)]
            bgu_tm = [fw.sb(f"bgu_tm{i}", [16, 128], F32) for i in range(2)]
            bgu = [fw.sb(f"bgu{i}", [128, 16], F32) for i in range(2)]
            bdn_bc = [fw.sb(f"bdn_bc{i}", [128, D], F32) for i in range(2)]
            NXR = max(2, CAP // 128)
            xr = [fw.sb(f"xr{i}", [128, D], F32) for i in range(NXR)]
            xgT = fw.sb("xgT", [128, 8, CAP], BF16)
            actT = fw.sb("actT", [128, 8, CAP], BF16)
            tgb = [fw.sb(f"tg{i}", [128, 512], F32) for i in range(2)]
            tsb = [fw.sb(f"ts{i}", [128, 512], F32) for i in range(2)]
            tub = [fw.sb(f"tu{i}", [128, 512], F32) for i in range(2)]
            ysb = [fw.sb(f"ysb{i}", [128, D], F32) for i in range(2)]
            nchunks = [(n0, min(512, CAP - n0)) for n0 in range(0, CAP, 512)]

            def load_expert(ex):
                st = ex % 2
                wg_v = w_gu_d.t[ex].rearrange("(kc p) n -> p kc n", p=128)
                wd_v = w_dn_d.t[ex].rearrange("(kc p) n -> p kc n", p=128)
                for j in range(2):
                    fw.dma('pool', wg[st][:, :, j * 512:(j + 1) * 512], wg_v[:, :, j * 512:(j + 1) * 512], r=[w_gu_d], w=[wg[st]])
                fw.dma('sp', bgu_tm[st][:], b_gu_d.t[ex].rearrange("(c p) -> c p", p=128), r=[b_gu_d], w=[bgu_tm[st]])
                fw.dma('sp', bdn_bc[st][:], b_dn_d.t[ex:ex + 1, :].partition_broadcast(128), r=[b_dn_d], w=[bdn_bc[st]])

            wst = [fw.sb(f"wst{i}", [128, 4, 512], F32) for i in range(2)]

            def wdn_piece(ex, j):
                kh, ch = (j % 4) // 2, j % 2
                if j < 4:
                    src = w_dn_d.t[ex].rearrange("(kc p) n -> p kc n", p=128)[:, kh * 4:(kh + 1) * 4, ch * 512:(ch + 1) * 512]
                    return src, wdn[ex % 2][:, kh * 4:(kh + 1) * 4, ch * 512:(ch + 1) * 512], w_dn_d, wdn[ex % 2]
                src = w_gu_d.t[ex].rearrange("(kc p) n -> p kc n", p=128)[:, kh * 4:(kh + 1) * 4, 1024 + ch * 512:1024 + (ch + 1) * 512]
                return src, wu[ex % 2][:, kh * 4:(kh + 1) * 4, ch * 512:(ch + 1) * 512], w_gu_d, wu[ex % 2]

            def wdn_load(ex, j):
                src, _, sb_, _ = wdn_piece(ex, j)
                fw.dma('sp', wst[j % 2][:], src, r=[sb_], w=[wst[j % 2]])

            def wdn_cast(ex, j):
                _, dst, _, db_ = wdn_piece(ex, j)
                A(lambda e: e.activation(out=dst, in_=wst[j % 2][:], func=AF.Copy), r=[wst[j % 2]], w=[db_])

            def load_x(ex):
                for i in range(CAP // 128):
                    xx = xr[i % NXR]
                    fw.dma('sp', xx[:], xg_d.t[ex * CAP + i * 128: ex * CAP + (i + 1) * 128, :], r=[xg_d], w=[xx])

            load_expert(0)
            load_x(0)
            for j in range(8):
                wdn_load(0, j)
                wdn_cast(0, j)
            for ex in range(NE):
                st = ex % 2
                if ex + 1 < NE:
                    load_expert(ex + 1)
                bk = bank()
                P(lambda e: e.transpose(bk[:, 0:16], bgu_tm[st][0:16, :], ident[0:16, 0:16]), r=[bgu_tm[st], ident], w=[bk])
                V(lambda e: e.tensor_copy(out=bgu[st][:], in_=bk[:, 0:16]), r=[bk], w=[bgu[st]])
                for i in range(CAP // 128):
                    xx = xr[i % NXR]
                    for c0 in range(0, 8, 4):
                        bk = bank()
                        for c in range(4):
                            P(lambda e: e.transpose(bk[:, c * 128:(c + 1) * 128], xx[:, (c0 + c) * 128:(c0 + c + 1) * 128], ident[:]), r=[xx, ident], w=[bk])
                        pv = bk[:, 0:512].rearrange("p (c t) -> p c t", t=128)
                        if c0 == 0:
                            A(lambda e: e.activation(out=xgT[:, c0:c0 + 4, i * 128:(i + 1) * 128], in_=pv, func=AF.Copy), r=[bk], w=[xgT])
                        else:
                            V(lambda e: e.tensor_copy(out=xgT[:, c0:c0 + 4, i * 128:(i + 1) * 128], in_=pv), r=[bk], w=[xgT])
                if ex + 1 < NE:
                    load_x(ex + 1)
                    wdn_load(ex + 1, 0)
                    wdn_load(ex + 1, 1)
                for f in range(8):
                    if ex + 1 < NE:
                        wdn_cast(ex + 1, f)
                        if f + 2 < 8:
                            wdn_load(ex + 1, f + 2)
                    for ci, (n0, nn) in enumerate(nchunks):
                        tg = tgb[ci % 2]; ts = tsb[ci % 2]; tu = tub[ci % 2]
                        bkg = bank()
                        for k in range(8):
                            P(lambda e: e.matmul(bkg[:, 0:nn], lhsT=wg[st][:, k, f * 128:(f + 1) * 128], rhs=xgT[:, k, n0:n0 + nn], start=(k == 0), stop=(k == 7)),
                              r=[wg[st], xgT], w=[bkg])
                        bku = bank()
                        for k in range(8):
                            P(lambda e: e.matmul(bku[:, 0:nn], lhsT=wu[st][:, k, f * 128:(f + 1) * 128], rhs=xgT[:, k, n0:n0 + nn], start=(k == 0), stop=(k == 7)),
                              r=[wu[st], xgT], w=[bku])
                        V(lambda e: e.tensor_scalar(out=tg[:, 0:nn], in0=bkg[:, 0:nn], scalar1=bgu[st][:, f:f + 1], scalar2=7.0, op0=ALU.add, op1=ALU.min), r=[bkg, bgu[st]], w=[tg])
                        A(lambda e: e.activation(out=ts[:, 0:nn], in_=tg[:, 0:nn], func=AF.Sigmoid, scale=1.702), r=[tg], w=[ts])
                        G(lambda e: e.tensor_tensor(out=tg[:, 0:nn], in0=tg[:, 0:nn], in1=ts[:, 0:nn], op=ALU.mult), r=[tg, ts], w=[tg])
                        V(lambda e: e.tensor_scalar(out=tu[:, 0:nn], in0=bku[:, 0:nn], scalar1=bgu[st][:, 8 + f:9 + f], scalar2=7.0, op0=ALU.add, op1=ALU.min), r=[bku, bgu[st]], w=[tu])
                        V(lambda e: e.tensor_scalar(out=tu[:, 0:nn], in0=tu[:, 0:nn], scalar1=-7.0, scalar2=1.0, op0=ALU.max, op1=ALU.add), r=[tu], w=[tu])
                        G(lambda e: e.tensor_tensor(out=actT[:, f, n0:n0 + nn], in0=tu[:, 0:nn], in1=tg[:, 0:nn], op=ALU.mult), r=[tu, tg], w=[actT])
                for i in range(CAP // 128):
                    ys = ysb[i % 2]
                    for half in range(2):
                        bk = bank()
                        for k in range(8):
                            P(lambda e: e.matmul(bk[:, 0:512], lhsT=actT[:, k, i * 128:(i + 1) * 128], rhs=wdn[st][:, k, half * 512:(half + 1) * 512], start=(k == 0), stop=(k == 7)),
                              r=[actT, wdn[st]], w=[bk])
                        V(lambda e: e.tensor_tensor(out=ys[:, half * 512:(half + 1) * 512], in0=bk[:, 0:512], in1=bdn_bc[st][:, half * 512:(half + 1) * 512], op=ALU.add),
                          r=[bk, bdn_bc[st]], w=[ys])
                    fw.dma('sp', yg_d.t[ex * CAP + i * 128: ex * CAP + (i + 1) * 128, :], ys[:], r=[ys], w=[yg_d])
            fw.barrier()
            es2.close()
            fw.es = es
            fh = [fw.sb(f"fh{i}", [128, D], F32) for i in range(2)]
            gb = [[fw.sb(f"gb{i}_{k}", [128, D], F32) for k in range(4)] for i in range(2)]
            for t in range(T // 128):
                hh2 = fh[t % 2]
                fw.dma('sp', hh2[:], h2_d.t[t * 128:(t + 1) * 128, :], r=[h2_d], w=[hh2])
                for k4 in range(4):
                    gk = gb[t % 2][k4]
                    V(lambda e: e.memset(gk[:], 0.0), w=[gk])
                    fw.dma('pool', None, None, r=[yg_d, slot_i], w=[gk],
                           fn=lambda e: e.indirect_dma_start(out=gk[:], out_offset=None, in_=yg_d.t,
                                                             in_offset=bass.IndirectOffsetOnAxis(ap=slot_i[:, t, k4:k4 + 1], axis=0),
                                                             bounds_check=bc_reg, oob_is_err=False))
                V(lambda e: e.tensor_scalar(out=hh2[:], in0=hh2[:], scalar1=DN_ALPHA, scalar2=None, op0=ALU.mult), r=[hh2], w=[hh2])
                for k4 in range(4):
                    gk = gb[t % 2][k4]
                    V(lambda e: e.scalar_tensor_tensor(out=hh2[:], in0=gk[:], scalar=gwk[:, t, k4:k4 + 1], in1=hh2[:], op0=ALU.mult, op1=ALU.add),
                      r=[gk, gwk, hh2], w=[hh2])
                layernorm(hh2, hh2, "ln3_g", "ln3_b")
                fw.dma('sp', out_d.t[t * 128:(t + 1) * 128, :], hh2[:], r=[hh2], w=[out_d])

        fw.finish([out_d] + list(dbg_d.values()))
        es1.close()
        print("instructions", fw.n_inst, "waits", fw.n_wait, "sems", fw.nsem)
    return nc


_IN_NAMES = ["x", "mem", "ln_in_g", "ln_in_b", "w_in", "ml_gate_bias", "ml_norm_g", "gdn_conv_w", "gdn_a_log",
             "gdn_dt_bias", "gdn_norm_g", "w_branch_ml", "w_branch_gdn", "w_mix_out", "ln1_g", "ln1_b",
             "xa_wq", "xa_wk", "xa_wv", "xa_wo", "ln2_g", "ln2_b", "w_router", "b_router", "w_gu", "b_gu",
             "w_dn", "b_dn", "ln3_g", "ln3_b"]


def make_in_map(inputs, b0, n_seq, S):
    f = lambda a: np.ascontiguousarray(np.asarray(a, dtype=np.float32))
    m = {}
    m["x"] = f(inputs["x"][b0:b0 + n_seq, :S]).reshape(n_seq * S, D)
    m["mem"] = f(inputs["mem"][b0:b0 + n_seq]).reshape(n_seq * MEM, D)
    for n in ["ln_in_g", "ln_in_b"]:
        m[n] = f(inputs[n]).reshape(1, D)
    for n in ["ml_norm_g", "ln1_g", "ln1_b", "ln2_g", "ln2_b", "ln3_g", "ln3_b"]:
        m[n] = f(inputs[n]).reshape(1, D)
    m["w_in"] = f(inputs["w_in"]).reshape(D, IN_W)
    m["ml_gate_bias"] = f(inputs["ml_gate_bias"]).reshape(1, 8)
    m["gdn_conv_w"] = f(inputs["gdn_conv_w"]).reshape(4, 4096)
    m["gdn_a_log"] = f(inputs["gdn_a_log"]).reshape(1, 16)
    m["gdn_dt_bias"] = f(inputs["gdn_dt_bias"]).reshape(1, 16)
    m["gdn_norm_g"] = f(inputs["gdn_norm_g"]).reshape(1, 128)
    m["w_branch_ml"] = f(inputs["w_branch_ml"]).reshape(D, D)
    m["w_branch_gdn"] = f(inputs["w_branch_gdn"]).reshape(2048, D)
    for n, k in [("w_mix_out", "w_mix_out"), ("xa_wq", "xa_wq"), ("xa_wk", "xa_wk"), ("xa_wv", "xa_wv"), ("xa_wo", "xa_wo")]:
        m[n] = f(inputs[k]).reshape(D, D)
    m["w_router"] = f(inputs["w_router"]).reshape(D, NE)
    m["b_router"] = f(inputs["b_router"]).reshape(1, NE)
    m["w_gu"] = f(inputs["w_gu"]).reshape(NE, D, 2 * D)
    m["b_gu"] = f(inputs["b_gu"]).reshape(NE, 2 * D)
    m["w_dn"] = f(inputs["w_dn"]).reshape(NE, D, D)
    m["b_dn"] = f(inputs["b_dn"]).reshape(NE, D)
    return m


def kernel(**inputs):
    n_cores = 8
    n_seq, S = 2, 2048
    nc = build_nc(n_seq, S)
    in_maps = [make_in_map(inputs, c * n_seq, n_seq, S) for c in range(n_cores)]
    res = run_bass_kernel_spmd(nc, in_maps, core_ids=list(range(n_cores)))
    outs = [np.asarray(r["out"], dtype=np.float32).reshape(n_seq, S, D) for r in res.results]
    return np.concatenate(outs, axis=0)
```

```python
import math
import numpy as np
from contextlib import ExitStack
import concourse.bass as bass
import concourse.mybir as mybir
from concourse.bass_utils import run_bass_kernel_spmd

F32 = mybir.dt.float32
BF16 = mybir.dt.bfloat16
I32 = mybir.dt.int32
AF = mybir.ActivationFunctionType
ALU = mybir.AluOpType
AX = mybir.AxisListType

D = 1024
IN_W = 11304
LN_EPS = 1e-5
RMS_EPS = 1e-6
DN_ALPHA = 2 ** 0.25
MEM = 256
NE = 32


class Buf:
    def __init__(self, t, name, parent=None):
        self.t = t
        self.name = name
        self._p = parent
        self._w = None
        self._r = []
        self.excl = False
        self.small = False

    @property
    def w(self):
        return self._p.w if self._p is not None else self._w

    @w.setter
    def w(self, v):
        if self._p is not None:
            self._p.w = v
        else:
            self._w = v

    @property
    def r(self):
        return self._p.r if self._p is not None else self._r

    @r.setter
    def r(self, v):
        if self._p is not None:
            self._p.r = v
        else:
            self._r = v

    def __getitem__(self, k):
        return self.t[k]


class FW:
    SEM_LIMIT = 30000

    def __init__(self, nc, es):
        self.nc = nc
        self.es = es
        self.es_sem = es
        self.eng = {'pe': nc.tensor, 'act': nc.scalar, 'dve': nc.vector, 'pool': nc.gpsimd, 'sp': nc.sync}
        self.esem = {}
        self.ecnt = {}
        self.known = {k: {} for k in self.eng}
        self.nsem = 0
        self.sem_owner = {}
        import os as _o
        self.force_small = False
        import os
        for k in self.eng:
            self._new_esem(k)
        self.dma_pool = [[self._sem(), 0] for _ in range(20)]
        self.dma_rr = 0
        self.n_inst = 0
        self.n_wait = 0

    def _sem(self):
        self.nsem += 1
        return self.es_sem.enter_context(self.nc.semaphore(f"sm{self.nsem}"))

    def _new_esem(self, k):
        self.esem[k] = self._sem()
        self.ecnt[k] = 0
        self.sem_owner[id(self.esem[k])] = k

    def sb(self, name, shape, dt, es=None):
        b = Buf((es or self.es).enter_context(self.nc.sbuf_tensor(name, shape, dt)), name)
        fs = 1
        for d in shape[1:]:
            fs *= d
        b.small = fs <= 256
        return b

    def ps(self, name, shape, dt, es=None):
        b = Buf((es or self.es).enter_context(self.nc.psum_tensor(name, shape, dt)), name)
        b.excl = True
        return b

    def _wait(self, ek, tok, small=True):
        if tok is None:
            return
        sem, val = tok
        key = id(sem)
        if self.sem_owner.get(key) == ek and (ek == 'pe' or (not small and not self.force_small)):
            return
        kn = self.known[ek]
        if kn.get(key, 0) >= val:
            return
        kn[key] = val
        self.eng[ek].wait_ge(sem, val)
        self.n_wait += 1

    def _deps(self, ek, r, w):
        for b in r:
            sm = b.small or (b._p is not None and b._p.small)
            self._wait(ek, b.w, sm)
            if b.excl:
                for t in b.r:
                    self._wait(ek, t, sm)
        for b in w:
            sm = b.small or (b._p is not None and b._p.small)
            self._wait(ek, b.w, sm)
            for t in b.r:
                self._wait(ek, t, sm)

    def _commit(self, tok, r, w):
        for b in r:
            if len(b.r) > 12:
                b.r = b.r[-12:] if False else b.r
            b.r.append(tok)
        for b in w:
            b.w = tok
            b.r = []

    def op(self, ek, fn, r=(), w=()):
        self._deps(ek, r, w)
        if self.ecnt[ek] >= self.SEM_LIMIT:
            self._new_esem(ek)
        ins = fn(self.eng[ek])
        self.ecnt[ek] += 1
        tok = (self.esem[ek], self.ecnt[ek])
        ins.then_inc(self.esem[ek], 1)
        self._commit(tok, r, w)
        self.n_inst += 1
        return ins

    def dma(self, ek, out, in_, r=(), w=(), fn=None, **kw):
        self._deps(ek, r, w)
        slot = self.dma_pool[self.dma_rr]
        self.dma_rr = (self.dma_rr + 1) % len(self.dma_pool)
        sem, cnt = slot
        if cnt > 0:
            self._wait(ek, (sem, cnt))
        if fn is not None:
            ins = fn(self.eng[ek])
        else:
            ins = self.eng[ek].dma_start(out=out, in_=in_, **kw)
        cnt += 16
        slot[1] = cnt
        ins.then_inc(sem, 16)
        tok = (sem, cnt)
        self._commit(tok, r, w)
        self.n_inst += 1
        return tok

    def barrier(self):
        toks = [(self.esem[k], self.ecnt[k]) for k in self.eng if self.ecnt[k] > 0]
        toks += [(sem, cnt) for sem, cnt in self.dma_pool if cnt > 0]
        for ek in self.eng:
            for t in toks:
                self._wait(ek, t)

    def finish(self, bufs):
        for b in bufs:
            self._wait('sp', b.w)


def build_nc(n_seq=2, S=2048, stage=99, dbg=()):
    nc = bass.Bass("TRN2", target_bir_lowering=False)
    T = n_seq * S
    NSUB = S // 128
    lnscale = math.log(128 ** -0.5)

    def din(name, shape, dt=F32):
        return Buf(nc.dram_tensor(name, list(shape), dt, kind="ExternalInput").ap(), name)

    def dscr(name, shape, dt):
        return Buf(nc.dram_tensor(name, list(shape), dt, kind="Internal").ap(), name)

    x_d = din("x", [T, D])
    mem_d = din("mem", [n_seq * MEM, D])
    vec_names = ["ln_in_g", "ln_in_b", "ln1_g", "ln1_b", "ln2_g", "ln2_b"]
    vec_late = ["ln3_g", "ln3_b"]
    vec_d = {n: din(n, [1, D]) for n in vec_names + vec_late}
    w_in_d = din("w_in", [D, IN_W])
    ml_gate_bias_d = din("ml_gate_bias", [1, 8])
    conv_w_d = din("gdn_conv_w", [4, 4096])
    a_log_d = din("gdn_a_log", [1, 16])
    dt_bias_d = din("gdn_dt_bias", [1, 16])
    gdn_norm_g_d = din("gdn_norm_g", [1, 128])
    w_br_ml_d = din("w_branch_ml", [D, D])
    w_br_gdn_d = din("w_branch_gdn", [2048, D])
    w_mix_d = din("w_mix_out", [D, D])
    xa_wq_d = din("xa_wq", [D, D])
    xa_wk_d = din("xa_wk", [D, D])
    xa_wv_d = din("xa_wv", [D, D])
    xa_wo_d = din("xa_wo", [D, D])
    w_router_d = din("w_router", [D, NE])
    b_router_d = din("b_router", [1, NE])
    w_gu_d = din("w_gu", [NE, D, 2 * D])
    b_gu_d = din("b_gu", [NE, 2 * D])
    w_dn_d = din("w_dn", [NE, D, D])
    b_dn_d = din("b_dn", [NE, D])
    out_d = Buf(nc.dram_tensor("out", [T, D], F32, kind="ExternalOutput").ap(), "out")
    dbg_d = {}
    for nm, shape in dbg:
        dbg_d[nm] = Buf(nc.dram_tensor(nm, list(shape), F32, kind="ExternalOutput").ap(), nm)

    w_in_b = dscr("w_in_b", [D, IN_W], BF16)
    w_br_ml_b = dscr("w_br_ml_b", [D, D], BF16)
    w_br_gdn_b = dscr("w_br_gdn_b", [2048, D], BF16)
    w_mix_b = dscr("w_mix_b", [D, D], BF16)
    xa_wq_b = dscr("xa_wq_b", [D, D], BF16)
    xa_wk_b = dscr("xa_wk_b", [D, D], BF16)
    xa_wv_b = dscr("xa_wv_b", [D, D], BF16)
    xa_wo_b = dscr("xa_wo_b", [D, D], BF16)

    h2_d = dscr("h2_d", [T, D], F32)
    h2T_d = dscr("h2T_d", [T // 128, 128, 8, 128], BF16)
    CAP = 640 if T >= 4096 else 128
    NSLOT = NE * CAP
    CBIG = 65535.0
    xg_d = dscr("xg_d", [NSLOT, D], F32)
    yg_d = dscr("yg_d", [NSLOT, D], F32)

    with ExitStack() as es:
        fw = FW(nc, es)
        es1 = es.enter_context(ExitStack())
        V = lambda fn, r=(), w=(): fw.op('dve', fn, r, w)
        A = lambda fn, r=(), w=(): fw.op('act', fn, r, w)
        G = lambda fn, r=(), w=(): fw.op('pool', fn, r, w)
        P = lambda fn, r=(), w=(): fw.op('pe', fn, r, w)

        ones_f = fw.sb("ones_f", [128, 128], F32)
        ident = fw.sb("ident", [128, 128], F32)
        UT = fw.sb("UT", [128, 128], F32)
        G(lambda e: e.memset(ones_f[:], 1.0), w=[ones_f])
        G(lambda e: e.affine_select(out=ident[:], in_=ones_f[:], pattern=[[-1, 128]], compare_op=ALU.is_equal,
                                    fill=0.0, base=0, channel_multiplier=1), r=[ones_f], w=[ident])
        G(lambda e: e.affine_select(out=UT[:], in_=ones_f[:], pattern=[[1, 128]], compare_op=ALU.is_ge,
                                    fill=0.0, base=0, channel_multiplier=-1), r=[ones_f], w=[UT])

        nhalf = fw.sb("nhalf", [128, 128], F32)
        G(lambda e: e.memset(nhalf[:], -0.5), w=[nhalf])
        st6 = fw.sb("st6", [128, 2, 6], F32)
        mv2 = fw.sb("mv2", [128, 2], F32)
        rstd1 = fw.sb("rstd1", [128, 1], F32)
        eps_t = fw.sb("eps_t", [128, 4], F32)
        NT_ALL = T // 128
        bc_reg = nc.gpsimd.to_reg(NSLOT - 1)
        slot_i = fw.sb("slot_i", [128, NT_ALL, 4], I32)
        gwk = fw.sb("gwk", [128, NT_ALL, 4], F32)
        fw.es = es1

        vec_t = {}
        for n in vec_names:
            vec_t[n] = fw.sb("c_" + n, [128, D], F32, es=es1)
            fw.dma('sp', vec_t[n][:], vec_d[n].t.partition_broadcast(128), r=[vec_d[n]], w=[vec_t[n]])
        mlb_t = fw.sb("mlb_t", [128, 8], F32)
        fw.dma('sp', mlb_t[:], ml_gate_bias_d.t.partition_broadcast(128), r=[ml_gate_bias_d], w=[mlb_t])
        w_small = fw.sb("w_small", [128, 8, 40], F32)
        wv = w_in_d.t.rearrange("(kc p) n -> p kc n", p=128)
        with nc.allow_non_contiguous_dma(reason="small gate columns"):
            fw.dma('sp', w_small[:, :, 0:8], wv[:, :, 3072:3080], r=[w_in_d], w=[w_small])
            fw.dma('sp', w_small[:, :, 8:40], wv[:, :, 9224:9256], r=[w_in_d], w=[w_small])

        banks = [fw.ps(f"bank{i}", [128, 512], F32) for i in range(8)]
        bank_rr = [0]

        def bank():
            b = banks[bank_rr[0]]
            bank_rr[0] = (bank_rr[0] + 1) % 8
            return b

        slabs = [fw.sb(f"slab{i}", [128, 8, 512], BF16, es=es1) for i in range(4)]
        cast_tmp = slabs
        cast_rr = [0]

        w_src = {"w_in_b": w_in_d, "w_br_ml_b": w_br_ml_d, "w_br_gdn_b": w_br_gdn_d, "w_mix_b": w_mix_d,
                 "xa_wq_b": xa_wq_d, "xa_wk_b": xa_wk_d, "xa_wv_b": xa_wv_d, "xa_wo_b": xa_wo_d}
        slab_keys = [("w_in_b", 0, c) for c in ([0, 512, 1024, 1536, 2048, 2560] + [3080 + 512 * j for j in range(8)]
                                               + [7176 + 512 * j for j in range(4)] + [9256, 9768, 10280, 10792])]
        if stage >= 3:
            slab_keys += [("w_br_ml_b", 0, 0), ("w_br_ml_b", 0, 512)]
            slab_keys += [("w_br_gdn_b", k0, c) for c in (0, 512) for k0 in (0, 8)]
            for nm in ["w_mix_b", "xa_wq_b", "xa_wk_b", "xa_wv_b", "xa_wo_b"]:
                slab_keys += [(nm, 0, 0), (nm, 0, 512)]
        slab_idx = {k: i for i, k in enumerate(slab_keys)}
        wslab_d = dscr("wslab_d", [len(slab_keys), 128, 4096], BF16)
        wslab_b = [Buf(wslab_d.t[i], f"wslab{i}") for i in range(len(slab_keys))]
        g8_tm = fw.sb("g8_tm", [8, 128], F32, es=es1)
        g1_tm = fw.sb("g1_tm", [1, 128], F32, es=es1)
        gfm_ml = fw.sb("gfm_ml", [128, 8], F32, es=es1)
        gfm_gdn = fw.sb("gfm_gdn", [128, 1], F32, es=es1)
        ml_norm_g_d = din("ml_norm_g", [1, D])
        fw.dma('sp', g8_tm[:], ml_norm_g_d.t.rearrange("o (c p) -> (o c) p", p=128), r=[ml_norm_g_d], w=[g8_tm])
        fw.dma('sp', g1_tm[:], gdn_norm_g_d.t, r=[gdn_norm_g_d], w=[g1_tm])
        bkq = bank()
        P(lambda e: e.transpose(bkq[:, 0:8], g8_tm[0:8, :], ident[0:8, 0:8]), r=[g8_tm, ident], w=[bkq])
        P(lambda e: e.transpose(bkq[:, 8:9], g1_tm[0:1, :], ident[0:1, 0:1]), r=[g1_tm, ident], w=[bkq])
        V(lambda e: e.tensor_copy(out=gfm_ml[:], in_=bkq[:, 0:8]), r=[bkq], w=[gfm_ml])
        V(lambda e: e.tensor_copy(out=gfm_gdn[:], in_=bkq[:, 8:9]), r=[bkq], w=[gfm_gdn])
        for (nm, k0, c0) in slab_keys:
            src = w_src[nm]
            tmp = cast_tmp[cast_rr[0]]
            cast_rr[0] = (cast_rr[0] + 1) % len(cast_tmp)
            sv = src.t[k0 * 128:(k0 + 8) * 128, c0:c0 + 512].rearrange("(kc p) n -> p kc n", p=128)
            fw.dma('pool', tmp[:], sv, r=[src], w=[tmp])
            if nm == "w_br_ml_b":
                for kc in range(8):
                    V(lambda e: e.tensor_scalar(out=tmp[:, kc, :], in0=tmp[:, kc, :], scalar1=gfm_ml[:, kc:kc + 1], scalar2=None, op0=ALU.mult),
                      r=[tmp, gfm_ml], w=[tmp])
            elif nm == "w_br_gdn_b":
                V(lambda e: e.tensor_scalar(out=tmp[:].rearrange("p a b -> p (a b)"), in0=tmp[:].rearrange("p a b -> p (a b)"),
                                            scalar1=gfm_gdn[:, 0:1], scalar2=None, op0=ALU.mult), r=[tmp, gfm_gdn], w=[tmp])
            fw.dma('sp', wslab_d.t[slab_idx[(nm, k0, c0)]], tmp[:].rearrange("p a b -> p (a b)"), r=[tmp], w=[wslab_b[slab_idx[(nm, k0, c0)]]])

        slab_rr = [0]

        def load_slab(wb, k0, kc, c0, w):
            sl = slabs[slab_rr[0]]
            slab_rr[0] = (slab_rr[0] + 1) % len(slabs)
            fw.dma('sp', sl[:].rearrange("p a b -> p (a b)"), wslab_d.t[slab_idx[(wb.name, k0, c0)]], r=[wslab_b[slab_idx[(wb.name, k0, c0)]]], w=[sl])
            return sl


        def layernorm(src, dst, gname, bname):
            for hf in range(2):
                V(lambda e: e.bn_stats(out=st6[:, hf, :], in_=src[:, hf * 512:(hf + 1) * 512]), r=[src], w=[st6])
            V(lambda e: e.bn_aggr(out=mv2[:], in_=st6[:].rearrange("p a b -> p (a b)")), r=[st6], w=[mv2])
            V(lambda e: e.tensor_scalar(out=rstd1[:], in0=mv2[:, 1:2], scalar1=LN_EPS, scalar2=None, op0=ALU.add), r=[mv2], w=[rstd1])
            G(lambda e: e.tensor_tensor(out=rstd1[:], in0=rstd1[:], in1=nhalf[:, 0:1], op=ALU.pow), r=[rstd1, nhalf], w=[rstd1])
            V(lambda e: e.tensor_scalar(out=dst[:], in0=src[:], scalar1=mv2[:, 0:1], scalar2=rstd1[:, 0:1],
                                        op0=ALU.subtract, op1=ALU.mult), r=[src, mv2, rstd1], w=[dst])
            V(lambda e: e.tensor_tensor(out=dst[:], in0=dst[:], in1=vec_t[gname][:], op=ALU.mult), r=[dst, vec_t[gname]], w=[dst])
            V(lambda e: e.tensor_tensor(out=dst[:], in0=dst[:], in1=vec_t[bname][:], op=ALU.add), r=[dst, vec_t[bname]], w=[dst])

        G(lambda e: e.memset(eps_t[:, 0:1], LN_EPS), w=[eps_t])
        G(lambda e: e.memset(eps_t[:, 1:2], RMS_EPS), w=[eps_t])
        G(lambda e: e.memset(eps_t[:, 2:3], lnscale), w=[eps_t])
        G(lambda e: e.memset(eps_t[:, 3:4], 1.0), w=[eps_t])


        def transpose_fm(src, ncol, dst_bf=None, dst_f=None):
            nch = ncol // 128
            for c0 in range(0, nch, 4):
                n = min(4, nch - c0)
                bk = bank()
                for c in range(n):
                    P(lambda e: e.transpose(bk[:, c * 128:(c + 1) * 128], src[:, (c0 + c) * 128:(c0 + c + 1) * 128], ident[:]),
                      r=[src, ident], w=[bk])
                pv = bk[:, 0:n * 128].rearrange("p (c t) -> p c t", t=128)
                if dst_bf is not None:
                    A(lambda e: e.activation(out=dst_bf[:, c0:c0 + n, :], in_=pv, func=AF.Copy), r=[bk], w=[dst_bf])
                if dst_f is not None:
                    V(lambda e: e.tensor_copy(out=dst_f[:, c0:c0 + n, :], in_=pv), r=[bk], w=[dst_f])

        def mm_tm(ps_ap, psb, xT, sl, kc, w, k_off=0):
            for k in range(kc):
                P(lambda e: e.matmul(ps_ap, lhsT=xT[:, k_off + k, :], rhs=sl[:, k, 0:w], start=(k == 0), stop=(k == kc - 1)),
                  r=[xT, sl], w=[psb])

        def mm_fm(ps_ap, psb, xT, sl, kc, m0, ntok=128):
            for k in range(kc):
                P(lambda e: e.matmul(ps_ap, lhsT=sl[:, k, m0:m0 + 128], rhs=xT[:, k, 0:ntok], start=(k == 0), stop=(k == kc - 1)),
                  r=[xT, sl], w=[psb])

        xt = fw.sb("xt", [128, D], F32)
        h0 = fw.sb("h0", [128, D], F32)
        xnT = fw.sb("xnT", [128, 8, 128], BF16)
        mqT = fw.sb("mqT", [128, 4, 128], BF16)
        mkT = fw.sb("mkT", [128, 4, 128], BF16)
        mk_tm = fw.sb("mk_tm", [128, 4, 128], F32)
        mv_aug = fw.sb("mv_aug", [128, 4, 258], BF16)
        mo_sig = fw.sb("mo_sig", [128, D], BF16)
        gsm = fw.sb("gsm", [128, 40], F32)
        C_f = [fw.sb(f"C_f{h}", [128, 258], F32) for h in range(4)]
        C_b = [fw.sb(f"C_b{h}", [128, 258], BF16) for h in range(4)]
        hm = fw.sb("hm", [128, D], F32)
        hm_n = fw.sb("hm_n", [128, D], F32)
        PTb = [fw.sb(f"PTb{i}", [128, 128], BF16) for i in range(2)]
        kwb = [fw.sb(f"kwb{i}", [128, 128], BF16) for i in range(2)]
        g8 = fw.sb("g8", [128, 8], F32)
        nlf = fw.sb("nlf", [128, 4], F32)
        t1 = fw.sb("t1", [128, 4], F32)
        t2 = fw.sb("t2", [128, 4], F32)
        a_p = fw.sb("a_p", [128, 4], F32)
        ag_p = fw.sb("ag_p", [128, 4], F32)
        eb = fw.sb("eb", [128, 4], F32)
        eG = fw.sb("eG", [128, 4], F32)
        dsc = fw.sb("dsc", [128, 4], F32)
        ssq = fw.sb("ssq", [128, 16], F32)
        junk = fw.sb("junk", [128, 256], F32)
        G(lambda e: e.memset(mv_aug[:], 1.0), w=[mv_aug])

        def dbg_out(name, src_ap, srcb, row0, ncol):
            if name in dbg_d:
                fw.dma('sp', dbg_d[name].t[row0:row0 + 128, 0:ncol], src_ap, r=[srcb], w=[dbg_d[name]])

        BLK = fw.sb("BLK", [128, 128], F32)
        Ublk = fw.sb("Ublk", [128, 128], F32)
        Yblk = fw.sb("Yblk", [128, 128], F32)
        maskS = fw.sb("maskS", [128, 128], F32)
        sel0 = fw.sb("sel0", [128, 128], F32)
        sel1 = fw.sb("sel1", [128, 128], F32)
        G(lambda e: e.memset(BLK[:], 0.0), w=[BLK])
        G(lambda e: e.memset(BLK[0:64, 0:64], 1.0), w=[BLK])
        G(lambda e: e.memset(BLK[64:128, 64:128], 1.0), w=[BLK])
        G(lambda e: e.memset(sel0[:], 0.0), w=[sel0])
        G(lambda e: e.memset(sel0[0:64, :], 1.0), w=[sel0])
        G(lambda e: e.memset(sel1[:], 0.0), w=[sel1])
        G(lambda e: e.memset(sel1[64:128, :], 1.0), w=[sel1])
        G(lambda e: e.tensor_tensor(out=Ublk[:], in0=UT[:], in1=BLK[:], op=ALU.mult), r=[UT, BLK], w=[Ublk])
        G(lambda e: e.tensor_tensor(out=Yblk[:], in0=BLK[:], in1=Ublk[:], op=ALU.subtract), r=[BLK, Ublk], w=[Yblk])
        G(lambda e: e.tensor_tensor(out=maskS[:], in0=Ublk[:], in1=ident[:], op=ALU.subtract), r=[Ublk, ident], w=[maskS])
        cw_tm = fw.sb("cw_tm", [32, 4, 128], F32)
        cw = fw.sb("cw", [128, 4, 32], F32)
        fw.dma('sp', cw_tm[:], conv_w_d.t.rearrange("j (c p) -> c j p", p=128), r=[conv_w_d], w=[cw_tm])
        bkc = bank()
        for j in range(4):
            P(lambda e: e.transpose(bkc[:, j * 32:(j + 1) * 32], cw_tm[0:32, j, :], ident[0:32, 0:32]), r=[cw_tm, ident], w=[bkc])
        V(lambda e: e.tensor_copy(out=cw[:].rearrange("p j c -> p (j c)"), in_=bkc[:, 0:128]), r=[bkc], w=[cw])
        expA = fw.sb("expA", [128, 16], F32)
        dtb = fw.sb("dtb", [128, 16], F32)
        fw.dma('sp', expA[:], a_log_d.t.partition_broadcast(128), r=[a_log_d], w=[expA])
        fw.dma('sp', dtb[:], dt_bias_d.t.partition_broadcast(128), r=[dt_bias_d], w=[dtb])
        A(lambda e: e.activation(out=expA[:], in_=expA[:], func=AF.Exp), r=[expA], w=[expA])
        halo = fw.sb("halo", [128, 32, 3], F32)
        zcb = [fw.sb(f"zc{i}", [128, 131], F32) for i in range(6)]
        ycb = [fw.sb(f"yc{i}", [128, 128], F32) for i in range(6)]
        scb = [fw.sb(f"sc{i}", [128, 128], F32) for i in range(6)]
        sqb = [fw.sb(f"sq{i}", [128, 128], F32) for i in range(6)]
        gqT = fw.sb("gqT", [128, 8, 128], BF16)
        gkT = fw.sb("gkT", [128, 8, 128], BF16)
        k_tm = fw.sb("k_tm", [128, 8, 128], F32)
        v_tm = fw.sb("v_tm", [128, 16, 128], BF16)
        gz_silu = fw.sb("gz_silu", [128, 2048], BF16)
        gg = fw.sb("gg", [128, 16 * 8], F32)
        S_f = [fw.sb(f"S_f{h}", [128, 128], F32) for h in range(16)]
        S_b = [fw.sb(f"S_b{h}", [128, 128], BF16) for h in range(16)]
        vnew = [fw.sb(f"vnew{h}", [128, 128], BF16) for h in range(16)]
        o_g = fw.sb("o_g", [128, 2048], F32)
        o_n = o_g
        xnT_f = Buf(o_g.t[:, 0:1024].rearrange("p (c t) -> p c t", t=128), "xnT_f", parent=o_g)
        GH = 4
        kkS = [fw.sb(f"kkS{i}", [128, 128], F32) for i in range(2)]
        qkS = [fw.sb(f"qkS{i}", [128, 128], F32) for i in range(2)]
        Ld = [fw.sb(f"Ld{i}", [128, 128], F32) for i in range(GH)]
        decb = [fw.sb(f"dec{i}", [128, 128], F32) for i in range(GH)]
        Nb = [[fw.sb(f"N{i}_{j}", [128, 128], F32) for j in range(2)] for i in range(GH)]
        Mb = [[fw.sb(f"M{i}_{j}", [128, 128], F32) for j in range(2)] for i in range(GH)]
        Rf = [fw.sb(f"Rf{i}", [128, 128], F32) for i in range(GH)]
        attnT = [fw.sb(f"attnT{i}", [128, 128], BF16) for i in range(8)]
        Rb = [fw.sb(f"Rb{i}", [128, 128], BF16) for i in range(8)]
        negWT = [fw.sb(f"negWT{i}", [128, 128], BF16) for i in range(8)]
        kd = [fw.sb(f"kd{i}", [128, 128], BF16) for i in range(8)]
        kgb = [fw.sb(f"kg{i}", [128, 128], BF16) for i in range(2)]
        p2s = [fw.sb(f"p2s{i}", [128, 128], F32) for i in range(2)]
        BETA, NBETA, NGD, EG, EGR, TMPG, ES = 0, 16, 32, 48, 64, 80, 96

        w_rt = fw.sb("w_rt", [128, 8, NE], F32)
        b_rt = fw.sb("b_rt", [128, NE], F32)
        with nc.allow_non_contiguous_dma(reason="router weights"):
            fw.dma('sp', w_rt[:], w_router_d.t.rearrange("(kc p) n -> p kc n", p=128), r=[w_router_d], w=[w_rt])
        fw.dma('sp', b_rt[:], b_router_d.t.partition_broadcast(128), r=[b_router_d], w=[b_rt])
        rt = fw.sb("rt", [128, 8, NE], F32)
        top8 = fw.sb("top8", [128, 24], F32)
        UTs = fw.sb("UTs", [128, 128], F32)
        G(lambda e: e.tensor_tensor(out=UTs[:], in0=UT[:], in1=ident[:], op=ALU.subtract), r=[UT, ident], w=[UTs])
        base_c = fw.sb("base_c", [128, NE], F32)
        G(lambda e: e.memset(base_c[:], 0.0), w=[base_c])
        eoff_i = fw.sb("eoff_i", [128, NE], I32)
        eoff = fw.sb("eoff", [128, NE], F32)
        G(lambda e: e.iota(eoff_i[:], pattern=[[CAP, NE]], base=0, channel_multiplier=0), w=[eoff_i])
        V(lambda e: e.tensor_copy(out=eoff[:], in_=eoff_i[:]), r=[eoff_i], w=[eoff])
        hmT = fw.sb("hmT", [128, 8, 128], BF16)
        oT = fw.sb("oT", [128, 16, 128], BF16)
        gsig = [fw.sb(f"gsig{i}", [128, 512], F32) for i in range(2)]
        memT = Buf(gz_silu.t[:].rearrange("p (k t) -> p k t", t=256), "memT", parent=gz_silu)
        KT = fw.sb("KT", [128, 8, 256], BF16)
        Vx = fw.sb("Vx", [128, 2, 1024], BF16)
        sc_all = fw.sb("sc_all", [128, 4, 256], F32)
        pT_all = fw.sb("pT_all", [128, 8, 128], BF16)
        smx = fw.sb("smx", [128, 16], F32)
        merged = hm
        h1 = xt
        h2 = hm_n

        import os as _os
        _prof = False
        cur_scope = [None]

        def mark(name, active=True):
            if cur_scope[0] is not None:
                nc.leave_named_scope(cur_scope[0][0], cur_scope[0][1], False)
                cur_scope[0] = None
            if _prof and active and name:
                sid, _ = nc.enter_named_scope(name, False)
                cur_scope[0] = (name, sid)

        for si in range(n_seq):
            for b_ in C_f + C_b + S_f + S_b:
                G(lambda e: e.memset(b_[:], 0.0), w=[b_])
            G(lambda e: e.memset(halo[:], 0.0), w=[halo])

            if stage >= 4:
                for mt in range(2):
                    fw.dma('sp', o_g[:, 0:1024], mem_d.t[si * MEM + mt * 128: si * MEM + (mt + 1) * 128, :], r=[mem_d], w=[o_g])
                    for c0 in range(0, 8, 4):
                        bk = bank()
                        for c in range(4):
                            P(lambda e: e.transpose(bk[:, c * 128:(c + 1) * 128], o_g[:, (c0 + c) * 128:(c0 + c + 1) * 128], ident[:]), r=[o_g, ident], w=[bk])
                        A(lambda e: e.activation(out=memT[:, c0:c0 + 4, mt * 128:(mt + 1) * 128], in_=bk[:, 0:512].rearrange("p (c t) -> p c t", t=128), func=AF.Copy),
                          r=[bk], w=[memT])
                for j in range(2):
                    sl = load_slab(xa_wk_b, 0, 8, j * 512, 512)
                    for m in range(4):
                        bk = bank()
                        mm_fm(bk[:, 0:256], bk, memT, sl, 8, m * 128, ntok=256)
                        A(lambda e: e.activation(out=KT[:, j * 4 + m, :], in_=bk[:, 0:256], func=AF.Copy), r=[bk], w=[KT])
                for j in range(2):
                    sl = load_slab(xa_wv_b, 0, 8, j * 512, 512)
                    for mt in range(2):
                        bk = bank()
                        for k in range(8):
                            P(lambda e: e.matmul(bk[:, 0:512], lhsT=memT[:, k, mt * 128:(mt + 1) * 128], rhs=sl[:, k, 0:512], start=(k == 0), stop=(k == 7)),
                              r=[memT, sl], w=[bk])
                        A(lambda e: e.activation(out=Vx[:, mt, j * 512:(j + 1) * 512], in_=bk[:, 0:512], func=AF.Copy), r=[bk], w=[Vx])
            for ti in range(NSUB):
                row0 = si * S + ti * 128
                mark('ln_in', si == 0 and ti == 2)
                fw.dma('sp', xt[:], x_d.t[row0:row0 + 128, :], r=[x_d], w=[xt])
                layernorm(xt, h0, "ln_in_g", "ln_in_b")
                if stage == 0:
                    fw.dma('sp', out_d.t[row0:row0 + 128, :], h0[:], r=[h0], w=[out_d])
                    continue
                transpose_fm(h0, D, dst_bf=xnT, dst_f=xnT_f)
                if stage == -1:
                    V(lambda e: e.tensor_copy(out=hm_n[:].rearrange("p (c t) -> p c t", t=128), in_=xnT[:]), r=[xnT], w=[hm_n])
                    fw.dma('sp', out_d.t[row0:row0 + 128, :], hm_n[:], r=[hm_n], w=[out_d])
                    continue
                mark('proj_small', si == 0 and ti == 2)
                bk = bank()
                for k in range(8):
                    P(lambda e: e.matmul(bk[:, 0:40], lhsT=xnT_f[:, k, :], rhs=w_small[:, k, :], start=(k == 0), stop=(k == 7)),
                      r=[xnT_f, w_small], w=[bk])
                V(lambda e: e.tensor_copy(out=gsm[:], in_=bk[:, 0:40]), r=[bk], w=[gsm])
                mark('proj_mlstm', si == 0 and ti == 2)
                sl = load_slab(w_in_b, 0, 8, 0, 512)
                for m in range(4):
                    bk = bank()
                    mm_fm(bk[:, 0:128], bk, xnT, sl, 8, m * 128)
                    A(lambda e: e.activation(out=mqT[:, m, :], in_=bk[:, 0:128], func=AF.Copy), r=[bk], w=[mqT])
                sl = load_slab(w_in_b, 0, 8, 512, 512)
                for m in range(4):
                    bk = bank()
                    mm_fm(bk[:, 0:128], bk, xnT, sl, 8, m * 128)
                    A(lambda e: e.activation(out=mkT[:, m, :], in_=bk[:, 0:128], func=AF.Copy), r=[bk], w=[mkT])
                bk = bank()
                mm_tm(bk[:, 0:512], bk, xnT, sl, 8, 512)
                V(lambda e: e.tensor_copy(out=mk_tm[:].rearrange("p h d -> p (h d)"), in_=bk[:, 0:512]), r=[bk], w=[mk_tm])
                for j in range(2):
                    sl = load_slab(w_in_b, 0, 8, 1024 + j * 512, 512)
                    bk = bank()
                    mm_tm(bk[:, 0:512], bk, xnT, sl, 8, 512)
                    A(lambda e: e.activation(out=mv_aug[:, 2 * j:2 * j + 2, 0:256], in_=bk[:, 0:512].rearrange("p (h d) -> p h d", d=256),
                                             func=AF.Copy), r=[bk], w=[mv_aug])
                for j in range(2):
                    sl = load_slab(w_in_b, 0, 8, 2048 + j * 512, 512)
                    bk = bank()
                    mm_tm(bk[:, 0:512], bk, xnT, sl, 8, 512)
                    A(lambda e: e.activation(out=mo_sig[:, j * 512:(j + 1) * 512], in_=bk[:, 0:512], func=AF.Sigmoid), r=[bk], w=[mo_sig])
                mark('mlstm_gates', si == 0 and ti == 2)
                V(lambda e: e.tensor_tensor(out=g8[:], in0=gsm[:, 0:8], in1=mlb_t[:], op=ALU.add), r=[gsm, mlb_t], w=[g8])
                A(lambda e: e.activation(out=nlf[:], in_=g8[:, 4:8], func=AF.Exp, scale=-1.0), r=[g8], w=[nlf])
                A(lambda e: e.activation(out=nlf[:], in_=nlf[:], func=AF.Ln, bias=eps_t[:, 3:4], scale=1.0), r=[nlf, eps_t], w=[nlf])
                bk = bank()
                P(lambda e: e.matmul(bk[:, 0:4], lhsT=UT[:], rhs=nlf[:], start=True, stop=True), r=[UT, nlf], w=[bk])
                P(lambda e: e.matmul(bk[:, 4:8], lhsT=ones_f[:], rhs=nlf[:], start=True, stop=True), r=[ones_f, nlf], w=[bk])
                V(lambda e: e.tensor_tensor(out=t1[:], in0=g8[:, 0:4], in1=bk[:, 0:4], op=ALU.add), r=[g8, bk], w=[t1])
                V(lambda e: e.tensor_tensor(out=t2[:], in0=t1[:], in1=bk[:, 4:8], op=ALU.subtract), r=[t1, bk], w=[t2])
                A(lambda e: e.activation(out=a_p[:], in_=t1[:], func=AF.Exp, bias=eps_t[:, 2:3], scale=1.0), r=[t1, eps_t], w=[a_p])
                A(lambda e: e.activation(out=ag_p[:], in_=t2[:], func=AF.Exp, bias=eps_t[:, 2:3], scale=1.0), r=[t2, eps_t], w=[ag_p])
                A(lambda e: e.activation(out=eb[:], in_=bk[:, 0:4], func=AF.Exp, scale=-1.0), r=[bk], w=[eb])
                A(lambda e: e.activation(out=eG[:], in_=bk[:, 4:8], func=AF.Exp, scale=-1.0), r=[bk], w=[eG])
                mark('mlstm_heads', si == 0 and ti == 2)
                for h in range(4):
                    pt = PTb[h % 2]
                    kw = kwb[h % 2]
                    bk = bank()
                    P(lambda e: e.matmul(bk[:, 0:128], lhsT=mkT[:, h, :], rhs=mqT[:, h, :], start=True, stop=True), r=[mkT, mqT], w=[bk])
                    V(lambda e: e.scalar_tensor_tensor(out=pt[:], in0=bk[:, 0:128], scalar=a_p[:, h:h + 1], in1=UT[:],
                                                       op0=ALU.mult, op1=ALU.mult), r=[bk, a_p, UT], w=[pt])
                    bk2 = bank()
                    P(lambda e: e.matmul(bk2[:, 0:257], lhsT=mqT[:, h, :], rhs=C_b[h][:, 0:257], start=True, stop=False), r=[mqT, C_b[h]], w=[bk2])
                    P(lambda e: e.matmul(bk2[:, 0:257], lhsT=pt[:], rhs=mv_aug[:, h, 0:257], start=False, stop=True), r=[pt, mv_aug], w=[bk2])
                    V(lambda e: e.tensor_tensor(out=dsc[:, 0:1], in0=bk2[:, 256:257], in1=eb[:, h:h + 1], op=ALU.mult), r=[bk2, eb], w=[dsc])
                    V(lambda e: e.tensor_scalar(out=dsc[:, 2:3], in0=dsc[:, 0:1], scalar1=-1.0, scalar2=None, op0=ALU.mult), r=[dsc], w=[dsc])
                    V(lambda e: e.scalar_tensor_tensor(out=dsc[:, 1:2], in0=dsc[:, 2:3], scalar=1.0, in1=dsc[:, 0:1], op0=ALU.max, op1=ALU.max),
                      r=[dsc], w=[dsc])
                    V(lambda e: e.reciprocal(out=dsc[:, 2:3], in_=dsc[:, 1:2]), r=[dsc], w=[dsc])
                    V(lambda e: e.tensor_tensor(out=dsc[:, 3:4], in0=dsc[:, 2:3], in1=eb[:, h:h + 1], op=ALU.mult), r=[dsc, eb], w=[dsc])
                    V(lambda e: e.tensor_scalar(out=hm[:, h * 256:(h + 1) * 256], in0=bk2[:, 0:256], scalar1=dsc[:, 3:4], scalar2=None,
                                                op0=ALU.mult), r=[bk2, dsc], w=[hm])
                    V(lambda e: e.tensor_scalar(out=kw[:], in0=mk_tm[:, h, :], scalar1=ag_p[:, h:h + 1], scalar2=None, op0=ALU.mult),
                      r=[mk_tm, ag_p], w=[kw])
                    bk3 = bank()
                    P(lambda e: e.matmul(bk3[:, 0:257], lhsT=kw[:], rhs=mv_aug[:, h, 0:257], start=True, stop=True), r=[kw, mv_aug], w=[bk3])
                    V(lambda e: e.scalar_tensor_tensor(out=C_f[h][:, 0:257], in0=C_f[h][:, 0:257], scalar=eG[:, h:h + 1], in1=bk3[:, 0:257],
                                                       op0=ALU.mult, op1=ALU.add), r=[C_f[h], eG, bk3], w=[C_f[h]])
                    G(lambda e: e.tensor_copy(out=C_b[h][:, 0:257], in_=C_f[h][:, 0:257]), r=[C_f[h]], w=[C_b[h]])
                    A(lambda e: e.activation(out=junk[:], in_=hm[:, h * 256:(h + 1) * 256], func=AF.Square, accum_out=ssq[:, h:h + 1]),
                      r=[hm], w=[junk, ssq])
                V(lambda e: e.tensor_scalar(out=ssq[:, 4:8], in0=ssq[:, 0:4], scalar1=1.0 / 256, scalar2=RMS_EPS, op0=ALU.mult, op1=ALU.add), r=[ssq], w=[ssq])
                G(lambda e: e.tensor_tensor(out=ssq[:, 8:12], in0=ssq[:, 4:8], in1=nhalf[:, 0:4], op=ALU.pow), r=[ssq, nhalf], w=[ssq])
                for h in range(4):
                    V(lambda e: e.scalar_tensor_tensor(out=hm_n[:, h * 256:(h + 1) * 256], in0=hm[:, h * 256:(h + 1) * 256],
                                                       scalar=ssq[:, 8 + h:9 + h], in1=mo_sig[:, h * 256:(h + 1) * 256],
                                                       op0=ALU.mult, op1=ALU.mult), r=[hm, ssq, mo_sig], w=[hm_n])
                dbg_out("d_h0", h0[:], h0, row0, D)
                dbg_out("d_hm", hm_n[:], hm_n, row0, D)
                if stage <= 1:
                    fw.dma('sp', out_d.t[row0:row0 + 128, :], hm_n[:], r=[hm_n], w=[out_d])
                    continue

                mark('gdn_conv', si == 0 and ti == 2)
                for sj in range(8):
                    sl = load_slab(w_in_b, 0, 8, 3080 + sj * 512, 512)
                    for m in range(4):
                        c = sj * 4 + m
                        zc = zcb[c % 6]; yc = ycb[c % 6]
                        bk = bank()
                        mm_fm(bk[:, 0:128], bk, xnT, sl, 8, m * 128)
                        G(lambda e: e.tensor_copy(out=zc[:, 0:3], in_=halo[:, c, :]), r=[halo], w=[zc])
                        A(lambda e: e.activation(out=zc[:, 3:131], in_=bk[:, 0:128], func=AF.Copy), r=[bk], w=[zc])
                        A(lambda e: e.activation(out=yc[:], in_=bk[:, 0:128], func=AF.Copy, scale=cw[:, 3, c:c + 1]), r=[bk, cw], w=[yc])
                        G(lambda e: e.tensor_copy(out=halo[:, c, :], in_=zc[:, 128:131]), r=[zc], w=[halo])
                    for m in range(4):
                        c = sj * 4 + m
                        zc = zcb[c % 6]; yc = ycb[c % 6]
                        for j in range(0, 3):
                            V(lambda e: e.scalar_tensor_tensor(out=yc[:], in0=zc[:, j:j + 128], scalar=cw[:, j, c:c + 1], in1=yc[:],
                                                               op0=ALU.mult, op1=ALU.add), r=[zc, cw, yc], w=[yc])
                    for m in range(4):
                        c = sj * 4 + m
                        yc = ycb[c % 6]; sc = scb[c % 6]
                        A(lambda e: e.activation(out=sc[:], in_=yc[:], func=AF.Silu), r=[yc], w=[sc])
                    for m in range(4):
                        c = sj * 4 + m
                        sc = scb[c % 6]; sq = sqb[c % 6]
                        if c < 16:
                            G(lambda e: e.tensor_tensor(out=sq[:], in0=sc[:], in1=sc[:], op=ALU.mult), r=[sc], w=[sq])
                            bk2 = bank()
                            P(lambda e: e.matmul(bk2[:, 0:128], lhsT=ones_f[:], rhs=sq[:], start=True, stop=True), r=[ones_f, sq], w=[bk2])
                            A(lambda e: e.activation(out=sq[:], in_=bk2[:, 0:128], func=AF.Sqrt, bias=eps_t[:, 1:2], scale=1.0), r=[bk2, eps_t], w=[sq])
                            V(lambda e: e.reciprocal(out=sq[:], in_=sq[:]), r=[sq], w=[sq])
                            if c < 8:
                                V(lambda e: e.scalar_tensor_tensor(out=gqT[:, c, :], in0=sc[:], scalar=128 ** -0.5, in1=sq[:],
                                                                   op0=ALU.mult, op1=ALU.mult), r=[sc, sq], w=[gqT])
                            else:
                                V(lambda e: e.tensor_tensor(out=sc[:], in0=sc[:], in1=sq[:], op=ALU.mult), r=[sc, sq], w=[sc])
                                G(lambda e: e.tensor_copy(out=gkT[:, c - 8, :], in_=sc[:]), r=[sc], w=[gkT])
                                bk3 = bank()
                                P(lambda e: e.transpose(bk3[:, 0:128], sc[:], ident[:]), r=[sc, ident], w=[bk3])
                                V(lambda e: e.tensor_copy(out=k_tm[:, c - 8, :], in_=bk3[:, 0:128]), r=[bk3], w=[k_tm])
                        else:
                            bk3 = bank()
                            P(lambda e: e.transpose(bk3[:, 0:128], sc[:], ident[:]), r=[sc, ident], w=[bk3])
                            A(lambda e: e.activation(out=v_tm[:, c - 16, :], in_=bk3[:, 0:128], func=AF.Copy), r=[bk3], w=[v_tm])
                mark('gdn_gz', si == 0 and ti == 2)
                for j in range(4):
                    sl = load_slab(w_in_b, 0, 8, 7176 + j * 512, 512)
                    bk = bank()
                    mm_tm(bk[:, 0:512], bk, xnT, sl, 8, 512)
                    A(lambda e: e.activation(out=gz_silu[:, j * 512:(j + 1) * 512], in_=bk[:, 0:512], func=AF.Silu), r=[bk], w=[gz_silu])
                mark('gdn_gates', si == 0 and ti == 2)
                A(lambda e: e.activation(out=gg[:, BETA:BETA + 16], in_=gsm[:, 24:40], func=AF.Sigmoid), r=[gsm], w=[gg])
                V(lambda e: e.tensor_scalar(out=gg[:, NBETA:NBETA + 16], in0=gg[:, BETA:BETA + 16], scalar1=-1.0, scalar2=None, op0=ALU.mult), r=[gg], w=[gg])
                V(lambda e: e.tensor_tensor(out=gg[:, TMPG:TMPG + 16], in0=gsm[:, 8:24], in1=dtb[:], op=ALU.add), r=[gsm, dtb], w=[gg])
                A(lambda e: e.activation(out=gg[:, TMPG:TMPG + 16], in_=gg[:, TMPG:TMPG + 16], func=AF.Exp), r=[gg], w=[gg])
                A(lambda e: e.activation(out=gg[:, TMPG:TMPG + 16], in_=gg[:, TMPG:TMPG + 16], func=AF.Ln, bias=eps_t[:, 3:4], scale=1.0), r=[gg, eps_t], w=[gg])
                V(lambda e: e.tensor_tensor(out=gg[:, NGD:NGD + 16], in0=gg[:, TMPG:TMPG + 16], in1=expA[:], op=ALU.mult), r=[gg, expA], w=[gg])
                bkg = bank()
                for qi, lm in enumerate([Ublk, BLK, sel0, sel1]):
                    P(lambda e: e.matmul(bkg[:, qi * 16:(qi + 1) * 16], lhsT=lm[:], rhs=gg[:, NGD:NGD + 16], start=True, stop=True), r=[lm, gg], w=[bkg])
                A(lambda e: e.activation(out=gg[:, EG:EG + 16], in_=bkg[:, 0:16], func=AF.Exp, scale=-1.0), r=[bkg], w=[gg])
                A(lambda e: e.activation(out=gg[:, TMPG:TMPG + 16], in_=bkg[:, 16:32], func=AF.Copy), r=[bkg], w=[gg])
                V(lambda e: e.tensor_tensor(out=gg[:, TMPG:TMPG + 16], in0=bkg[:, 0:16], in1=gg[:, TMPG:TMPG + 16], op=ALU.subtract), r=[bkg, gg], w=[gg])
                A(lambda e: e.activation(out=gg[:, EGR:EGR + 16], in_=gg[:, TMPG:TMPG + 16], func=AF.Exp), r=[gg], w=[gg])
                A(lambda e: e.activation(out=gg[:, ES:ES + 32], in_=bkg[:, 32:64], func=AF.Exp, scale=-1.0), r=[bkg], w=[gg])
                for half in range(2):
                  mark('gdn_inv%d' % half, si == 0 and ti == 2)
                  for g0 in range(half * 8, half * 8 + 8, GH):
                      for q2 in range(GH // 2):
                          hq = g0 // 2 + q2
                          bk = bank()
                          P(lambda e: e.matmul(bk[:, 0:128], lhsT=gkT[:, hq, :], rhs=gkT[:, hq, :], start=True, stop=True), r=[gkT], w=[bk])
                          V(lambda e: e.tensor_tensor(out=kkS[q2][:], in0=bk[:, 0:128], in1=maskS[:], op=ALU.mult), r=[bk, maskS], w=[kkS[q2]])
                          bk = bank()
                          P(lambda e: e.matmul(bk[:, 0:128], lhsT=gkT[:, hq, :], rhs=gqT[:, hq, :], start=True, stop=True), r=[gkT, gqT], w=[bk])
                          V(lambda e: e.tensor_tensor(out=qkS[q2][:], in0=bk[:, 0:128], in1=Ublk[:], op=ALU.mult), r=[bk, Ublk], w=[qkS[q2]])
                      for i in range(GH):
                          hv = g0 + i
                          G(lambda e: e.tensor_scalar(out=Ld[i][:], in0=Yblk[:], scalar1=gg[:, NGD + hv:NGD + hv + 1], scalar2=1.0,
                                                      op0=ALU.mult, op1=ALU.mult), r=[Yblk, gg], w=[Ld[i]])
                      for i in range(GH):
                          hv = g0 + i
                          bk = bank()
                          P(lambda e: e.matmul(bk[:, 0:128], lhsT=Ld[i][:], rhs=Ublk[:], start=True, stop=True), r=[Ld[i], Ublk], w=[bk])
                          A(lambda e: e.activation(out=decb[i][:], in_=bk[:, 0:128], func=AF.Exp, scale=-1.0), r=[bk], w=[decb[i]])
                          V(lambda e: e.scalar_tensor_tensor(out=Nb[i][0][:], in0=kkS[i // 2][:], scalar=gg[:, NBETA + hv:NBETA + hv + 1], in1=decb[i][:],
                                                             op0=ALU.mult, op1=ALU.mult), r=[kkS[i // 2], gg, decb[i]], w=[Nb[i][0]])
                          G(lambda e: e.tensor_tensor(out=attnT[hv % 8][:], in0=qkS[i // 2][:], in1=decb[i][:], op=ALU.mult), r=[qkS[i // 2], decb[i]], w=[attnT[hv % 8]])
                          G(lambda e: e.tensor_tensor(out=Rf[i][:], in0=Nb[i][0][:], in1=ident[:], op=ALU.add), r=[Nb[i][0], ident], w=[Rf[i]])
                      for i in range(GH):
                          bk = bank()
                          P(lambda e: e.transpose(bk[:, 0:128], Nb[i][0][:], ident[:]), r=[Nb[i][0], ident], w=[bk])
                          A(lambda e: e.activation(out=Mb[i][0][:], in_=bk[:, 0:128], func=AF.Copy), r=[bk], w=[Mb[i][0]])
                      cur = 0
                      for lv in range(5):
                          nxt = 1 - cur
                          for i in range(GH):
                              bk = bank()
                              P(lambda e: e.matmul(bk[:, 0:128], lhsT=Nb[i][cur][:], rhs=Mb[i][cur][:], start=True, stop=True), r=[Nb[i][cur], Mb[i][cur]], w=[bk])
                              A(lambda e: e.activation(out=Mb[i][nxt][:], in_=bk[:, 0:128], func=AF.Copy), r=[bk], w=[Mb[i][nxt]])
                          if lv < 4:
                              for i in range(GH):
                                  bk = bank()
                                  P(lambda e: e.matmul(bk[:, 0:128], lhsT=Mb[i][cur][:], rhs=Nb[i][cur][:], start=True, stop=True), r=[Nb[i][cur], Mb[i][cur]], w=[bk])
                                  V(lambda e: e.tensor_copy(out=Nb[i][nxt][:], in_=bk[:, 0:128]), r=[bk], w=[Nb[i][nxt]])
                          for i in range(GH):
                              bk = bank()
                              P(lambda e: e.matmul(bk[:, 0:128], lhsT=Mb[i][nxt][:], rhs=Rf[i][:], start=True, stop=True), r=[Mb[i][nxt], Rf[i]], w=[bk])
                              V(lambda e: e.tensor_tensor(out=Rf[i][:], in0=Rf[i][:], in1=bk[:, 0:128], op=ALU.add), r=[Rf[i], bk], w=[Rf[i]])
                          cur = nxt
                      for i in range(GH):
                          hv = g0 + i
                          hq = hv // 2
                          kg = kgb[i % 2]
                          G(lambda e: e.tensor_copy(out=Rb[hv % 8][:], in_=Rf[i][:]), r=[Rf[i]], w=[Rb[hv % 8]])
                          V(lambda e: e.tensor_scalar(out=kg[:], in0=k_tm[:, hq, :], scalar1=gg[:, EG + hv:EG + hv + 1], scalar2=None, op0=ALU.mult),
                            r=[k_tm, gg], w=[kg])
                          V(lambda e: e.tensor_scalar(out=kd[hv % 8][:], in0=k_tm[:, hq, :], scalar1=gg[:, EGR + hv:EGR + hv + 1], scalar2=None, op0=ALU.mult),
                            r=[k_tm, gg], w=[kd[hv % 8]])
                          bk = bank()
                          P(lambda e: e.matmul(bk[:, 0:128], lhsT=kg[:], rhs=Rb[hv % 8][:], start=True, stop=True), r=[kg, Rb[hv % 8]], w=[bk])
                          A(lambda e: e.activation(out=negWT[hv % 8][:], in_=bk[:, 0:128], func=AF.Copy, scale=-1.0), r=[bk], w=[negWT[hv % 8]])
                  mark('gdn_rec%d' % half, si == 0 and ti == 2)
                  for c2 in range(2):
                      hs = slice(c2 * 64, c2 * 64 + 64)
                      for hv in range(half * 8, half * 8 + 8):
                          bk = bank()
                          P(lambda e: e.matmul(bk[hs, 0:128], lhsT=Rb[hv % 8][hs, hs], rhs=v_tm[hs, hv, :], start=True, stop=False), r=[Rb[hv % 8], v_tm], w=[bk])
                          P(lambda e: e.matmul(bk[hs, 0:128], lhsT=negWT[hv % 8][:, hs], rhs=S_b[hv][:], start=False, stop=True), r=[negWT[hv % 8], S_b[hv]], w=[bk])
                          A(lambda e: e.activation(out=vnew[hv][hs, :], in_=bk[hs, 0:128], func=AF.Copy, scale=gg[hs, BETA + hv:BETA + hv + 1]),
                            r=[bk, gg], w=[vnew[hv]])
                      for hv in range(half * 8, half * 8 + 8):
                          hq = hv // 2
                          p2 = p2s[hv % 2]
                          bk = bank()
                          P(lambda e: e.matmul(bk[hs, 0:128], lhsT=gqT[:, hq, hs], rhs=S_b[hv][:], start=True, stop=True), r=[gqT, S_b[hv]], w=[bk])
                          P(lambda e: e.matmul(bk[hs, 128:256], lhsT=attnT[hv % 8][hs, hs], rhs=vnew[hv][hs, :], start=True, stop=True), r=[attnT[hv % 8], vnew[hv]], w=[bk])
                          A(lambda e: e.activation(out=p2[hs, :], in_=bk[hs, 128:256], func=AF.Copy), r=[bk], w=[p2])
                          V(lambda e: e.scalar_tensor_tensor(out=o_g[hs, hv * 128:(hv + 1) * 128], in0=bk[hs, 0:128], scalar=gg[hs, EG + hv:EG + hv + 1],
                                                             in1=p2[hs, :], op0=ALU.mult, op1=ALU.add), r=[bk, gg, p2], w=[o_g])
                          bk2 = bank()
                          P(lambda e: e.matmul(bk2[:, 0:128], lhsT=kd[hv % 8][hs, :], rhs=vnew[hv][hs, :], start=True, stop=True), r=[kd[hv % 8], vnew[hv]], w=[bk2])
                          V(lambda e: e.scalar_tensor_tensor(out=S_f[hv][:], in0=S_f[hv][:], scalar=gg[:, ES + c2 * 16 + hv:ES + c2 * 16 + hv + 1],
                                                             in1=bk2[:, 0:128], op0=ALU.mult, op1=ALU.add), r=[S_f[hv], gg, bk2], w=[S_f[hv]])
                          G(lambda e: e.tensor_copy(out=S_b[hv][:], in_=S_f[hv][:]), r=[S_f[hv]], w=[S_b[hv]])
                mark('gdn_norm', si == 0 and ti == 2)
                for hv in range(16):
                    A(lambda e: e.activation(out=junk[:, 0:128], in_=o_g[:, hv * 128:(hv + 1) * 128], func=AF.Square, accum_out=ssq[:, hv:hv + 1]),
                      r=[o_g], w=[junk, ssq])
                V(lambda e: e.tensor_scalar(out=ssq[:, 0:16], in0=ssq[:, 0:16], scalar1=1.0 / 128, scalar2=RMS_EPS, op0=ALU.mult, op1=ALU.add), r=[ssq], w=[ssq])
                G(lambda e: e.tensor_tensor(out=ssq[:, 0:16], in0=ssq[:, 0:16], in1=nhalf[:, 0:16], op=ALU.pow), r=[ssq, nhalf], w=[ssq])
                for hv in range(16):
                    V(lambda e: e.scalar_tensor_tensor(out=o_g[:, hv * 128:(hv + 1) * 128], in0=o_g[:, hv * 128:(hv + 1) * 128],
                                                       scalar=ssq[:, hv:hv + 1], in1=gz_silu[:, hv * 128:(hv + 1) * 128],
                                                       op0=ALU.mult, op1=ALU.mult), r=[o_g, ssq, gz_silu], w=[o_g])
                dbg_out("d_o", o_n[:], o_n, row0, 2048)
                dbg_out("d_kg", k_tm[:].rearrange("p h d -> p (h d)"), k_tm, row0, 1024)
                if stage <= 2:
                    fw.dma('sp', out_d.t[row0:row0 + 128, :], o_n[:, 0:1024], r=[o_n], w=[out_d])
                    continue

                mark('branch_mix', si == 0 and ti == 2)
                transpose_fm(hm_n, 1024, dst_bf=hmT)
                transpose_fm(o_n, 2048, dst_bf=oT)
                for j in range(2):
                    gs = gsig[0]
                    sl = load_slab(w_in_b, 0, 8, 9256 + j * 512, 512)
                    bk = bank()
                    mm_tm(bk[:, 0:512], bk, xnT, sl, 8, 512)
                    A(lambda e: e.activation(out=gs[:], in_=bk[:, 0:512], func=AF.Sigmoid), r=[bk], w=[gs])
                    sl = load_slab(w_br_ml_b, 0, 8, j * 512, 512)
                    bk = bank()
                    mm_tm(bk[:, 0:512], bk, hmT, sl, 8, 512)
                    V(lambda e: e.tensor_tensor(out=merged[:, j * 512:(j + 1) * 512], in0=bk[:, 0:512], in1=gs[:], op=ALU.mult), r=[bk, gs], w=[merged])
                    gs = gsig[1]
                    sl = load_slab(w_in_b, 0, 8, 10280 + j * 512, 512)
                    bk = bank()
                    mm_tm(bk[:, 0:512], bk, xnT, sl, 8, 512)
                    A(lambda e: e.activation(out=gs[:], in_=bk[:, 0:512], func=AF.Sigmoid), r=[bk], w=[gs])
                    bk = bank()
                    for kh in range(2):
                        sl = load_slab(w_br_gdn_b, kh * 8, 8, j * 512, 512)
                        for k in range(8):
                            P(lambda e: e.matmul(bk[:, 0:512], lhsT=oT[:, kh * 8 + k, :], rhs=sl[:, k, 0:512], start=(kh == 0 and k == 0), stop=(kh == 1 and k == 7)),
                              r=[oT, sl], w=[bk])
                    V(lambda e: e.tensor_tensor(out=gs[:], in0=bk[:, 0:512], in1=gs[:], op=ALU.mult), r=[bk, gs], w=[gs])
                    V(lambda e: e.tensor_tensor(out=merged[:, j * 512:(j + 1) * 512], in0=merged[:, j * 512:(j + 1) * 512], in1=gs[:], op=ALU.add), r=[merged, gs], w=[merged])
                transpose_fm(merged, 1024, dst_bf=hmT)
                for j in range(2):
                    sl = load_slab(w_mix_b, 0, 8, j * 512, 512)
                    bk = bank()
                    mm_tm(bk[:, 0:512], bk, hmT, sl, 8, 512)
                    V(lambda e: e.scalar_tensor_tensor(out=h1[:, j * 512:(j + 1) * 512], in0=h0[:, j * 512:(j + 1) * 512], scalar=DN_ALPHA, in1=bk[:, 0:512],
                                                       op0=ALU.mult, op1=ALU.add), r=[h0, bk], w=[h1])
                layernorm(h1, h1, "ln1_g", "ln1_b")
                dbg_out("d_h1", h1[:], h1, row0, D)
                if stage <= 3:
                    fw.dma('sp', out_d.t[row0:row0 + 128, :], h1[:], r=[h1], w=[out_d])
                    continue
                mark('xa', si == 0 and ti == 2)
                import os
                XC = 0
                if XC == 1:
                    fw.dma('sp', out_d.t[row0:row0 + 128, :], h1[:], r=[h1], w=[out_d])
                    continue
                transpose_fm(h1, 1024, dst_bf=xnT)
                for j in range(2):
                    sl = load_slab(xa_wq_b, 0, 8, j * 512, 512)
                    for m in range(4):
                        bk = bank()
                        mm_fm(bk[:, 0:128], bk, xnT, sl, 8, m * 128)
                        A(lambda e: e.activation(out=gqT[:, j * 4 + m, :], in_=bk[:, 0:128], func=AF.Copy, scale=1.0 / 16), r=[bk], w=[gqT])
                if XC == 2:
                    fw.dma('sp', out_d.t[row0:row0 + 128, :], h1[:], r=[h1], w=[out_d])
                    continue
                bkS = [bank(), bank()]
                for hh in range(4):
                    bk = bkS[hh // 2]
                    for c in range(2):
                        P(lambda e: e.matmul(bk[:, (hh % 2) * 256:(hh % 2 + 1) * 256], lhsT=gqT[:, 2 * hh + c, :], rhs=KT[:, 2 * hh + c, :], start=(c == 0), stop=(c == 1)),
                          r=[gqT, KT], w=[bk])
                for i2 in range(2):
                    V(lambda e: e.tensor_reduce(out=smx[:, 2 * i2:2 * i2 + 2], in_=bkS[i2][:, 0:512].rearrange("p (h m) -> p h m", m=256), axis=AX.X, op=ALU.max),
                      r=[bkS[i2]], w=[smx])
                V(lambda e: e.tensor_scalar(out=smx[:, 4:8], in0=smx[:, 0:4], scalar1=-1.0, scalar2=None, op0=ALU.mult), r=[smx], w=[smx])
                for hh in range(4):
                    A(lambda e: e.activation(out=sc_all[:, hh, :], in_=bkS[hh // 2][:, (hh % 2) * 256:(hh % 2 + 1) * 256], func=AF.Exp, bias=smx[:, 4 + hh:5 + hh], scale=1.0,
                                             accum_out=smx[:, 8 + hh:9 + hh]), r=[bkS[hh // 2], smx], w=[sc_all, smx])
                V(lambda e: e.reciprocal(out=smx[:, 12:16], in_=smx[:, 8:12]), r=[smx], w=[smx])
                for hh in range(4):
                    V(lambda e: e.tensor_scalar(out=sc_all[:, hh, :], in0=sc_all[:, hh, :], scalar1=smx[:, 12 + hh:13 + hh], scalar2=None, op0=ALU.mult), r=[sc_all, smx], w=[sc_all])
                for i2 in range(2):
                    bk = bank()
                    for q in range(4):
                        hh = 2 * i2 + q // 2
                        mt = q % 2
                        P(lambda e: e.transpose(bk[:, q * 128:(q + 1) * 128], sc_all[:, hh, mt * 128:(mt + 1) * 128], ident[:]), r=[sc_all, ident], w=[bk])
                    A(lambda e: e.activation(out=pT_all[:, 4 * i2:4 * i2 + 4, :], in_=bk[:, 0:512].rearrange("p (c t) -> p c t", t=128), func=AF.Copy), r=[bk], w=[pT_all])
                for i2 in range(2):
                    bk = bank()
                    for q in range(4):
                        hh = 2 * i2 + q // 2
                        c = q % 2
                        for mt in range(2):
                            P(lambda e: e.matmul(bk[:, q * 128:(q + 1) * 128], lhsT=Vx[:, mt, (2 * hh + c) * 128:(2 * hh + c + 1) * 128], rhs=pT_all[:, 2 * hh + mt, :],
                                                 start=(mt == 0), stop=(mt == 1)), r=[Vx, pT_all], w=[bk])
                    A(lambda e: e.activation(out=gkT[:, 4 * i2:4 * i2 + 4, :], in_=bk[:, 0:512].rearrange("p (c t) -> p c t", t=128), func=AF.Copy), r=[bk], w=[gkT])
                if XC in (3, 4, 5):
                    fw.dma('sp', out_d.t[row0:row0 + 128, :], h1[:], r=[h1], w=[out_d])
                    continue
                for j in range(2):
                    sl = load_slab(xa_wo_b, 0, 8, j * 512, 512)
                    bk = bank()
                    mm_tm(bk[:, 0:512], bk, gkT, sl, 8, 512)
                    V(lambda e: e.scalar_tensor_tensor(out=h2[:, j * 512:(j + 1) * 512], in0=h1[:, j * 512:(j + 1) * 512], scalar=DN_ALPHA, in1=bk[:, 0:512],
                                                       op0=ALU.mult, op1=ALU.add), r=[h1, bk], w=[h2])
                layernorm(h2, h2, "ln2_g", "ln2_b")
                dbg_out("d_h2", h2[:], h2, row0, D)
                if stage <= 4:
                    fw.dma('sp', out_d.t[row0:row0 + 128, :], h2[:], r=[h2], w=[out_d])
                    continue
                mark('router', si == 0 and ti == 2)
                fw.dma('sp', h2_d.t[row0:row0 + 128, :], h2[:], r=[h2], w=[h2_d])
                transpose_fm(h2, 1024, dst_f=xnT_f)
                bk = bank()
                for k in range(8):
                    P(lambda e: e.matmul(bk[:, 0:NE], lhsT=xnT_f[:, k, :], rhs=w_rt[:, k, :], start=(k == 0), stop=(k == 7)), r=[xnT_f, w_rt], w=[bk])
                V(lambda e: e.tensor_tensor(out=rt[:, 0, :], in0=bk[:, 0:NE], in1=b_rt[:], op=ALU.add), r=[bk, b_rt], w=[rt])
                V(lambda e: e.max(out=top8[:, 0:8], in_=rt[:, 0, :]), r=[rt], w=[top8])
                V(lambda e: e.tensor_scalar(out=rt[:, 1, :], in0=rt[:, 0, :], scalar1=top8[:, 3:4], scalar2=None, op0=ALU.is_ge), r=[rt, top8], w=[rt])
                V(lambda e: e.tensor_scalar(out=top8[:, 8:9], in0=top8[:, 0:1], scalar1=-1.0, scalar2=None, op0=ALU.mult), r=[top8], w=[top8])
                A(lambda e: e.activation(out=rt[:, 2, :], in_=rt[:, 0, :], func=AF.Exp, bias=top8[:, 8:9], scale=1.0), r=[rt, top8], w=[rt])
                V(lambda e: e.tensor_tensor(out=rt[:, 2, :], in0=rt[:, 2, :], in1=rt[:, 1, :], op=ALU.mult), r=[rt], w=[rt])
                V(lambda e: e.tensor_reduce(out=top8[:, 9:10], in_=rt[:, 2, :], axis=AX.X, op=ALU.add), r=[rt], w=[top8])
                V(lambda e: e.reciprocal(out=top8[:, 10:11], in_=top8[:, 9:10]), r=[top8], w=[top8])
                V(lambda e: e.tensor_scalar(out=rt[:, 3, :], in0=rt[:, 2, :], scalar1=top8[:, 10:11], scalar2=None, op0=ALU.mult), r=[rt, top8], w=[rt])
                tg_i = row0 // 128
                bk = bank()
                P(lambda e: e.matmul(bk[:, 0:NE], lhsT=UTs[:], rhs=rt[:, 1, :], start=True, stop=True), r=[UTs, rt], w=[bk])
                P(lambda e: e.matmul(bk[:, NE:2 * NE], lhsT=ones_f[:], rhs=rt[:, 1, :], start=True, stop=True), r=[ones_f, rt], w=[bk])
                V(lambda e: e.tensor_tensor(out=rt[:, 4, :], in0=bk[:, 0:NE], in1=base_c[:], op=ALU.add), r=[bk, base_c], w=[rt])
                V(lambda e: e.tensor_tensor(out=base_c[:], in0=bk[:, NE:2 * NE], in1=base_c[:], op=ALU.add), r=[bk, base_c], w=[base_c])
                V(lambda e: e.tensor_scalar(out=rt[:, 5, :], in0=rt[:, 4, :], scalar1=CAP - 0.5, scalar2=None, op0=ALU.is_lt), r=[rt], w=[rt])
                V(lambda e: e.tensor_tensor(out=rt[:, 5, :], in0=rt[:, 5, :], in1=rt[:, 1, :], op=ALU.mult), r=[rt], w=[rt])
                V(lambda e: e.tensor_tensor(out=rt[:, 4, :], in0=rt[:, 4, :], in1=eoff[:], op=ALU.add), r=[rt, eoff], w=[rt])
                V(lambda e: e.tensor_scalar(out=rt[:, 4, :], in0=rt[:, 4, :], scalar1=-1.0, scalar2=CBIG + 1.0, op0=ALU.mult, op1=ALU.add), r=[rt], w=[rt])
                V(lambda e: e.tensor_tensor(out=rt[:, 6, :], in0=rt[:, 4, :], in1=rt[:, 5, :], op=ALU.mult), r=[rt], w=[rt])
                V(lambda e: e.max(out=top8[:, 12:20], in_=rt[:, 6, :]), r=[rt], w=[top8])
                V(lambda e: e.tensor_scalar(out=top8[:, 20:24], in0=top8[:, 12:16], scalar1=-1.0, scalar2=CBIG + 1.0, op0=ALU.mult, op1=ALU.add), r=[top8], w=[top8])
                V(lambda e: e.tensor_copy(out=slot_i[:, tg_i, :], in_=top8[:, 20:24]), r=[top8], w=[slot_i])
                for k4 in range(4):
                    V(lambda e: e.scalar_tensor_tensor(out=rt[:, 7, :], in0=rt[:, 6, :], scalar=top8[:, 12 + k4:13 + k4], in1=rt[:, 3, :],
                                                       op0=ALU.is_equal, op1=ALU.mult), r=[rt, top8], w=[rt])
                    V(lambda e: e.tensor_reduce(out=gwk[:, tg_i, k4:k4 + 1], in_=rt[:, 7, :], axis=AX.X, op=ALU.add), r=[rt], w=[gwk])
                for k4 in range(4):
                    fw.dma('pool', None, None, r=[h2, slot_i], w=[xg_d],
                           fn=lambda e: e.indirect_dma_start(out=xg_d.t, out_offset=bass.IndirectOffsetOnAxis(ap=slot_i[:, tg_i, k4:k4 + 1], axis=0),
                                                             in_=h2[:], in_offset=None, bounds_check=bc_reg, oob_is_err=False))

        mark(None)
        if stage > 4:
            if "d_cnt" in dbg_d:
                fw.dma('sp', dbg_d["d_cnt"].t[0:128, 0:NE], base_c[:], r=[base_c], w=[dbg_d["d_cnt"]])
            fw.barrier()
            es1.close()
            fw.es = es
            for n in vec_late:
                vec_t[n] = fw.sb("c_" + n, [128, D], F32)
                fw.dma('sp', vec_t[n][:], vec_d[n].t.partition_broadcast(128), r=[vec_d[n]], w=[vec_t[n]])
            es2 = es.enter_context(ExitStack())
            fw.es = es2
            wg = [fw.sb(f"wg{i}", [128, 8, 1024], BF16) for i in range(2)]
            wu = [fw.sb(f"wu{i}", [128, 8, 1024], BF16) for i in range(2)]
            wdn = [fw.sb(f"wdn{i}", [128, 8, 1024], BF16) for i in range(2)]
            bgu_tm = [fw.sb(f"bgu_tm{i}", [16, 128], F32) for i in range(2)]
            bgu = [fw.sb(f"bgu{i}", [128, 16], F32) for i in range(2)]
            bdn_bc = [fw.sb(f"bdn_bc{i}", [128, D], F32) for i in range(2)]
            NXR = max(2, CAP // 128)
            xr = [fw.sb(f"xr{i}", [128, D], F32) for i in range(NXR)]
            xgT = fw.sb("xgT", [128, 8, CAP], BF16)
            actT = fw.sb("actT", [128, 8, CAP], BF16)
            tgb = [fw.sb(f"tg{i}", [128, 512], F32) for i in range(2)]
            tsb = [fw.sb(f"ts{i}", [128, 512], F32) for i in range(2)]
            tub = [fw.sb(f"tu{i}", [128, 512], F32) for i in range(2)]
            ysb = [fw.sb(f"ysb{i}", [128, D], F32) for i in range(2)]
            nchunks = [(n0, min(512, CAP - n0)) for n0 in range(0, CAP, 512)]

            def load_expert(ex):
                st = ex % 2
                wg_v = w_gu_d.t[ex].rearrange("(kc p) n -> p kc n", p=128)
                wd_v = w_dn_d.t[ex].rearrange("(kc p) n -> p kc n", p=128)
                for j in range(2):
                    fw.dma('pool', wg[st][:, :, j * 512:(j + 1) * 512], wg_v[:, :, j * 512:(j + 1) * 512], r=[w_gu_d], w=[wg[st]])
                fw.dma('sp', bgu_tm[st][:], b_gu_d.t[ex].rearrange("(c p) -> c p", p=128), r=[b_gu_d], w=[bgu_tm[st]])
                fw.dma('sp', bdn_bc[st][:], b_dn_d.t[ex:ex + 1, :].partition_broadcast(128), r=[b_dn_d], w=[bdn_bc[st]])

            wst = [fw.sb(f"wst{i}", [128, 4, 512], F32) for i in range(2)]

            def wdn_piece(ex, j):
                kh, ch = (j % 4) // 2, j % 2
                if j < 4:
                    src = w_dn_d.t[ex].rearrange("(kc p) n -> p kc n", p=128)[:, kh * 4:(kh + 1) * 4, ch * 512:(ch + 1) * 512]
                    return src, wdn[ex % 2][:, kh * 4:(kh + 1) * 4, ch * 512:(ch + 1) * 512], w_dn_d, wdn[ex % 2]
                src = w_gu_d.t[ex].rearrange("(kc p) n -> p kc n", p=128)[:, kh * 4:(kh + 1) * 4, 1024 + ch * 512:1024 + (ch + 1) * 512]
                return src, wu[ex % 2][:, kh * 4:(kh + 1) * 4, ch * 512:(ch + 1) * 512], w_gu_d, wu[ex % 2]

            def wdn_load(ex, j):
                src, _, sb_, _ = wdn_piece(ex, j)
                fw.dma('sp', wst[j % 2][:], src, r=[sb_], w=[wst[j % 2]])

            def wdn_cast(ex, j):
                _, dst, _, db_ = wdn_piece(ex, j)
                A(lambda e: e.activation(out=dst, in_=wst[j % 2][:], func=AF.Copy), r=[wst[j % 2]], w=[db_])

            def load_x(ex):
                for i in range(CAP // 128):
                    xx = xr[i % NXR]
                    fw.dma('sp', xx[:], xg_d.t[ex * CAP + i * 128: ex * CAP + (i + 1) * 128, :], r=[xg_d], w=[xx])

            load_expert(0)
            load_x(0)
            for j in range(8):
                wdn_load(0, j)
                wdn_cast(0, j)
            for ex in range(NE):
                st = ex % 2
                if ex + 1 < NE:
                    load_expert(ex + 1)
                bk = bank()
                P(lambda e: e.transpose(bk[:, 0:16], bgu_tm[st][0:16, :], ident[0:16, 0:16]), r=[bgu_tm[st], ident], w=[bk])
                V(lambda e: e.tensor_copy(out=bgu[st][:], in_=bk[:, 0:16]), r=[bk], w=[bgu[st]])
                for i in range(CAP // 128):
                    xx = xr[i % NXR]
                    for c0 in range(0, 8, 4):
                        bk = bank()
                        for c in range(4):
                            P(lambda e: e.transpose(bk[:, c * 128:(c + 1) * 128], xx[:, (c0 + c) * 128:(c0 + c + 1) * 128], ident[:]), r=[xx, ident], w=[bk])
                        pv = bk[:, 0:512].rearrange("p (c t) -> p c t", t=128)
                        if c0 == 0:
                            A(lambda e: e.activation(out=xgT[:, c0:c0 + 4, i * 128:(i + 1) * 128], in_=pv, func=AF.Copy), r=[bk], w=[xgT])
                        else:
                            V(lambda e: e.tensor_copy(out=xgT[:, c0:c0 + 4, i * 128:(i + 1) * 128], in_=pv), r=[bk], w=[xgT])
                if ex + 1 < NE:
                    load_x(ex + 1)
                    wdn_load(ex + 1, 0)
                    wdn_load(ex + 1, 1)
                for f in range(8):
                    if ex + 1 < NE:
                        wdn_cast(ex + 1, f)
                        if f + 2 < 8:
                            wdn_load(ex + 1, f + 2)
                    for ci, (n0, nn) in enumerate(nchunks):
                        tg = tgb[ci % 2]; ts = tsb[ci % 2]; tu = tub[ci % 2]
                        bkg = bank()
                        for k in range(8):
                            P(lambda e: e.matmul(bkg[:, 0:nn], lhsT=wg[st][:, k, f * 128:(f + 1) * 128], rhs=xgT[:, k, n0:n0 + nn], start=(k == 0), stop=(k == 7)),
                              r=[wg[st], xgT], w=[bkg])
                        bku = bank()
                        for k in range(8):
                            P(lambda e: e.matmul(bku[:, 0:nn], lhsT=wu[st][:, k, f * 128:(f + 1) * 128], rhs=xgT[:, k, n0:n0 + nn], start=(k == 0), stop=(k == 7)),
                              r=[wu[st], xgT], w=[bku])
                        V(lambda e: e.tensor_scalar(out=tg[:, 0:nn], in0=bkg[:, 0:nn], scalar1=bgu[st][:, f:f + 1], scalar2=7.0, op0=ALU.add, op1=ALU.min), r=[bkg, bgu[st]], w=[tg])
                        A(lambda e: e.activation(out=ts[:, 0:nn], in_=tg[:, 0:nn], func=AF.Sigmoid, scale=1.702), r=[tg], w=[ts])
                        G(lambda e: e.tensor_tensor(out=tg[:, 0:nn], in0=tg[:, 0:nn], in1=ts[:, 0:nn], op=ALU.mult), r=[tg, ts], w=[tg])
                        V(lambda e: e.tensor_scalar(out=tu[:, 0:nn], in0=bku[:, 0:nn], scalar1=bgu[st][:, 8 + f:9 + f], scalar2=7.0, op0=ALU.add, op1=ALU.min), r=[bku, bgu[st]], w=[tu])
                        V(lambda e: e.tensor_scalar(out=tu[:, 0:nn], in0=tu[:, 0:nn], scalar1=-7.0, scalar2=1.0, op0=ALU.max, op1=ALU.add), r=[tu], w=[tu])
                        G(lambda e: e.tensor_tensor(out=actT[:, f, n0:n0 + nn], in0=tu[:, 0:nn], in1=tg[:, 0:nn], op=ALU.mult), r=[tu, tg], w=[actT])
                for i in range(CAP // 128):
                    ys = ysb[i % 2]
                    for half in range(2):
                        bk = bank()
                        for k in range(8):
                            P(lambda e: e.matmul(bk[:, 0:512], lhsT=actT[:, k, i * 128:(i + 1) * 128], rhs=wdn[st][:, k, half * 512:(half + 1) * 512], start=(k == 0), stop=(k == 7)),
                              r=[actT, wdn[st]], w=[bk])
                        V(lambda e: e.tensor_tensor(out=ys[:, half * 512:(half + 1) * 512], in0=bk[:, 0:512], in1=bdn_bc[st][:, half * 512:(half + 1) * 512], op=ALU.add),
                          r=[bk, bdn_bc[st]], w=[ys])
                    fw.dma('sp', yg_d.t[ex * CAP + i * 128: ex * CAP + (i + 1) * 128, :], ys[:], r=[ys], w=[yg_d])
            fw.barrier()
            es2.close()
            fw.es = es
            fh = [fw.sb(f"fh{i}", [128, D], F32) for i in range(2)]
            gb = [[fw.sb(f"gb{i}_{k}", [128, D], F32) for k in range(4)] for i in range(2)]
            for t in range(T // 128):
                hh2 = fh[t % 2]
                fw.dma('sp', hh2[:], h2_d.t[t * 128:(t + 1) * 128, :], r=[h2_d], w=[hh2])
                for k4 in range(4):
                    gk = gb[t % 2][k4]
                    V(lambda e: e.memset(gk[:], 0.0), w=[gk])
                    fw.dma('pool', None, None, r=[yg_d, slot_i], w=[gk],
                           fn=lambda e: e.indirect_dma_start(out=gk[:], out_offset=None, in_=yg_d.t,
                                                             in_offset=bass.IndirectOffsetOnAxis(ap=slot_i[:, t, k4:k4 + 1], axis=0),
                                                             bounds_check=bc_reg, oob_is_err=False))
                V(lambda e: e.tensor_scalar(out=hh2[:], in0=hh2[:], scalar1=DN_ALPHA, scalar2=None, op0=ALU.mult), r=[hh2], w=[hh2])
                for k4 in range(4):
                    gk = gb[t % 2][k4]
                    V(lambda e: e.scalar_tensor_tensor(out=hh2[:], in0=gk[:], scalar=gwk[:, t, k4:k4 + 1], in1=hh2[:], op0=ALU.mult, op1=ALU.add),
                      r=[gk, gwk, hh2], w=[hh2])
                layernorm(hh2, hh2, "ln3_g", "ln3_b")
                fw.dma('sp', out_d.t[t * 128:(t + 1) * 128, :], hh2[:], r=[hh2], w=[out_d])

        fw.finish([out_d] + list(dbg_d.values()))
        es1.close()
        print("instructions", fw.n_inst, "waits", fw.n_wait, "sems", fw.nsem)
    return nc


_IN_NAMES = ["x", "mem", "ln_in_g", "ln_in_b", "w_in", "ml_gate_bias", "ml_norm_g", "gdn_conv_w", "gdn_a_log",
             "gdn_dt_bias", "gdn_norm_g", "w_branch_ml", "w_branch_gdn", "w_mix_out", "ln1_g", "ln1_b",
             "xa_wq", "xa_wk", "xa_wv", "xa_wo", "ln2_g", "ln2_b", "w_router", "b_router", "w_gu", "b_gu",
             "w_dn", "b_dn", "ln3_g", "ln3_b"]


def make_in_map(inputs, b0, n_seq, S):
    f = lambda a: np.ascontiguousarray(np.asarray(a, dtype=np.float32))
    m = {}
    m["x"] = f(inputs["x"][b0:b0 + n_seq, :S]).reshape(n_seq * S, D)
    m["mem"] = f(inputs["mem"][b0:b0 + n_seq]).reshape(n_seq * MEM, D)
    for n in ["ln_in_g", "ln_in_b"]:
        m[n] = f(inputs[n]).reshape(1, D)
    for n in ["ml_norm_g", "ln1_g", "ln1_b", "ln2_g", "ln2_b", "ln3_g", "ln3_b"]:
        m[n] = f(inputs[n]).reshape(1, D)
    m["w_in"] = f(inputs["w_in"]).reshape(D, IN_W)
    m["ml_gate_bias"] = f(inputs["ml_gate_bias"]).reshape(1, 8)
    m["gdn_conv_w"] = f(inputs["gdn_conv_w"]).reshape(4, 4096)
    m["gdn_a_log"] = f(inputs["gdn_a_log"]).reshape(1, 16)
    m["gdn_dt_bias"] = f(inputs["gdn_dt_bias"]).reshape(1, 16)
    m["gdn_norm_g"] = f(inputs["gdn_norm_g"]).reshape(1, 128)
    m["w_branch_ml"] = f(inputs["w_branch_ml"]).reshape(D, D)
    m["w_branch_gdn"] = f(inputs["w_branch_gdn"]).reshape(2048, D)
    for n, k in [("w_mix_out", "w_mix_out"), ("xa_wq", "xa_wq"), ("xa_wk", "xa_wk"), ("xa_wv", "xa_wv"), ("xa_wo", "xa_wo")]:
        m[n] = f(inputs[k]).reshape(D, D)
    m["w_router"] = f(inputs["w_router"]).reshape(D, NE)
    m["b_router"] = f(inputs["b_router"]).reshape(1, NE)
    m["w_gu"] = f(inputs["w_gu"]).reshape(NE, D, 2 * D)
    m["b_gu"] = f(inputs["b_gu"]).reshape(NE, 2 * D)
    m["w_dn"] = f(inputs["w_dn"]).reshape(NE, D, D)
    m["b_dn"] = f(inputs["b_dn"]).reshape(NE, D)
    return m


def kernel(**inputs):
    n_cores = 8
    n_seq, S = 2, 2048
    nc = build_nc(n_seq, S)
    in_maps = [make_in_map(inputs, c * n_seq, n_seq, S) for c in range(n_cores)]
    res = run_bass_kernel_spmd(nc, in_maps, core_ids=list(range(n_cores)))
    outs = [np.asarray(r["out"], dtype=np.float32).reshape(n_seq, S, D) for r in res.results]
    return np.concatenate(outs, axis=0)
```
